# Optimizing a Trainium2 kernel written in Bass

```python
import math
import jax, jax.numpy as jnp
from jax import lax
import numpy as np

D_MODEL = 2048
BATCH = 2
SEQ = 4096
DEPTH = 1

CHUNK = 64
Q_BLOCK = 2 * CHUNK
HEAD_DIM = 128
SB_HEADS = 8
FOX_HEADS = 8
SB_WIDTH = SB_HEADS * HEAD_DIM
FOX_WIDTH = FOX_HEADS * HEAD_DIM
IN_SPLITS = [SB_WIDTH, SB_WIDTH, SB_WIDTH,
             FOX_WIDTH, FOX_WIDTH, FOX_WIDTH,
             FOX_HEADS,
             D_MODEL, D_MODEL]
IN_WIDTH = sum(IN_SPLITS)
PEER_HEADS = 8
N_KEYS = 128
N_EXPERTS = N_KEYS * N_KEYS
PEER_TOPK = 16
D_KEY = 256
HALF_KEY = D_KEY // 2
PEER_BLOCK = 128
EPS = 1e-6
NEG_INF = -1e30

kernel_name = "hybrid_sb_fox_peer_block"


def rms_norm(x, gain):
    x32 = x.astype(jnp.float32)
    y = x32 * lax.rsqrt(jnp.mean(x32 * x32, axis=-1, keepdims=True) + EPS)
    return (y * gain.astype(jnp.float32)).astype(x.dtype)


def to_heads(t, n_heads):
    b, s, _ = t.shape
    return t.reshape(b, s, n_heads, HEAD_DIM).transpose(0, 2, 1, 3)


def from_heads(t):
    b, h, s, d = t.shape
    return t.transpose(0, 2, 1, 3).reshape(b, s, h * d)


def stick_breaking_attention(q, k, v):
    s_len = q.shape[2]
    scale = 1.0 / math.sqrt(HEAD_DIM)
    outs = []
    for blk in range(s_len // Q_BLOCK):
        t0, t1 = blk * Q_BLOCK, (blk + 1) * Q_BLOCK
        qb = q[:, :, t0:t1].astype(jnp.float32)
        kb = k[:, :, :t1].astype(jnp.float32)
        vb = v[:, :, :t1].astype(jnp.float32)
        z = jnp.einsum('bhqd,bhkd->bhqk', qb, kb) * scale
        t_idx = jnp.arange(t0, t1)[:, None]
        s_idx = jnp.arange(t1)[None, :]
        strict = s_idx < t_idx
        log_1m_beta = jnp.where(strict, jax.nn.log_sigmoid(-z), 0.0)
        suffix = lax.cumsum(log_1m_beta, axis=3, reverse=True) - log_1m_beta
        weights = jnp.where(strict, jnp.exp(jax.nn.log_sigmoid(z) + suffix), 0.0)
        outs.append(jnp.einsum('bhqk,bhkd->bhqd', weights, vb))
    return jnp.concatenate(outs, axis=2).astype(q.dtype)


def forgetting_attention(q, k, v, log_f):
    s_len = q.shape[2]
    scale = 1.0 / math.sqrt(HEAD_DIM)
    cum_f = jnp.cumsum(log_f.astype(jnp.float32), axis=-1)
    outs = []
    for blk in range(s_len // Q_BLOCK):
        t0, t1 = blk * Q_BLOCK, (blk + 1) * Q_BLOCK
        qb = q[:, :, t0:t1].astype(jnp.float32)
        kb = k[:, :, :t1].astype(jnp.float32)
        vb = v[:, :, :t1].astype(jnp.float32)
        logits = jnp.einsum('bhqd,bhkd->bhqk', qb, kb) * scale
        logits = logits + cum_f[:, :, t0:t1, None] - cum_f[:, :, None, :t1]
        causal = jnp.arange(t1)[None, :] <= jnp.arange(t0, t1)[:, None]
        probs = jax.nn.softmax(jnp.where(causal, logits, NEG_INF), axis=-1)
        outs.append(jnp.einsum('bhqk,bhkd->bhqd', probs, vb))
    return jnp.concatenate(outs, axis=2).astype(q.dtype)


def hybrid_mixer(h, w_in, b_forget, w_branch_a, w_branch_b, w_out):
    proj = h @ w_in
    cuts = list(np.cumsum(IN_SPLITS)[:-1])
    q_a, k_a, v_a, q_b, k_b, v_b, f_logit, g_a, g_b = jnp.split(proj, cuts, axis=-1)
    o_a = stick_breaking_attention(to_heads(q_a, SB_HEADS), to_heads(k_a, SB_HEADS),
                                   to_heads(v_a, SB_HEADS))
    log_f = jax.nn.log_sigmoid(f_logit.astype(jnp.float32) + b_forget.astype(jnp.float32))
    log_f = log_f.transpose(0, 2, 1)
    o_b = forgetting_attention(to_heads(q_b, FOX_HEADS), to_heads(k_b, FOX_HEADS),
                               to_heads(v_b, FOX_HEADS), log_f)
    merged = (jax.nn.sigmoid(g_a) * (from_heads(o_a) @ w_branch_a)
              + jax.nn.sigmoid(g_b) * (from_heads(o_b) @ w_branch_b))
    return merged @ w_out


def peer_ffn(h, w_query, sub_keys, expert_u, expert_v):
    b, s, d = h.shape
    n_tok = b * s
    ht = h.reshape(n_tok, d)
    q = (ht @ w_query).astype(jnp.float32).reshape(n_tok, PEER_HEADS, 2, HALF_KEY)
    scores = jnp.einsum('thpc,hpnc->thpn', q, sub_keys.astype(jnp.float32))
    s1, i1 = lax.top_k(scores[:, :, 0], PEER_TOPK)
    s2, i2 = lax.top_k(scores[:, :, 1], PEER_TOPK)
    cand_score = (s1[..., :, None] + s2[..., None, :]).reshape(n_tok, PEER_HEADS, PEER_TOPK * PEER_TOPK)
    cand_id = (i1[..., :, None] * N_KEYS + i2[..., None, :]).reshape(n_tok, PEER_HEADS, PEER_TOPK * PEER_TOPK)
    top_score, pos = lax.top_k(cand_score, PEER_TOPK)
    expert_ids = jnp.take_along_axis(cand_id, pos, axis=-1)
    gates = jax.nn.softmax(top_score, axis=-1)

    n_blk = n_tok // PEER_BLOCK

    def expert_block(args):
        xb, idb, gb = args
        u_sel = expert_u[idb]
        act = jax.nn.gelu(jnp.einsum('thkd,td->thk', u_sel, xb).astype(jnp.float32))
        w = (gb * act).astype(xb.dtype)
        v_sel = expert_v[idb]
        return jnp.einsum('thk,thkd->td', w, v_sel)

    out = lax.map(expert_block, (ht.reshape(n_blk, PEER_BLOCK, d),
                                 expert_ids.reshape(n_blk, PEER_BLOCK, PEER_HEADS, PEER_TOPK),
                                 gates.reshape(n_blk, PEER_BLOCK, PEER_HEADS, PEER_TOPK)))
    return out.reshape(b, s, d)


def setup_inputs(seed: int = 0) -> dict:
    key = jax.random.key(seed)
    ks = jax.random.split(key, 14)
    f32 = jnp.float32
    x = jax.random.normal(ks[0], (BATCH, SEQ, D_MODEL), f32)
    norm_mix_gain = 1.0 + 0.02 * jax.random.normal(ks[1], (DEPTH, D_MODEL), f32)
    w_in = jax.random.normal(ks[2], (DEPTH, D_MODEL, IN_WIDTH), f32) * D_MODEL ** -0.5
    b_forget = jax.random.uniform(ks[3], (DEPTH, FOX_HEADS), f32, minval=1.0, maxval=4.0)
    w_branch_a = jax.random.normal(ks[4], (DEPTH, SB_WIDTH, D_MODEL), f32) * SB_WIDTH ** -0.5
    w_branch_b = jax.random.normal(ks[5], (DEPTH, FOX_WIDTH, D_MODEL), f32) * FOX_WIDTH ** -0.5
    w_out = jax.random.normal(ks[6], (DEPTH, D_MODEL, D_MODEL), f32) * D_MODEL ** -0.5
    norm_ffn_gain = 1.0 + 0.02 * jax.random.normal(ks[7], (DEPTH, D_MODEL), f32)
    w_query = jax.random.normal(ks[8], (DEPTH, D_MODEL, PEER_HEADS * D_KEY), f32) * D_MODEL ** -0.5
    sub_keys = jax.random.normal(ks[9], (DEPTH, PEER_HEADS, 2, N_KEYS, HALF_KEY), f32) * HALF_KEY ** -0.5
    expert_u = jax.random.normal(ks[10], (DEPTH, N_EXPERTS, D_MODEL), f32) * D_MODEL ** -0.5
    expert_v = jax.random.normal(ks[11], (DEPTH, N_EXPERTS, D_MODEL), f32) * PEER_HEADS ** -0.5
    norm_final_gain = 1.0 + 0.02 * jax.random.normal(ks[12], (D_MODEL,), f32)
    return {"x": x, "norm_mix_gain": norm_mix_gain, "w_in": w_in, "b_forget": b_forget,
            "w_branch_a": w_branch_a, "w_branch_b": w_branch_b, "w_out": w_out,
            "norm_ffn_gain": norm_ffn_gain, "w_query": w_query, "sub_keys": sub_keys,
            "expert_u": expert_u, "expert_v": expert_v, "norm_final_gain": norm_final_gain}


def reference(x, norm_mix_gain, w_in, b_forget, w_branch_a, w_branch_b, w_out,
              norm_ffn_gain, w_query, sub_keys, expert_u, expert_v, norm_final_gain):
    for layer in range(DEPTH):
        h = rms_norm(x, norm_mix_gain[layer])
        x = x + hybrid_mixer(h, w_in[layer], b_forget[layer], w_branch_a[layer],
                             w_branch_b[layer], w_out[layer])
        h = rms_norm(x, norm_ffn_gain[layer])
        x = x + peer_ffn(h, w_query[layer], sub_keys[layer], expert_u[layer], expert_v[layer])
    return rms_norm(x, norm_final_gain)
```

```python
import math
from contextlib import ExitStack

import numpy as np
import concourse.bass as bass
import concourse.mybir as mybir
from concourse.bass_utils import run_bass_kernel_spmd

F32 = mybir.dt.float32
BF16 = mybir.dt.bfloat16
U32 = mybir.dt.uint32
AF = mybir.ActivationFunctionType
ALU = mybir.AluOpType
AX = mybir.AxisListType

ENGS = ['pe', 'act', 'dve', 'pool', 'sp']
NRING = 6
EPS = 1e-6
D = 2048
NEG = -30000.0
STAGE = 99
DEBUG = False


def _conflict(a, b):
    n = min(len(a), len(b))
    return a[:n] == b[:n]


class Prog:
    def __init__(self, nc, stack):
        self.nc = nc
        self.ops = {e: [] for e in ENGS}
        self.ncomp = {e: 0 for e in ENGS}
        self.ndma = {e: 0 for e in ENGS}
        self.waited = {e: {} for e in ENGS}
        self.track = {}
        self.S = {e: stack.enter_context(nc.semaphore('S_' + e)) for e in ['pe', 'act', 'dve', 'pool']}
        self.Dm = {e: [stack.enter_context(nc.semaphore('D_%s%d' % (e, i))) for i in range(NRING)]
                   for e in ['sp', 'act', 'pool']}

    def _sem_of(self, dep):
        kind, e, i = dep
        if kind == 'c':
            return ('S', e), self.S[e], i + 1
        return ('D', e, i % NRING), self.Dm[e][i % NRING], 16 * (i // NRING + 1)

    def _collect(self, reads, writes, me):
        deps = set()
        reads = [r if isinstance(r, tuple) else (r,) for r in reads]
        writes = [w if isinstance(w, tuple) else (w,) for w in writes]
        for r in reads:
            tr = self.track.setdefault(r[0], {})
            for k, (lw, rd) in tr.items():
                if _conflict(k, r) and lw is not None:
                    deps.add(lw)
        for w in writes:
            tr = self.track.setdefault(w[0], {})
            for k, (lw, rd) in tr.items():
                if _conflict(k, w):
                    if lw is not None:
                        deps.add(lw)
                    deps.update(rd)
        for w in writes:
            tr = self.track[w[0]]
            for k in [k for k in tr if _conflict(k, w) and len(k) > len(w)]:
                del tr[k]
            tr[w] = [me, []]
        for r in reads:
            tr = self.track[r[0]]
            if r not in tr:
                lw = None
                for k, (lw2, rd) in tr.items():
                    if _conflict(k, r) and len(k) < len(r) and lw2 is not None:
                        lw = lw2
                tr[r] = [lw, []]
            tr[r][1].append(me)
        deps.discard(me)
        return deps

    def _waits(self, eng, deps, is_dma):
        waits = []
        for dep in sorted(deps):
            kind, e, i = dep
            if kind == 'c' and e == eng and not is_dma and eng == 'pe':
                continue
            key, sem, val = self._sem_of(dep)
            if self.waited[eng].get(key, 0) >= val:
                continue
            self.waited[eng][key] = val
            waits.append((sem, val))
        return waits

    def op(self, eng, fn, reads=(), writes=()):
        me = ('c', eng, self.ncomp[eng])
        deps = self._collect(list(reads), list(writes), me)
        waits = self._waits(eng, deps, False)
        self.ncomp[eng] += 1
        self.ops[eng].append((waits, fn, (self.S[eng], 1)))

    def dma(self, eng, fn, reads=(), writes=()):
        k = self.ndma[eng]
        me = ('d', eng, k)
        deps = self._collect(list(reads), list(writes), me)
        if k >= NRING:
            deps.add(('d', eng, k - NRING))
        waits = self._waits(eng, deps, True)
        self.ndma[eng] += 1
        self.ops[eng].append((waits, fn, (self.Dm[eng][k % NRING], 16)))

    def barrier(self):
        allw = []
        for x in ['pe', 'act', 'dve', 'pool']:
            if self.ncomp[x] > 0:
                allw.append((('S', x), self.S[x], self.ncomp[x]))
        for q in ['sp', 'act', 'pool']:
            for r in range(NRING):
                n = 0 if self.ndma[q] <= r else (self.ndma[q] - r + NRING - 1) // NRING
                if n > 0:
                    allw.append((('D', q, r), self.Dm[q][r], 16 * n))
        for e in ENGS:
            waits = []
            for key, sem, val in allw:
                if self.waited[e].get(key, 0) >= val:
                    continue
                self.waited[e][key] = val
                waits.append((sem, val))
            self.ops[e].append((waits, None, None))

    def emit(self):
        nc = self.nc
        names = {'pe': 'tensor', 'act': 'scalar', 'dve': 'vector', 'pool': 'gpsimd', 'sp': 'sync'}
        with nc.Block() as block:
            for e in ENGS:
                ops = self.ops[e]

                def body(engine, ops=ops):
                    for waits, fn, inc in ops:
                        for sem, val in waits:
                            engine.wait_ge(sem, val)
                        if fn is not None:
                            ins = fn(engine)
                            ins.then_inc(inc[0], inc[1])
                getattr(block, names[e])(body)


def DMA(P, q, out, in_, reads, writes):
    P.dma(q, lambda e: e.dma_start(out=out, in_=in_), reads, writes)


def MM(P, out, lhsT, rhs, start, stop, reads, writes):
    P.op('pe', lambda e: e.matmul(out, lhsT=lhsT, rhs=rhs, start=start, stop=stop), reads, writes)


def TR(P, out, in_, ident, reads, writes):
    P.op('pe', lambda e: e.transpose(out=out, in_=in_, identity=ident), reads, writes)


def ACT(P, out, in_, func, reads, writes, **kw):
    P.op('act', lambda e: e.activation(out=out, in_=in_, func=func, **kw), reads, writes)


def TT(P, eng, out, in0, in1, op, reads, writes):
    P.op(eng, lambda e: e.tensor_tensor(out=out, in0=in0, in1=in1, op=op), reads, writes)


def TS(P, eng, out, in0, s1, s2, op0, op1, reads, writes, **kw):
    if op1 is None:
        P.op(eng, lambda e: e.tensor_scalar(out=out, in0=in0, scalar1=s1, scalar2=None, op0=op0, **kw), reads, writes)
    else:
        P.op(eng, lambda e: e.tensor_scalar(out=out, in0=in0, scalar1=s1, scalar2=s2, op0=op0, op1=op1, **kw),
             reads, writes)


def STT(P, out, in0, scalar, in1, op0, op1, reads, writes, **kw):
    P.op('dve', lambda e: e.scalar_tensor_tensor(out=out, in0=in0, scalar=scalar, in1=in1, op0=op0, op1=op1, **kw),
         reads, writes)


def CP(P, eng, out, in_, reads, writes):
    if eng == 'act':
        P.op('act', lambda e: e.activation(out=out, in_=in_, func=AF.Copy), reads, writes)
    else:
        P.op(eng, lambda e: e.tensor_copy(out=out, in_=in_), reads, writes)


C_QA, C_KA, C_VA, C_QB, C_KB, C_VB, C_F, C_GA, C_GB = 0, 1024, 2048, 3072, 4096, 5120, 6144, 6152, 8200
IN_W = 10248


def build_program():
    nc = bass.Bass("TRN2", target_bir_lowering=False)

    def din(name, shape, dt=F32):
        return nc.dram_tensor(name, shape, dt, kind="ExternalInput").ap()

    xT_full = din("xT_full", [D, 4096])
    xT_own = din("xT_own", [D, 1024])
    x_own = din("x_own", [1024, D])
    w_in = din("w_in", [D, IN_W])
    w_ba = din("w_ba", [1024, D])
    w_bb = din("w_bb", [1024, D])
    w_out = din("w_out", [D, D])
    w_query = din("w_query", [D, D])
    skT = din("skT", [16, 128, 128])
    exp_u = din("exp_u", [16384, D])
    exp_v = din("exp_v", [16384, D])
    gmix_in = din("gmix", [128, 16])
    gffn_in = din("gffn", [128, D])
    gfin_in = din("gfin", [128, D])
    bfor_in = din("bfor", [128, 8])
    cst_in = din("cst", [128, 3 * 128])
    msk_in = din("msk", [128, 4 * 512])
    sel_in = din("sel", [128, 4])
    iota_in = din("iota", [128, 256])
    out = nc.dram_tensor("out", [1024, D], F32, kind="ExternalOutput").ap()
    kt_scr = nc.dram_tensor("kt_scr", [16, 128, 4096], BF16, kind="Internal").ap()
    v_scr = nc.dram_tensor("v_scr", [16, 128, 32, 128], BF16, kind="Internal").ap()
    x1_scr = nc.dram_tensor("x1_scr", [8, 128, D], F32, kind="Internal").ap()
    u_bf = nc.dram_tensor("u_bf", [16384, D], BF16, kind="ExternalOutput" if DEBUG else "Internal").ap()
    v_bf = nc.dram_tensor("v_bf", [16384, D], BF16, kind="ExternalOutput" if DEBUG else "Internal").ap()
    dbg = {}
    if DEBUG:
        dbg['OT'] = nc.dram_tensor("dbg_OT", [128, 16, 1024], BF16, kind="ExternalOutput").ap()
        dbg['x1'] = nc.dram_tensor("dbg_x1", [8, 128, D], F32, kind="ExternalOutput").ap()
        dbg['NF'] = nc.dram_tensor("dbg_NF", [128, 32, 8], F32, kind="ExternalOutput").ap()
        dbg['ids'] = nc.dram_tensor("dbg_ids", [128, 8, 128], F32, kind="ExternalOutput").ap()
        dbg['gw'] = nc.dram_tensor("dbg_gw", [128, 8, 128], F32, kind="ExternalOutput").ap()
        dbg['acc'] = nc.dram_tensor("dbg_acc", [8, 128, D], F32, kind="ExternalOutput").ap()

    with ExitStack() as st:
        P = Prog(nc, st)

        def sb(stack, name, shape, dt):
            return stack.enter_context(nc.sbuf_tensor("s_" + name, shape, dt))

        def ps(stack, name, shape, dt):
            return stack.enter_context(nc.psum_tensor("p_" + name, shape, dt))

        cst = sb(st, "cst", [128, 384], F32)
        cstb = sb(st, "cstb", [128, 384], BF16)
        gmix = sb(st, "gmix", [128, 16], F32)
        bfor = sb(st, "bfor", [128, 8], F32)
        sel = sb(st, "sel", [128, 4], F32)
        msk = sb(st, "msk", [128, 2048], F32)
        NFt = sb(st, "NFt", [128, 32, 8], F32)
        nNFo = sb(st, "nNFo", [128, 8, 8], F32)
        DMA(P, 'sp', cst[:], cst_in[:, :], [], ['cst'])
        DMA(P, 'sp', gmix[:], gmix_in[:, :], [], ['gmix'])
        DMA(P, 'sp', bfor[:], bfor_in[:, :], [], ['bfor'])
        DMA(P, 'sp', sel[:], sel_in[:, :], [], ['sel'])
        DMA(P, 'sp', msk[:], msk_in[:, :], [], ['msk'])
        CP(P, 'dve', cstb[:], cst[:], ['cst'], ['cstb'])
        ident_f, triu_f, ones_f = cst[:, 0:128], cst[:, 128:256], cst[:, 256:384]
        ident_b, ones_b = cstb[:, 0:128], cstb[:, 256:384]
        sb01, sbadd, fxadd = msk[:, 0:512], msk[:, 512:1024], msk[:, 1024:1536]

        def normT(ph, src3, nchunks, dst, dkey, pss):
            xs = sb(ph, "xs_" + dkey, [128, 16, 256], F32)
            sq = sb(ph, "sq_" + dkey, [128, 16, 256], BF16)
            rs = sb(ph, "rs_" + dkey, [128, 256], F32)
            for c in range(nchunks):
                DMA(P, 'sp', xs[:], src3[:, :, c * 256:(c + 1) * 256], [], ['xs'])
                ACT(P, sq[:], xs[:], AF.Square, ['xs'], ['sq'])
                for kc in range(16):
                    MM(P, pss[:, 0:256], ones_b, sq[:, kc, :], kc == 0, kc == 15, ['sq', 'cstb'], ['pss'])
                TS(P, 'dve', rs[:], pss[:, 0:256], 1.0 / D, EPS, ALU.mult, ALU.add, ['pss'], ['rs'])
                ACT(P, rs[:], rs[:], AF.Sqrt, ['rs'], ['rs'])
                P.op('dve', lambda e: e.reciprocal(out=rs[:], in_=rs[:]), ['rs'], ['rs'])
                for kc in range(16):
                    STT(P, dst[:, kc, c * 256:(c + 1) * 256], xs[:, kc, :], gmix[:, kc:kc + 1], rs[:],
                        ALU.mult, ALU.mult, ['xs', 'rs', 'gmix'], [(dkey, c // 2)])

        w_in3 = w_in.rearrange("(kc p) n -> p kc n", p=128)

        with ExitStack() as ph:
            hTf = sb(ph, "hTf", [128, 16, 4096], BF16)
            pss = ps(ph, "pssA", [128, 512], F32)
            with ExitStack() as ph1:
                normT(ph1, xT_full.rearrange("(kc p) t -> p kc t", p=128), 16, hTf, 'hTf', pss)
                P.barrier()
            wg = [sb(ph, "wg%d" % i, [128, 16, 256], BF16) for i in range(2)]
            wf = sb(ph, "wf", [128, 16, 8], BF16)
            kst = [sb(ph, "kst%d" % i, [128, 512], BF16) for i in range(4)]
            vst = [sb(ph, "vst%d" % i, [128, 256], BF16) for i in range(4)]
            pk = [ps(ph, "pk%d" % i, [128, 512], F32) for i in range(4)]
            pf = ps(ph, "pf", [128, 8], F32)
            flg = sb(ph, "flg", [128, 32, 8], F32)
            gi = 0
            nev = 0
            nk = 0
            for (c0, h0) in [(C_KA + 256 * g, 2 * g) for g in range(4)] + [(C_KB + 256 * g, 8 + 2 * g) for g in range(4)]:
                w = wg[gi % 2]
                wk = 'wg%d' % (gi % 2)
                gi += 1
                DMA(P, 'pool', w[:], w_in3[:, :, c0:c0 + 256], [], [wk])
                for hh in range(2):
                    for c in range(8):
                        pt = pk[nev % 4]
                        pkk = 'pk%d' % (nev % 4)
                        for kc in range(16):
                            MM(P, pt[:], w[:, kc, hh * 128:(hh + 1) * 128], hTf[:, kc, c * 512:(c + 1) * 512],
                               kc == 0, kc == 15, [wk, ('hTf', c)], [pkk])
                        ks = kst[nk % 4]
                        kk = 'kst%d' % (nk % 4)
                        nk += 1
                        CP(P, 'act' if nev % 2 == 0 else 'dve', ks[:], pt[:], [pkk], [kk])
                        nev += 1
                        DMA(P, 'sp', kt_scr[h0 + hh][:, c * 512:(c + 1) * 512], ks[:], [kk], [('kt_scr', h0 + hh, c)])
            nv = 0
            for (c0, h0) in [(C_VA + 256 * g, 2 * g) for g in range(4)] + [(C_VB + 256 * g, 8 + 2 * g) for g in range(4)]:
                w = wg[gi % 2]
                wk = 'wg%d' % (gi % 2)
                gi += 1
                DMA(P, 'pool', w[:], w_in3[:, :, c0:c0 + 256], [], [wk])
                for tb in range(32):
                    pt = pk[nev % 4]
                    pkk = 'pk%d' % (nev % 4)
                    for kc in range(16):
                        MM(P, pt[:, 0:256], hTf[:, kc, tb * 128:(tb + 1) * 128], w[:, kc, :], kc == 0, kc == 15,
                           [wk, ('hTf', tb // 4)], [pkk])
                    vs = vst[nv % 4]
                    vk = 'vst%d' % (nv % 4)
                    nv += 1
                    CP(P, 'act' if nev % 2 == 0 else 'dve', vs[:], pt[:, 0:256], [pkk], [vk])
                    nev += 1
                    DMA(P, 'sp', v_scr[h0:h0 + 2, :, tb, :].rearrange("h p d -> p h d"),
                        vs[:].rearrange("p (h d) -> p h d", h=2), [vk], [('v_scr', h0 // 2, tb)])
            DMA(P, 'pool', wf[:], w_in3[:, :, C_F:C_F + 8], [], ['wf'])
            for tb in range(32):
                for kc in range(16):
                    MM(P, pf[:], hTf[:, kc, tb * 128:(tb + 1) * 128], wf[:, kc, :], kc == 0, kc == 15,
                       ['wf', ('hTf', tb // 4)], ['pf'])
                CP(P, 'dve', flg[:, tb, :], pf[:], ['pf'], [('flg', tb)])
            TT(P, 'dve', flg[:], flg[:], bfor[:].unsqueeze(1).to_broadcast([128, 32, 8]), ALU.add,
               ['flg', 'bfor'], ['flg'])
            ACT(P, flg[:], flg[:], AF.Exp, ['flg'], ['flg'], scale=-1.0)
            ACT(P, flg[:], flg[:], AF.Ln, ['flg'], ['flg'], bias=1.0)
            ppre = pk[0]
            ptot = pk[1]
            flat = flg[:].rearrange("p a b -> p (a b)")
            MM(P, ppre[:, 0:256], triu_f, flat, True, True, ['flg', 'cst'], ['pk0'])
            MM(P, ptot[:, 0:256], ones_f, flat, True, True, ['flg', 'cst'], ['pk1'])
            tot = sb(ph, "tot", [128, 32, 8], F32)
            car = sb(ph, "car", [128, 32, 8], F32)
            CP(P, 'dve', tot[:].rearrange("p a b -> p (a b)"), ptot[:, 0:256], ['pk1'], ['tot'])
            P.op('dve', lambda e: e.memset(car[:, 0, :], 0.0), [], ['car'])
            for bk in range(1, 32):
                TT(P, 'dve', car[:, bk, :], car[:, bk - 1, :], tot[:, bk - 1, :], ALU.add, ['car', 'tot'], ['car'])
            TT(P, 'dve', NFt[:].rearrange("p a b -> p (a b)"), ppre[:, 0:256],
               car[:].rearrange("p a b -> p (a b)"), ALU.add, ['pk0', 'car'], ['NFt'])
            NF4 = NFt[:].rearrange("p (i k) h -> p i k h", k=4)
            TS(P, 'dve', nNFo[:], NF4[:, :, 0, :], sel[:, 0:1], None, ALU.mult, None, ['NFt', 'sel'], ['nNFo'])
            for k in range(1, 4):
                STT(P, nNFo[:], NF4[:, :, k, :], sel[:, k:k + 1], nNFo[:], ALU.mult, ALU.add,
                    ['NFt', 'sel', 'nNFo'], ['nNFo'])
            TS(P, 'dve', nNFo[:], nNFo[:], -1.0, None, ALU.mult, None, ['nNFo'], ['nNFo'])
            if DEBUG:
                DMA(P, 'sp', dbg['NF'], NFt[:], ['NFt'], ['dbgNF'])
            P.barrier()

        bc = ExitStack()
        hTo = sb(bc, "hTo", [128, 16, 1024], BF16)
        OT = sb(bc, "OT", [128, 16, 1024], BF16)
        if STAGE >= 2:
            with ExitStack() as ph:
                pss = ps(ph, "pssB", [128, 512], F32)
                with ExitStack() as ph1:
                    normT(ph1, xT_own.rearrange("(kc p) t -> p kc t", p=128), 4, hTo, 'hTo', pss)
                    P.barrier()
                KT = sb(ph, "KT", [128, 4096], BF16)
                VH = sb(ph, "VH", [128, 32, 128], BF16)
                wq = sb(ph, "wq", [128, 16, 128], BF16)
                QH = sb(ph, "QH", [128, 1024], BF16)
                Z = sb(ph, "Z", [128, 4096], F32)
                T1 = sb(ph, "T1", [128, 4096], F32)
                PP = sb(ph, "PP", [128, 4096], F32)
                NFbc = sb(ph, "NFbc", [128, 4096], F32)
                W = sb(ph, "W", [128, 4096], BF16)
                WT = [sb(ph, "WT%d" % i, [128, 512], BF16) for i in range(2)]
                Ob = sb(ph, "Ob", [128, 128], BF16)
                small = sb(ph, "small", [128, 8], F32)
                zp = [ps(ph, "zp%d" % i, [128, 512], F32) for i in range(2)]
                tp = [ps(ph, "tp%d" % i, [128, 512], BF16) for i in range(2)]
                op_ = ps(ph, "op", [128, 128], F32)
                otp = ps(ph, "otp", [128, 128], BF16)
                nz = 0
                ntp = 0
                scale = 1.0 / math.sqrt(128.0)
                for hd in range(16):
                    is_sb = hd < 8
                    qc = C_QA + hd * 128 if is_sb else C_QB + (hd - 8) * 128
                    DMA(P, 'sp', KT[:], kt_scr[hd], [('kt_scr', hd)], ['KT'])
                    DMA(P, 'sp', VH[:], v_scr[hd], [('v_scr', hd // 2)], ['VH'])
                    DMA(P, 'pool', wq[:], w_in3[:, :, qc:qc + 128], [], ['wq'])
                    src_t, dst_t, tk_ = (exp_u, u_bf, 'u_bf') if hd < 8 else (exp_v, v_bf, 'v_bf')
                    r0 = (hd % 8) * 2048
                    DMA(P, 'pool', dst_t[r0:r0 + 2048, :], src_t[r0:r0 + 2048, :], [], [(tk_, hd % 8)])
                    for half in range(2):
                        for kc in range(16):
                            MM(P, pss[:], wq[:, kc, :], hTo[:, kc, half * 512:(half + 1) * 512], kc == 0, kc == 15,
                               ['wq', 'hTo'], ['pss'])
                        ACT(P, QH[:, half * 512:(half + 1) * 512], pss[:], AF.Copy, ['pss'], ['QH'], scale=scale)
                    if not is_sb:
                        h = hd - 8
                        dg = T1[:].rearrange("p (a b) -> p a b", b=128)
                        TT(P, 'dve', dg, ident_f.unsqueeze(1).to_broadcast([128, 32, 128]),
                           NFt[:, :, h:h + 1].to_broadcast([128, 32, 128]), ALU.mult, ['cst', 'NFt'], ['T1'])
                        for c in range(8):
                            z = zp[nz % 2]
                            zk = 'zp%d' % (nz % 2)
                            nz += 1
                            MM(P, z[:], ones_f, T1[:, c * 512:(c + 1) * 512], True, True, ['cst', 'T1'], [zk])
                            CP(P, 'act', NFbc[:, c * 512:(c + 1) * 512], z[:], [zk], ['NFbc'])
                    for i in range(8):
                        nch = i + 1
                        L = 512 * nch
                        dsl = slice(512 * i, 512 * i + 512)
                        for c in range(nch):
                            z = zp[nz % 2]
                            zk = 'zp%d' % (nz % 2)
                            nz += 1
                            MM(P, z[:], QH[:, i * 128:(i + 1) * 128], KT[:, c * 512:(c + 1) * 512], True, True,
                               ['QH', 'KT'], [zk])
                            if is_sb:
                                CP(P, 'act', Z[:, c * 512:(c + 1) * 512], z[:], [zk], [('Z', c)])
                            else:
                                TT(P, 'dve', Z[:, c * 512:(c + 1) * 512], z[:], NFbc[:, c * 512:(c + 1) * 512],
                                   ALU.add, [zk, 'NFbc'], [('Z', c)])
                        if is_sb:
                            ACT(P, T1[:, :L], Z[:, :L], AF.Exp, ['Z'], ['T1'])
                            ACT(P, T1[:, :L], T1[:, :L], AF.Ln, ['T1'], ['T1'], bias=1.0)
                            TT(P, 'pool', T1[:, dsl], T1[:, dsl], sb01, ALU.mult, ['T1', 'msk'], ['T1'])
                            P.op('dve', lambda e, L=L: e.tensor_tensor_scan(
                                out=PP[:, :L], data0=ones_f[:, 0:1].to_broadcast([128, L]), data1=T1[:, :L],
                                initial=0.0, op0=ALU.mult, op1=ALU.add), ['T1', 'cst'], ['PP'])
                            TT(P, 'dve', Z[:, 1:L], Z[:, 1:L], PP[:, 0:L - 1], ALU.add, ['Z', 'PP'], ['Z'])
                            TT(P, 'pool', Z[:, dsl], Z[:, dsl], sbadd, ALU.add, ['Z', 'msk'], ['Z'])
                            TS(P, 'dve', small[:, 0:1], PP[:, L - 1:L], -1.0, None, ALU.mult, None, ['PP'], ['small'])
                            ACT(P, W[:, :L], Z[:, :L], AF.Exp, ['Z', 'small'], ['W'], bias=small[:, 0:1])
                        else:
                            TT(P, 'pool', Z[:, dsl], Z[:, dsl], fxadd, ALU.add, ['Z', 'msk'], ['Z'])
                            ACT(P, W[:, :L], Z[:, :L], AF.Exp, ['Z', 'nNFo'], ['W', 'small'],
                                bias=nNFo[:, i, hd - 8:hd - 7], accum_out=small[:, 1:2])
                        nkb = 4 * nch
                        for g in range(nch):
                            t = tp[ntp % 2]
                            tk = 'tp%d' % (ntp % 2)
                            wt = WT[ntp % 2]
                            wtk = 'WT%d' % (ntp % 2)
                            ntp += 1
                            for q in range(4):
                                kb = 4 * g + q
                                TR(P, t[:, q * 128:(q + 1) * 128], W[:, kb * 128:(kb + 1) * 128], ident_b,
                                   ['W', 'cstb'], [tk])
                            CP(P, 'dve', wt[:], t[:], [tk], [wtk])
                            for q in range(4):
                                kb = 4 * g + q
                                MM(P, op_[:], wt[:, q * 128:(q + 1) * 128], VH[:, kb, :], kb == 0, kb == nkb - 1,
                                   [wtk, 'VH'], ['op'])
                        if is_sb:
                            CP(P, 'act', Ob[:], op_[:], ['op'], ['Ob'])
                        else:
                            P.op('dve', lambda e: e.reciprocal(out=small[:, 2:3], in_=small[:, 1:2]),
                                 ['small'], ['small'])
                            TS(P, 'dve', Ob[:], op_[:], small[:, 2:3], None, ALU.mult, None, ['op', 'small'], ['Ob'])
                        TR(P, otp[:], Ob[:], ident_b, ['Ob', 'cstb'], ['otp'])
                        CP(P, 'act', OT[:, hd, i * 128:(i + 1) * 128], otp[:], ['otp'], [('OT', hd)])
                if DEBUG:
                    DMA(P, 'sp', dbg['OT'], OT[:], ['OT'], ['dbgOT'])
                P.barrier()

        if STAGE >= 3:
            with ExitStack() as ph:
                mT = sb(ph, "mT", [128, 16, 1024], BF16)
                wga = sb(ph, "wga", [128, 16, 512], BF16)
                wgb = sb(ph, "wgb", [128, 16, 512], BF16)
                wa = sb(ph, "wa", [128, 8, 512], BF16)
                wb = sb(ph, "wb", [128, 8, 512], BF16)
                sga = sb(ph, "sga", [128, 512], F32)
                sgb = sb(ph, "sgb", [128, 512], F32)
                m1 = sb(ph, "m1", [128, 512], F32)
                m2 = sb(ph, "m2", [128, 512], F32)
                pc = [ps(ph, "pc%d" % i, [128, 512], F32) for i in range(8)]
                w_ba3 = w_ba.rearrange("(kc p) n -> p kc n", p=128)
                w_bb3 = w_bb.rearrange("(kc p) n -> p kc n", p=128)
                w_out3 = w_out.rearrange("(kc p) n -> p kc n", p=128)
                xo = [sb(ph, "xo%d" % i, [128, D], F32) for i in range(2)]
                it = 0
                for ng in range(4):
                    DMA(P, 'pool', wga[:], w_in3[:, :, C_GA + ng * 512:C_GA + (ng + 1) * 512], [], ['wga'])
                    DMA(P, 'pool', wgb[:], w_in3[:, :, C_GB + ng * 512:C_GB + (ng + 1) * 512], [], ['wgb'])
                    DMA(P, 'pool', wa[:], w_ba3[:, :, ng * 512:(ng + 1) * 512], [], ['wa'])
                    DMA(P, 'pool', wb[:], w_bb3[:, :, ng * 512:(ng + 1) * 512], [], ['wb'])
                    for nt in range(4):
                        ns = slice(nt * 128, (nt + 1) * 128)
                        for half in range(2):
                            hs = slice(half * 512, (half + 1) * 512)
                            b0 = 4 * (it % 2)
                            it += 1
                            pga, pgb, pya, pyb = pc[b0], pc[b0 + 1], pc[b0 + 2], pc[b0 + 3]
                            kga, kgb, kya, kyb = ['pc%d' % (b0 + x) for x in range(4)]
                            for kc in range(16):
                                MM(P, pga[:], wga[:, kc, ns], hTo[:, kc, hs], kc == 0, kc == 15, ['wga', 'hTo'], [kga])
                            for kc in range(16):
                                MM(P, pgb[:], wgb[:, kc, ns], hTo[:, kc, hs], kc == 0, kc == 15, ['wgb', 'hTo'], [kgb])
                            for kc in range(8):
                                MM(P, pya[:], wa[:, kc, ns], OT[:, kc, hs], kc == 0, kc == 7, ['wa', 'OT'], [kya])
                            for kc in range(8):
                                MM(P, pyb[:], wb[:, kc, ns], OT[:, 8 + kc, hs], kc == 0, kc == 7, ['wb', 'OT'], [kyb])
                            ACT(P, sga[:], pga[:], AF.Sigmoid, [kga], ['sga'])
                            ACT(P, sgb[:], pgb[:], AF.Sigmoid, [kgb], ['sgb'])
                            TT(P, 'dve', m1[:], sga[:], pya[:], ALU.mult, ['sga', kya], ['m1'])
                            TT(P, 'dve', m2[:], sgb[:], pyb[:], ALU.mult, ['sgb', kyb], ['m2'])
                            TT(P, 'pool', mT[:, ng * 4 + nt, hs], m1[:], m2[:], ALU.add, ['m1', 'm2'], ['mT'])
                wo = sb(ph, "wo", [128, 16, 512], BF16)
                it = 0
                for ng in range(4):
                    DMA(P, 'pool', wo[:], w_out3[:, :, ng * 512:(ng + 1) * 512], [], ['wo'])
                    for tb in range(8):
                        pt = pc[it % 4]
                        pk_ = 'pc%d' % (it % 4)
                        xb_ = xo[it % 2]
                        xk = 'xo%d' % (it % 2)
                        it += 1
                        cs = slice(ng * 512, (ng + 1) * 512)
                        DMA(P, 'sp', xb_[:, 0:512], x_own[tb * 128:(tb + 1) * 128, cs], [], [xk])
                        for kc in range(16):
                            MM(P, pt[:], mT[:, kc, tb * 128:(tb + 1) * 128], wo[:, kc, :], kc == 0, kc == 15,
                               ['mT', 'wo'], [pk_])
                        TT(P, 'dve', xb_[:, 0:512], xb_[:, 0:512], pt[:], ALU.add, [xk, pk_], [xk])
                        DMA(P, 'sp', x1_scr[tb][:, cs], xb_[:, 0:512], [xk], [('x1s', tb, ng)])
                        if DEBUG:
                            DMA(P, 'sp', dbg['x1'][tb][:, cs], xb_[:, 0:512], [xk], [('dbgx1', tb, ng)])
                P.barrier()

        bc.close()
        if STAGE >= 4:
            with ExitStack() as ph:
                gffn = sb(ph, "gffn", [128, D], F32)
                gfin = sb(ph, "gfin", [128, D], F32)
                skb = sb(ph, "skb", [128, 16, 128], BF16)
                rstd2 = sb(ph, "rstd2", [128, 8], F32)
                qT = sb(ph, "qT", [128, 16, 1024], BF16)
                DMA(P, 'sp', gffn[:], gffn_in[:, :], [], ['gffn'])
                DMA(P, 'sp', gfin[:], gfin_in[:, :], [], ['gfin'])
                DMA(P, 'pool', skb[:], skT.rearrange("ch c n -> c ch n"), [], ['skb'])
                pd = [ps(ph, "pd%d" % i, [128, 512], F32) for i in range(4)]
                junk = sb(ph, "junk", [128, D], F32)
                x1bs = [sb(ph, "x1b%d" % i, [128, D], F32) for i in range(2)]
                x1b = x1bs[0]
                with ExitStack() as ph1:
                    pdb = [ps(ph1, "pdb%d" % i, [128, 512], BF16) for i in range(2)]
                    h2T = sb(ph1, "h2T", [128, 16, 1024], BF16)
                    h2b = sb(ph1, "h2b", [128, D], BF16)
                    wqp = sb(ph1, "wqp", [128, 16, 512], BF16)
                    w_q3 = w_query.rearrange("(kc p) n -> p kc n", p=128)
                    ntp = 0
                    for tb in range(8):
                        DMA(P, 'sp', x1b[:], x1_scr[tb], [('x1s', tb)], ['x1b'])
                        ACT(P, junk[:], x1b[:], AF.Square, ['x1b'], ['junk', ('rstd2', tb)],
                            accum_out=rstd2[:, tb:tb + 1])
                        TS(P, 'dve', rstd2[:, tb:tb + 1], rstd2[:, tb:tb + 1], 1.0 / D, EPS, ALU.mult, ALU.add,
                           [('rstd2', tb)], [('rstd2', tb)])
                        ACT(P, rstd2[:, tb:tb + 1], rstd2[:, tb:tb + 1], AF.Sqrt, [('rstd2', tb)], [('rstd2', tb)])
                        P.op('dve', lambda e, tb=tb: e.reciprocal(out=rstd2[:, tb:tb + 1], in_=rstd2[:, tb:tb + 1]),
                             [('rstd2', tb)], [('rstd2', tb)])
                        STT(P, h2b[:], x1b[:], rstd2[:, tb:tb + 1], gffn[:], ALU.mult, ALU.mult,
                            ['x1b', ('rstd2', tb), 'gffn'], ['h2b'])
                        for g in range(4):
                            t = pdb[ntp % 2]
                            tk = 'pdb%d' % (ntp % 2)
                            ntp += 1
                            for q in range(4):
                                kc = 4 * g + q
                                TR(P, t[:, q * 128:(q + 1) * 128], h2b[:, kc * 128:(kc + 1) * 128], ident_b,
                                   ['h2b', 'cstb'], [tk])
                            CP(P, 'act', h2T[:, 4 * g:4 * g + 4, tb * 128:(tb + 1) * 128],
                               t[:].rearrange("p (a b) -> p a b", a=4), [tk], ['h2T'])
                    it = 0
                    for ng in range(4):
                        DMA(P, 'pool', wqp[:], w_q3[:, :, ng * 512:(ng + 1) * 512], [], ['wqp'])
                        for nt in range(4):
                            for half in range(2):
                                pt = pd[it % 4]
                                pk_ = 'pd%d' % (it % 4)
                                it += 1
                                for kc in range(16):
                                    MM(P, pt[:], wqp[:, kc, nt * 128:(nt + 1) * 128],
                                       h2T[:, kc, half * 512:(half + 1) * 512], kc == 0, kc == 15, ['wqp', 'h2T'], [pk_])
                                CP(P, 'act' if it % 2 else 'dve', qT[:, ng * 4 + nt, half * 512:(half + 1) * 512], pt[:],
                                   [pk_], ['qT'])
                    P.barrier()
                po = [ps(ph, "po%d" % i, [128, 512], F32) for i in range(4)]
                sc = sb(ph, "sc", [128, 16, 128], F32)
                tmp = sb(ph, "tmp", [128, 256], F32)
                v16 = sb(ph, "v16", [128, 8, 2, 16], F32)
                ix = sb(ph, "ix", [128, 8, 2, 16], U32)
                ixf = sb(ph, "ixf", [128, 8, 2, 16], F32)
                cand = sb(ph, "cand", [128, 8, 16, 16], F32)
                cid = sb(ph, "cid", [128, 8, 16, 16], F32)
                t16 = sb(ph, "t16", [128, 8, 16], F32)
                pos = sb(ph, "pos", [128, 8, 16], U32)
                posf = sb(ph, "posf", [128, 8, 16], F32)
                iot = sb(ph, "iot", [128, 256], F32)
                DMA(P, 'sp', iot[:], iota_in[:, :], [], ['iot'])
                idf = sb(ph, "idf", [128, 128], F32)
                idus = [sb(ph, "idu%d" % i, [128, 128], U32) for i in range(2)]
                gts = [sb(ph, "gt%d" % i, [128, 8, 16], F32) for i in range(2)]
                gs = sb(ph, "gs", [128, 8], F32)
                aa = sb(ph, "aa", [128, 128], F32)
                ww = sb(ph, "ww", [128, 128], F32)
                g1 = sb(ph, "g1", [128, 128], F32)
                h2f = sb(ph, "h2f", [128, D], F32)
                acc = sb(ph, "acc", [128, D], F32)
                ssf = sb(ph, "ssf", [128, 2], F32)
                dg = sb(ph, "dg", [128, 128, 128], BF16)
                jb = sb(ph, "jb", [128, D], BF16)
                NB = 8
                gb = [sb(ph, "gb%d" % i, [128, D], BF16) for i in range(NB)]
                ngb = [0]

                def topk(tb):
                    idu = idus[tb % 2]
                    ik = 'idu%d' % (tb % 2)
                    gt = gts[tb % 2]
                    gk = 'gt%d' % (tb % 2)
                    for g in range(4):
                        for q in range(4):
                            ch = 4 * g + q
                            MM(P, pd[g][:, q * 128:(q + 1) * 128], qT[:, ch, tb * 128:(tb + 1) * 128], skb[:, ch, :],
                               True, True, ['qT', 'skb'], ['pd%d' % g])
                        CP(P, 'act', sc[:, 4 * g:4 * g + 4, :], pd[g][:].rearrange("p (a b) -> p a b", a=4),
                           ['pd%d' % g], ['sc'])
                    for ch in range(16):
                        hh, pp = ch // 2, ch % 2
                        P.op('dve', lambda e, ch=ch, hh=hh, pp=pp: e.max(out=v16[:, hh, pp, 0:8], in_=sc[:, ch, :]),
                             ['sc'], ['v16'])
                        P.op('dve', lambda e, ch=ch, hh=hh, pp=pp: e.max_index(
                            out=ix[:, hh, pp, 0:8], in_max=v16[:, hh, pp, 0:8], in_values=sc[:, ch, :]),
                            ['sc', 'v16'], ['ix'])
                        P.op('dve', lambda e, ch=ch, hh=hh, pp=pp: e.match_replace(
                            out=tmp[:, 0:128], in_to_replace=v16[:, hh, pp, 0:8], in_values=sc[:, ch, :],
                            imm_value=-1e30), ['sc', 'v16'], ['tmp'])
                        P.op('dve', lambda e, ch=ch, hh=hh, pp=pp: e.max(out=v16[:, hh, pp, 8:16], in_=tmp[:, 0:128]),
                             ['tmp'], ['v16'])
                        P.op('dve', lambda e, ch=ch, hh=hh, pp=pp: e.max_index(
                            out=ix[:, hh, pp, 8:16], in_max=v16[:, hh, pp, 8:16], in_values=tmp[:, 0:128]),
                            ['tmp', 'v16'], ['ix'])
                    CP(P, 'dve', ixf[:], ix[:], ['ix'], ['ixf'])
                    TS(P, 'dve', ixf[:, :, 0, :], ixf[:, :, 0, :], 128.0, None, ALU.mult, None, ['ixf'], ['ixf'])
                    TT(P, 'dve', cand[:], v16[:, :, 0, :].unsqueeze(3).to_broadcast([128, 8, 16, 16]),
                       v16[:, :, 1, :].unsqueeze(2).to_broadcast([128, 8, 16, 16]), ALU.add, ['v16'], ['cand'])
                    TT(P, 'dve', cid[:], ixf[:, :, 0, :].unsqueeze(3).to_broadcast([128, 8, 16, 16]),
                       ixf[:, :, 1, :].unsqueeze(2).to_broadcast([128, 8, 16, 16]), ALU.add, ['ixf'], ['cid'])
                    for hh in range(8):
                        cf = cand[:, hh].rearrange("p a b -> p (a b)")
                        P.op('dve', lambda e, hh=hh, cf=cf: e.max(out=t16[:, hh, 0:8], in_=cf), ['cand'], ['t16'])
                        P.op('dve', lambda e, hh=hh, cf=cf: e.max_index(
                            out=pos[:, hh, 0:8], in_max=t16[:, hh, 0:8], in_values=cf), ['cand', 't16'], ['pos'])
                        P.op('dve', lambda e, hh=hh, cf=cf: e.match_replace(
                            out=tmp[:], in_to_replace=t16[:, hh, 0:8], in_values=cf, imm_value=-1e30),
                            ['cand', 't16'], ['tmp'])
                        P.op('dve', lambda e, hh=hh: e.max(out=t16[:, hh, 8:16], in_=tmp[:]), ['tmp'], ['t16'])
                        P.op('dve', lambda e, hh=hh: e.max_index(
                            out=pos[:, hh, 8:16], in_max=t16[:, hh, 8:16], in_values=tmp[:]), ['tmp', 't16'], ['pos'])
                    CP(P, 'dve', posf[:], pos[:], ['pos'], ['posf'])
                    for hh in range(8):
                        cidf = cid[:, hh].rearrange("p a b -> p (a b)")
                        for k in range(16):
                            STT(P, tmp[:], iot[:], posf[:, hh, k:k + 1], cidf, ALU.is_equal, ALU.mult,
                                ['iot', 'cid', 'posf'], ['tmp', 'idf'], accum_out=idf[:, hh * 16 + k:hh * 16 + k + 1])
                    CP(P, 'dve', idu[:], idf[:], ['idf'], [ik])
                    if DEBUG:
                        DMA(P, 'sp', dbg['ids'][:, tb, :], idf[:], ['idf'], [('dbgids', tb)])
                    TT(P, 'dve', gt[:], t16[:], t16[:, :, 0:1].to_broadcast([128, 8, 16]), ALU.subtract, ['t16'], [gk])
                    ACT(P, gt[:], gt[:], AF.Exp, [gk], [gk])
                    P.op('dve', lambda e, gt=gt: e.reduce_sum(out=gs[:], in_=gt[:], axis=AX.X), [gk], ['gs'])
                    P.op('dve', lambda e: e.reciprocal(out=gs[:], in_=gs[:]), ['gs'], ['gs'])
                    TT(P, 'dve', gt[:], gt[:], gs[:].unsqueeze(2).to_broadcast([128, 8, 16]), ALU.mult, [gk, 'gs'], [gk])

                def gather(table, tkey, idu, ik, hk):
                    bfr = gb[ngb[0] % NB]
                    bk = 'gb%d' % (ngb[0] % NB)
                    ngb[0] += 1
                    P.dma('pool', lambda e, bfr=bfr, hk=hk: e.indirect_dma_start(
                        out=bfr[:, :], out_offset=None, in_=table[:, :],
                        in_offset=bass.IndirectOffsetOnAxis(ap=idu[:, hk:hk + 1], axis=0)), [ik, tkey], [bk])
                    return bfr, bk

                topk(0)
                for tb in range(8):
                    idu = idus[tb % 2]
                    ik = 'idu%d' % (tb % 2)
                    gt = gts[tb % 2]
                    gk = 'gt%d' % (tb % 2)
                    x1b = x1bs[tb % 2]
                    xk = 'x1b%d' % (tb % 2)
                    DMA(P, 'sp', x1b[:], x1_scr[tb], [('x1s', tb)], [xk])
                    STT(P, h2f[:], x1b[:], rstd2[:, tb:tb + 1], gffn[:], ALU.mult, ALU.mult,
                        [xk, 'rstd2', 'gffn'], ['h2f'])
                    for hk in range(128):
                        bfr, bk = gather(u_bf, 'u_bf', idu, ik, hk)
                        STT(P, jb[:], bfr[:], 1.0, h2f[:], ALU.mult, ALU.mult, [bk, 'h2f'], ['jb', 'aa'],
                            accum_out=aa[:, hk:hk + 1])
                    TT(P, 'dve', g1[:], aa[:], aa[:], ALU.mult, ['aa'], ['g1'])
                    TS(P, 'dve', g1[:], g1[:], 0.044715, 1.0, ALU.mult, ALU.add, ['g1'], ['g1'])
                    TT(P, 'dve', g1[:], g1[:], aa[:], ALU.mult, ['g1', 'aa'], ['g1'])
                    ACT(P, g1[:], g1[:], AF.Sigmoid, ['g1'], ['g1'], scale=2.0 * math.sqrt(2.0 / math.pi))
                    TT(P, 'dve', g1[:], g1[:], aa[:], ALU.mult, ['g1', 'aa'], ['g1'])
                    TT(P, 'dve', ww[:], g1[:], gt[:].rearrange("p a b -> p (a b)"), ALU.mult, ['g1', gk], ['ww'])
                    if DEBUG:
                        DMA(P, 'sp', dbg['gw'][:, tb, :], ww[:], ['ww'], [('dbggw', tb)])
                    for q4 in range(4):
                        hs = slice(32 * q4, 32 * q4 + 32)
                        TT(P, 'dve', dg[:, hs, :], ident_b.unsqueeze(1).to_broadcast([128, 32, 128]),
                           ww[:, hs].unsqueeze(2).to_broadcast([128, 32, 128]), ALU.mult, ['cstb', 'ww'], [('dg', q4)])
                    if tb + 1 < 8:
                        topk(tb + 1)
                    for hk in range(128):
                        bfr, bk = gather(v_bf, 'v_bf', idu, ik, hk)
                        for c in range(4):
                            MM(P, po[c][:], dg[:, hk, :], bfr[:, c * 512:(c + 1) * 512], hk == 0, hk == 127,
                               [('dg', hk // 32), bk], ['po%d' % c])
                    for c in range(4):
                        TT(P, 'dve', acc[:, c * 512:(c + 1) * 512], x1b[:, c * 512:(c + 1) * 512], po[c][:], ALU.add,
                           [xk, 'po%d' % c], [('acc', c)])
                    if DEBUG:
                        DMA(P, 'sp', dbg['acc'][tb], acc[:], ['acc'], [('dbgacc', tb)])
                    ACT(P, junk[:], acc[:], AF.Square, ['acc'], ['junk', 'ssf'], accum_out=ssf[:, 0:1])
                    TS(P, 'dve', ssf[:, 0:1], ssf[:, 0:1], 1.0 / D, EPS, ALU.mult, ALU.add, ['ssf'], ['ssf'])
                    ACT(P, ssf[:, 0:1], ssf[:, 0:1], AF.Sqrt, ['ssf'], ['ssf'])
                    P.op('dve', lambda e: e.reciprocal(out=ssf[:, 0:1], in_=ssf[:, 0:1]), ['ssf'], ['ssf'])
                    STT(P, junk[:], acc[:], ssf[:, 0:1], gfin[:], ALU.mult, ALU.mult, ['acc', 'ssf', 'gfin'], ['junk'])
                    DMA(P, 'sp', out[tb * 128:(tb + 1) * 128, :], junk[:], ['junk'], [('out', tb)])
                P.barrier()
        P.barrier()
        P.emit()
    return nc


def make_core_inputs(inputs):
    x = np.asarray(inputs["x"], dtype=np.float32)
    w_in = np.ascontiguousarray(np.asarray(inputs["w_in"], dtype=np.float32)[0])
    w_ba = np.ascontiguousarray(np.asarray(inputs["w_branch_a"], dtype=np.float32)[0])
    w_bb = np.ascontiguousarray(np.asarray(inputs["w_branch_b"], dtype=np.float32)[0])
    w_out = np.ascontiguousarray(np.asarray(inputs["w_out"], dtype=np.float32)[0])
    w_query = np.ascontiguousarray(np.asarray(inputs["w_query"], dtype=np.float32)[0])
    sk = np.asarray(inputs["sub_keys"], dtype=np.float32)[0]
    skT = np.ascontiguousarray(sk.transpose(0, 1, 3, 2).reshape(16, 128, 128))
    exp_u = np.ascontiguousarray(np.asarray(inputs["expert_u"], dtype=np.float32)[0])
    exp_v = np.ascontiguousarray(np.asarray(inputs["expert_v"], dtype=np.float32)[0])
    gmix = np.ascontiguousarray(np.asarray(inputs["norm_mix_gain"], dtype=np.float32)[0].reshape(16, 128).T)
    gffn = np.ascontiguousarray(np.broadcast_to(np.asarray(inputs["norm_ffn_gain"], dtype=np.float32)[0][None, :], (128, D)))
    gfin = np.ascontiguousarray(np.broadcast_to(np.asarray(inputs["norm_final_gain"], dtype=np.float32)[None, :], (128, D)))
    bfor = np.ascontiguousarray(np.broadcast_to(np.asarray(inputs["b_forget"], dtype=np.float32)[0][None, :], (128, 8)))
    idx = np.arange(128)
    ident = np.eye(128, dtype=np.float32)
    triu = (idx[:, None] <= idx[None, :]).astype(np.float32)
    cst = np.ascontiguousarray(np.concatenate([ident, triu, np.ones((128, 128), np.float32)], axis=1))
    xT = [np.ascontiguousarray(x[b].T) for b in range(2)]
    iota = np.ascontiguousarray(np.broadcast_to(np.arange(256, dtype=np.float32)[None, :], (128, 256)))
    maps = []
    toks = []
    for c in range(8):
        b, j = c // 4, c % 4
        tok = np.concatenate([np.arange((4 * i + j) * 128, (4 * i + j + 1) * 128) for i in range(8)])
        toks.append((b, tok))
        sb01 = np.zeros((128, 4, 128), np.float32)
        fx01 = np.zeros((128, 4, 128), np.float32)
        for k in range(4):
            if k < j:
                sb01[:, k, :] = 1.0
                fx01[:, k, :] = 1.0
            elif k == j:
                sb01[:, k, :] = (idx[None, :] < idx[:, None])
                fx01[:, k, :] = (idx[None, :] <= idx[:, None])
        sbadd = (1.0 - sb01) * NEG
        fxadd = (1.0 - fx01) * NEG
        msk = np.ascontiguousarray(np.concatenate(
            [sb01.reshape(128, 512), sbadd.reshape(128, 512), fxadd.reshape(128, 512), np.zeros((128, 512), np.float32)],
            axis=1).astype(np.float32))
        sel = np.zeros((128, 4), np.float32)
        sel[:, j] = 1.0
        maps.append({
            "xT_full": xT[b], "xT_own": np.ascontiguousarray(xT[b][:, tok]), "x_own": np.ascontiguousarray(x[b][tok]),
            "w_in": w_in, "w_ba": w_ba, "w_bb": w_bb, "w_out": w_out, "w_query": w_query, "skT": skT,
            "exp_u": exp_u, "exp_v": exp_v, "gmix": gmix, "gffn": gffn, "gfin": gfin, "bfor": bfor,
            "cst": cst, "msk": msk, "sel": sel, "iota": iota,
        })
    return maps, toks


def kernel(**inputs):
    maps, toks = make_core_inputs(inputs)
    nc = build_program()
    res = run_bass_kernel_spmd(nc, maps, core_ids=list(range(8)))
    outp = np.zeros((2, 4096, D), np.float32)
    for c in range(8):
        b, tok = toks[c]
        outp[b, tok] = np.asarray(res.results[c]["out"], dtype=np.float32)
    return outp
```

```python
import math
from contextlib import ExitStack

import numpy as np
import concourse.bass as bass
import concourse.mybir as mybir
from concourse.bass_utils import run_bass_kernel_spmd

F32 = mybir.dt.float32
BF16 = mybir.dt.bfloat16
U32 = mybir.dt.uint32
AF = mybir.ActivationFunctionType
ALU = mybir.AluOpType
AX = mybir.AxisListType

ENGS = ['pe', 'act', 'dve', 'pool', 'sp']
NRING = 6
EPS = 1e-6
D = 2048
NEG = -30000.0
STAGE = 99
DEBUG = False


def _conflict(a, b):
    n = min(len(a), len(b))
    return a[:n] == b[:n]


class Prog:
    def __init__(self, nc, stack):
        self.nc = nc
        self.ops = {e: [] for e in ENGS}
        self.ncomp = {e: 0 for e in ENGS}
        self.ndma = {e: 0 for e in ENGS}
        self.waited = {e: {} for e in ENGS}
        self.track = {}
        self.S = {e: stack.enter_context(nc.semaphore('S_' + e)) for e in ['pe', 'act', 'dve', 'pool']}
        self.Dm = {e: [stack.enter_context(nc.semaphore('D_%s%d' % (e, i))) for i in range(NRING)]
                   for e in ['sp', 'act', 'pool']}

    def _sem_of(self, dep):
        kind, e, i = dep
        if kind == 'c':
            return ('S', e), self.S[e], i + 1
        return ('D', e, i % NRING), self.Dm[e][i % NRING], 16 * (i // NRING + 1)

    def _collect(self, reads, writes, me):
        deps = set()
        reads = [r if isinstance(r, tuple) else (r,) for r in reads]
        writes = [w if isinstance(w, tuple) else (w,) for w in writes]
        for r in reads:
            tr = self.track.setdefault(r[0], {})
            for k, (lw, rd) in tr.items():
                if _conflict(k, r) and lw is not None:
                    deps.add(lw)
        for w in writes:
            tr = self.track.setdefault(w[0], {})
            for k, (lw, rd) in tr.items():
                if _conflict(k, w):
                    if lw is not None:
                        deps.add(lw)
                    deps.update(rd)
        for w in writes:
            tr = self.track[w[0]]
            for k in [k for k in tr if _conflict(k, w) and len(k) > len(w)]:
                del tr[k]
            tr[w] = [me, []]
        for r in reads:
            tr = self.track[r[0]]
            if r not in tr:
                lw = None
                for k, (lw2, rd) in tr.items():
                    if _conflict(k, r) and len(k) < len(r) and lw2 is not None:
                        lw = lw2
                tr[r] = [lw, []]
            tr[r][1].append(me)
        deps.discard(me)
        return deps

    def _waits(self, eng, deps, is_dma):
        waits = []
        for dep in sorted(deps):
            kind, e, i = dep
            if kind == 'c' and e == eng and not is_dma and eng == 'pe':
                continue
            key, sem, val = self._sem_of(dep)
            if self.waited[eng].get(key, 0) >= val:
                continue
            self.waited[eng][key] = val
            waits.append((sem, val))
        return waits

    def op(self, eng, fn, reads=(), writes=()):
        me = ('c', eng, self.ncomp[eng])
        deps = self._collect(list(reads), list(writes), me)
        waits = self._waits(eng, deps, False)
        self.ncomp[eng] += 1
        self.ops[eng].append((waits, fn, (self.S[eng], 1)))

    def dma(self, eng, fn, reads=(), writes=()):
        k = self.ndma[eng]
        me = ('d', eng, k)
        deps = self._collect(list(reads), list(writes), me)
        if k >= NRING:
            deps.add(('d', eng, k - NRING))
        waits = self._waits(eng, deps, True)
        self.ndma[eng] += 1
        self.ops[eng].append((waits, fn, (self.Dm[eng][k % NRING], 16)))

    def barrier(self):
        allw = []
        for x in ['pe', 'act', 'dve', 'pool']:
            if self.ncomp[x] > 0:
                allw.append((('S', x), self.S[x], self.ncomp[x]))
        for q in ['sp', 'act', 'pool']:
            for r in range(NRING):
                n = 0 if self.ndma[q] <= r else (self.ndma[q] - r + NRING - 1) // NRING
                if n > 0:
                    allw.append((('D', q, r), self.Dm[q][r], 16 * n))
        for e in ENGS:
            waits = []
            for key, sem, val in allw:
                if self.waited[e].get(key, 0) >= val:
                    continue
                self.waited[e][key] = val
                waits.append((sem, val))
            self.ops[e].append((waits, None, None))

    def emit(self):
        nc = self.nc
        names = {'pe': 'tensor', 'act': 'scalar', 'dve': 'vector', 'pool': 'gpsimd', 'sp': 'sync'}
        with nc.Block() as block:
            for e in ENGS:
                ops = self.ops[e]

                def body(engine, ops=ops):
                    for waits, fn, inc in ops:
                        for sem, val in waits:
                            engine.wait_ge(sem, val)
                        if fn is not None:
                            ins = fn(engine)
                            ins.then_inc(inc[0], inc[1])
                getattr(block, names[e])(body)


def DMA(P, q, out, in_, reads, writes):
    P.dma(q, lambda e: e.dma_start(out=out, in_=in_), reads, writes)


def MM(P, out, lhsT, rhs, start, stop, reads, writes):
    P.op('pe', lambda e: e.matmul(out, lhsT=lhsT, rhs=rhs, start=start, stop=stop), reads, writes)


def TR(P, out, in_, ident, reads, writes):
    P.op('pe', lambda e: e.transpose(out=out, in_=in_, identity=ident), reads, writes)


def ACT(P, out, in_, func, reads, writes, **kw):
    P.op('act', lambda e: e.activation(out=out, in_=in_, func=func, **kw), reads, writes)


def TT(P, eng, out, in0, in1, op, reads, writes):
    P.op(eng, lambda e: e.tensor_tensor(out=out, in0=in0, in1=in1, op=op), reads, writes)


def TS(P, eng, out, in0, s1, s2, op0, op1, reads, writes, **kw):
    if op1 is None:
        P.op(eng, lambda e: e.tensor_scalar(out=out, in0=in0, scalar1=s1, scalar2=None, op0=op0, **kw), reads, writes)
    else:
        P.op(eng, lambda e: e.tensor_scalar(out=out, in0=in0, scalar1=s1, scalar2=s2, op0=op0, op1=op1, **kw),
             reads, writes)


def STT(P, out, in0, scalar, in1, op0, op1, reads, writes, **kw):
    P.op('dve', lambda e: e.scalar_tensor_tensor(out=out, in0=in0, scalar=scalar, in1=in1, op0=op0, op1=op1, **kw),
         reads, writes)


def CP(P, eng, out, in_, reads, writes):
    if eng == 'act':
        P.op('act', lambda e: e.activation(out=out, in_=in_, func=AF.Copy), reads, writes)
    else:
        P.op(eng, lambda e: e.tensor_copy(out=out, in_=in_), reads, writes)


C_QA, C_KA, C_VA, C_QB, C_KB, C_VB, C_F, C_GA, C_GB = 0, 1024, 2048, 3072, 4096, 5120, 6144, 6152, 8200
IN_W = 10248


def build_program():
    nc = bass.Bass("TRN2", target_bir_lowering=False)

    def din(name, shape, dt=F32):
        return nc.dram_tensor(name, shape, dt, kind="ExternalInput").ap()

    xT_full = din("xT_full", [D, 4096])
    xT_own = din("xT_own", [D, 1024])
    x_own = din("x_own", [1024, D])
    w_in = din("w_in", [D, IN_W])
    w_ba = din("w_ba", [1024, D])
    w_bb = din("w_bb", [1024, D])
    w_out = din("w_out", [D, D])
    w_query = din("w_query", [D, D])
    skT = din("skT", [16, 128, 128])
    exp_u = din("exp_u", [16384, D])
    exp_v = din("exp_v", [16384, D])
    gmix_in = din("gmix", [128, 16])
    gffn_in = din("gffn", [128, D])
    gfin_in = din("gfin", [128, D])
    bfor_in = din("bfor", [128, 8])
    cst_in = din("cst", [128, 3 * 128])
    msk_in = din("msk", [128, 4 * 512])
    sel_in = din("sel", [128, 4])
    iota_in = din("iota", [128, 256])
    out = nc.dram_tensor("out", [1024, D], F32, kind="ExternalOutput").ap()
    kt_scr = nc.dram_tensor("kt_scr", [16, 128, 4096], BF16, kind="Internal").ap()
    v_scr = nc.dram_tensor("v_scr", [16, 128, 32, 128], BF16, kind="Internal").ap()
    x1_scr = nc.dram_tensor("x1_scr", [8, 128, D], F32, kind="Internal").ap()
    q_scr = nc.dram_tensor("q_scr", [128, 16, 1024], BF16, kind="Internal").ap()
    uv_bf = nc.dram_tensor("uv_bf", [16384, 2 * D], BF16, kind="Internal").ap()
    dbg = {}
    if DEBUG:
        dbg['OT'] = nc.dram_tensor("dbg_OT", [128, 16, 1024], BF16, kind="ExternalOutput").ap()
        dbg['x1'] = nc.dram_tensor("dbg_x1", [8, 128, D], F32, kind="ExternalOutput").ap()
        dbg['NF'] = nc.dram_tensor("dbg_NF", [128, 32, 8], F32, kind="ExternalOutput").ap()
        dbg['ids'] = nc.dram_tensor("dbg_ids", [128, 8, 128], F32, kind="ExternalOutput").ap()
        dbg['gw'] = nc.dram_tensor("dbg_gw", [128, 8, 128], F32, kind="ExternalOutput").ap()
        dbg['acc'] = nc.dram_tensor("dbg_acc", [8, 128, D], F32, kind="ExternalOutput").ap()

    with ExitStack() as st:
        P = Prog(nc, st)

        def sb(stack, name, shape, dt):
            return stack.enter_context(nc.sbuf_tensor("s_" + name, shape, dt))

        def ps(stack, name, shape, dt):
            return stack.enter_context(nc.psum_tensor("p_" + name, shape, dt))

        cst = sb(st, "cst", [128, 384], F32)
        cstb = sb(st, "cstb", [128, 384], BF16)
        gmix = sb(st, "gmix", [128, 16], F32)
        bfor = sb(st, "bfor", [128, 8], F32)
        sel = sb(st, "sel", [128, 4], F32)
        msk = sb(st, "msk", [128, 2048], F32)
        NFt = sb(st, "NFt", [128, 32, 8], F32)
        nNFo = sb(st, "nNFo", [128, 8, 8], F32)
        DMA(P, 'sp', cst[:], cst_in[:, :], [], ['cst'])
        DMA(P, 'sp', gmix[:], gmix_in[:, :], [], ['gmix'])
        DMA(P, 'sp', bfor[:], bfor_in[:, :], [], ['bfor'])
        DMA(P, 'sp', sel[:], sel_in[:, :], [], ['sel'])
        DMA(P, 'sp', msk[:], msk_in[:, :], [], ['msk'])
        CP(P, 'dve', cstb[:], cst[:], ['cst'], ['cstb'])
        ident_f, triu_f, ones_f = cst[:, 0:128], cst[:, 128:256], cst[:, 256:384]
        ident_b, ones_b = cstb[:, 0:128], cstb[:, 256:384]
        sb01, sbadd, fxadd = msk[:, 0:512], msk[:, 512:1024], msk[:, 1024:1536]

        def normT(ph, src3, nchunks, dst, dkey, pss):
            xs = sb(ph, "xs_" + dkey, [128, 16, 256], F32)
            sq = sb(ph, "sq_" + dkey, [128, 16, 256], BF16)
            rs = sb(ph, "rs_" + dkey, [128, 256], F32)
            for c in range(nchunks):
                DMA(P, 'sp', xs[:], src3[:, :, c * 256:(c + 1) * 256], [], ['xs'])
                ACT(P, sq[:], xs[:], AF.Square, ['xs'], ['sq'])
                for kc in range(16):
                    MM(P, pss[:, 0:256], ones_b, sq[:, kc, :], kc == 0, kc == 15, ['sq', 'cstb'], ['pss'])
                TS(P, 'dve', rs[:], pss[:, 0:256], 1.0 / D, EPS, ALU.mult, ALU.add, ['pss'], ['rs'])
                ACT(P, rs[:], rs[:], AF.Sqrt, ['rs'], ['rs'])
                P.op('dve', lambda e: e.reciprocal(out=rs[:], in_=rs[:]), ['rs'], ['rs'])
                for kc in range(16):
                    STT(P, dst[:, kc, c * 256:(c + 1) * 256], xs[:, kc, :], gmix[:, kc:kc + 1], rs[:],
                        ALU.mult, ALU.mult, ['xs', 'rs', 'gmix'], [(dkey, c // 2)])

        w_in3 = w_in.rearrange("(kc p) n -> p kc n", p=128)

        with ExitStack() as ph:
            hTf = sb(ph, "hTf", [128, 16, 4096], BF16)
            pss = ps(ph, "pssA", [128, 512], F32)
            with ExitStack() as ph1:
                normT(ph1, xT_full.rearrange("(kc p) t -> p kc t", p=128), 16, hTf, 'hTf', pss)
                P.barrier()
            wg = [sb(ph, "wg%d" % i, [128, 16, 256], BF16) for i in range(2)]
            wf = sb(ph, "wf", [128, 16, 8], BF16)
            kst = [sb(ph, "kst%d" % i, [128, 512], BF16) for i in range(4)]
            vst = [sb(ph, "vst%d" % i, [128, 256], BF16) for i in range(4)]
            pk = [ps(ph, "pk%d" % i, [128, 512], F32) for i in range(4)]
            pf = ps(ph, "pf", [128, 8], F32)
            flg = sb(ph, "flg", [128, 32, 8], F32)
            gi = 0
            nev = 0
            nk = 0
            for (c0, h0) in [(C_KA + 256 * g, 2 * g) for g in range(4)] + [(C_KB + 256 * g, 8 + 2 * g) for g in range(4)]:
                w = wg[gi % 2]
                wk = 'wg%d' % (gi % 2)
                gi += 1
                DMA(P, 'pool', w[:], w_in3[:, :, c0:c0 + 256], [], [wk])
                for hh in range(2):
                    for c in range(8):
                        pt = pk[nev % 4]
                        pkk = 'pk%d' % (nev % 4)
                        for kc in range(16):
                            MM(P, pt[:], w[:, kc, hh * 128:(hh + 1) * 128], hTf[:, kc, c * 512:(c + 1) * 512],
                               kc == 0, kc == 15, [wk, ('hTf', c)], [pkk])
                        ks = kst[nk % 4]
                        kk = 'kst%d' % (nk % 4)
                        nk += 1
                        CP(P, 'act' if nev % 2 == 0 else 'dve', ks[:], pt[:], [pkk], [kk])
                        nev += 1
                        DMA(P, 'sp', kt_scr[h0 + hh][:, c * 512:(c + 1) * 512], ks[:], [kk], [('kt_scr', h0 + hh, c)])
            nv = 0
            for (c0, h0) in [(C_VA + 256 * g, 2 * g) for g in range(4)] + [(C_VB + 256 * g, 8 + 2 * g) for g in range(4)]:
                w = wg[gi % 2]
                wk = 'wg%d' % (gi % 2)
                gi += 1
                DMA(P, 'pool', w[:], w_in3[:, :, c0:c0 + 256], [], [wk])
                for tb in range(32):
                    pt = pk[nev % 4]
                    pkk = 'pk%d' % (nev % 4)
                    for kc in range(16):
                        MM(P, pt[:, 0:256], hTf[:, kc, tb * 128:(tb + 1) * 128], w[:, kc, :], kc == 0, kc == 15,
                           [wk, ('hTf', tb // 4)], [pkk])
                    vs = vst[nv % 4]
                    vk = 'vst%d' % (nv % 4)
                    nv += 1
                    CP(P, 'act' if nev % 2 == 0 else 'dve', vs[:], pt[:, 0:256], [pkk], [vk])
                    nev += 1
                    DMA(P, 'sp', v_scr[h0:h0 + 2, :, tb, :].rearrange("h p d -> p h d"),
                        vs[:].rearrange("p (h d) -> p h d", h=2), [vk], [('v_scr', h0 // 2, tb)])
            DMA(P, 'pool', wf[:], w_in3[:, :, C_F:C_F + 8], [], ['wf'])
            for tb in range(32):
                for kc in range(16):
                    MM(P, pf[:], hTf[:, kc, tb * 128:(tb + 1) * 128], wf[:, kc, :], kc == 0, kc == 15,
                       ['wf', ('hTf', tb // 4)], ['pf'])
                CP(P, 'dve', flg[:, tb, :], pf[:], ['pf'], [('flg', tb)])
            TT(P, 'dve', flg[:], flg[:], bfor[:].unsqueeze(1).to_broadcast([128, 32, 8]), ALU.add,
               ['flg', 'bfor'], ['flg'])
            ACT(P, flg[:], flg[:], AF.Exp, ['flg'], ['flg'], scale=-1.0)
            ACT(P, flg[:], flg[:], AF.Ln, ['flg'], ['flg'], bias=1.0)
            ppre = pk[0]
            ptot = pk[1]
            flat = flg[:].rearrange("p a b -> p (a b)")
            MM(P, ppre[:, 0:256], triu_f, flat, True, True, ['flg', 'cst'], ['pk0'])
            MM(P, ptot[:, 0:256], ones_f, flat, True, True, ['flg', 'cst'], ['pk1'])
            tot = sb(ph, "tot", [128, 32, 8], F32)
            car = sb(ph, "car", [128, 32, 8], F32)
            CP(P, 'dve', tot[:].rearrange("p a b -> p (a b)"), ptot[:, 0:256], ['pk1'], ['tot'])
            P.op('dve', lambda e: e.memset(car[:, 0, :], 0.0), [], ['car'])
            for bk in range(1, 32):
                TT(P, 'dve', car[:, bk, :], car[:, bk - 1, :], tot[:, bk - 1, :], ALU.add, ['car', 'tot'], ['car'])
            TT(P, 'dve', NFt[:].rearrange("p a b -> p (a b)"), ppre[:, 0:256],
               car[:].rearrange("p a b -> p (a b)"), ALU.add, ['pk0', 'car'], ['NFt'])
            NF4 = NFt[:].rearrange("p (i k) h -> p i k h", k=4)
            TS(P, 'dve', nNFo[:], NF4[:, :, 0, :], sel[:, 0:1], None, ALU.mult, None, ['NFt', 'sel'], ['nNFo'])
            for k in range(1, 4):
                STT(P, nNFo[:], NF4[:, :, k, :], sel[:, k:k + 1], nNFo[:], ALU.mult, ALU.add,
                    ['NFt', 'sel', 'nNFo'], ['nNFo'])
            TS(P, 'dve', nNFo[:], nNFo[:], -1.0, None, ALU.mult, None, ['nNFo'], ['nNFo'])
            if DEBUG:
                DMA(P, 'sp', dbg['NF'], NFt[:], ['NFt'], ['dbgNF'])
            P.barrier()

        bc = ExitStack()
        hTo = sb(bc, "hTo", [128, 16, 1024], BF16)
        OT = sb(bc, "OT", [128, 16, 1024], BF16)
        if STAGE >= 2:
            with ExitStack() as ph:
                pss = ps(ph, "pssB", [128, 512], F32)
                with ExitStack() as ph1:
                    normT(ph1, xT_own.rearrange("(kc p) t -> p kc t", p=128), 4, hTo, 'hTo', pss)
                    P.barrier()
                KT = sb(ph, "KT", [128, 4096], BF16)
                VH = sb(ph, "VH", [128, 32, 128], BF16)
                wq = sb(ph, "wq", [128, 16, 128], BF16)
                QH = sb(ph, "QH", [128, 1024], BF16)
                Z = sb(ph, "Z", [128, 4096], F32)
                T1 = sb(ph, "T1", [128, 4096], F32)
                PP = sb(ph, "PP", [128, 4096], F32)
                NFbc = sb(ph, "NFbc", [128, 4096], F32)
                W = sb(ph, "W", [128, 4096], BF16)
                WT = [sb(ph, "WT%d" % i, [128, 512], BF16) for i in range(2)]
                Ob = sb(ph, "Ob", [128, 128], BF16)
                small = sb(ph, "small", [128, 8], F32)
                zp = [ps(ph, "zp%d" % i, [128, 512], F32) for i in range(2)]
                tp = [ps(ph, "tp%d" % i, [128, 512], BF16) for i in range(2)]
                op_ = ps(ph, "op", [128, 128], F32)
                otp = ps(ph, "otp", [128, 128], BF16)
                nz = 0
                ntp = 0
                scale = 1.0 / math.sqrt(128.0)
                for hd in range(16):
                    is_sb = hd < 8
                    qc = C_QA + hd * 128 if is_sb else C_QB + (hd - 8) * 128
                    DMA(P, 'sp', KT[:], kt_scr[hd], [('kt_scr', hd)], ['KT'])
                    DMA(P, 'sp', VH[:], v_scr[hd], [('v_scr', hd // 2)], ['VH'])
                    DMA(P, 'pool', wq[:], w_in3[:, :, qc:qc + 128], [], ['wq'])
                    src_t, c0_ = (exp_u, 0) if hd < 8 else (exp_v, D)
                    r0 = (hd % 8) * 2048
                    DMA(P, 'pool', uv_bf[r0:r0 + 2048, c0_:c0_ + D], src_t[r0:r0 + 2048, :], [], [('uv_bf', hd)])
                    for half in range(2):
                        for kc in range(16):
                            MM(P, pss[:], wq[:, kc, :], hTo[:, kc, half * 512:(half + 1) * 512], kc == 0, kc == 15,
                               ['wq', 'hTo'], ['pss'])
                        ACT(P, QH[:, half * 512:(half + 1) * 512], pss[:], AF.Copy, ['pss'], ['QH'], scale=scale)
                    if not is_sb:
                        h = hd - 8
                        dg = T1[:].rearrange("p (a b) -> p a b", b=128)
                        TT(P, 'dve', dg, ident_f.unsqueeze(1).to_broadcast([128, 32, 128]),
                           NFt[:, :, h:h + 1].to_broadcast([128, 32, 128]), ALU.mult, ['cst', 'NFt'], ['T1'])
                        for c in range(8):
                            z = zp[nz % 2]
                            zk = 'zp%d' % (nz % 2)
                            nz += 1
                            MM(P, z[:], ones_f, T1[:, c * 512:(c + 1) * 512], True, True, ['cst', 'T1'], [zk])
                            CP(P, 'act', NFbc[:, c * 512:(c + 1) * 512], z[:], [zk], ['NFbc'])
                    for i in range(8):
                        nch = i + 1
                        L = 512 * nch
                        dsl = slice(512 * i, 512 * i + 512)
                        for c in range(nch):
                            z = zp[nz % 2]
                            zk = 'zp%d' % (nz % 2)
                            nz += 1
                            MM(P, z[:], QH[:, i * 128:(i + 1) * 128], KT[:, c * 512:(c + 1) * 512], True, True,
                               ['QH', 'KT'], [zk])
                            if is_sb:
                                CP(P, 'act', Z[:, c * 512:(c + 1) * 512], z[:], [zk], [('Z', c)])
                            else:
                                TT(P, 'dve', Z[:, c * 512:(c + 1) * 512], z[:], NFbc[:, c * 512:(c + 1) * 512],
                                   ALU.add, [zk, 'NFbc'], [('Z', c)])
                        if is_sb:
                            ACT(P, T1[:, :L], Z[:, :L], AF.Exp, ['Z'], ['T1'])
                            ACT(P, T1[:, :L], T1[:, :L], AF.Ln, ['T1'], ['T1'], bias=1.0)
                            TT(P, 'pool', T1[:, dsl], T1[:, dsl], sb01, ALU.mult, ['T1', 'msk'], ['T1'])
                            P.op('dve', lambda e, L=L: e.tensor_tensor_scan(
                                out=PP[:, :L], data0=ones_f[:, 0:1].to_broadcast([128, L]), data1=T1[:, :L],
                                initial=0.0, op0=ALU.mult, op1=ALU.add), ['T1', 'cst'], ['PP'])
                            TT(P, 'dve', Z[:, 1:L], Z[:, 1:L], PP[:, 0:L - 1], ALU.add, ['Z', 'PP'], ['Z'])
                            TT(P, 'pool', Z[:, dsl], Z[:, dsl], sbadd, ALU.add, ['Z', 'msk'], ['Z'])
                            TS(P, 'dve', small[:, 0:1], PP[:, L - 1:L], -1.0, None, ALU.mult, None, ['PP'], ['small'])
                            ACT(P, W[:, :L], Z[:, :L], AF.Exp, ['Z', 'small'], ['W'], bias=small[:, 0:1])
                        else:
                            TT(P, 'pool', Z[:, dsl], Z[:, dsl], fxadd, ALU.add, ['Z', 'msk'], ['Z'])
                            ACT(P, W[:, :L], Z[:, :L], AF.Exp, ['Z', 'nNFo'], ['W', 'small'],
                                bias=nNFo[:, i, hd - 8:hd - 7], accum_out=small[:, 1:2])
                        nkb = 4 * nch
                        for g in range(nch):
                            t = tp[ntp % 2]
                            tk = 'tp%d' % (ntp % 2)
                            wt = WT[ntp % 2]
                            wtk = 'WT%d' % (ntp % 2)
                            ntp += 1
                            for q in range(4):
                                kb = 4 * g + q
                                TR(P, t[:, q * 128:(q + 1) * 128], W[:, kb * 128:(kb + 1) * 128], ident_b,
                                   ['W', 'cstb'], [tk])
                            CP(P, 'dve', wt[:], t[:], [tk], [wtk])
                            for q in range(4):
                                kb = 4 * g + q
                                MM(P, op_[:], wt[:, q * 128:(q + 1) * 128], VH[:, kb, :], kb == 0, kb == nkb - 1,
                                   [wtk, 'VH'], ['op'])
                        if is_sb:
                            CP(P, 'act', Ob[:], op_[:], ['op'], ['Ob'])
                        else:
                            P.op('dve', lambda e: e.reciprocal(out=small[:, 2:3], in_=small[:, 1:2]),
                                 ['small'], ['small'])
                            TS(P, 'dve', Ob[:], op_[:], small[:, 2:3], None, ALU.mult, None, ['op', 'small'], ['Ob'])
                        TR(P, otp[:], Ob[:], ident_b, ['Ob', 'cstb'], ['otp'])
                        CP(P, 'act', OT[:, hd, i * 128:(i + 1) * 128], otp[:], ['otp'], [('OT', hd)])
                if DEBUG:
                    DMA(P, 'sp', dbg['OT'], OT[:], ['OT'], ['dbgOT'])
                P.barrier()

        if STAGE >= 3:
            with ExitStack() as ph:
                mT = sb(ph, "mT", [128, 16, 1024], BF16)
                wga = sb(ph, "wga", [128, 16, 512], BF16)
                wgb = sb(ph, "wgb", [128, 16, 512], BF16)
                wa = sb(ph, "wa", [128, 8, 512], BF16)
                wb = sb(ph, "wb", [128, 8, 512], BF16)
                sga = sb(ph, "sga", [128, 512], F32)
                sgb = sb(ph, "sgb", [128, 512], F32)
                m1 = sb(ph, "m1", [128, 512], F32)
                m2 = sb(ph, "m2", [128, 512], F32)
                pc = [ps(ph, "pc%d" % i, [128, 512], F32) for i in range(8)]
                w_ba3 = w_ba.rearrange("(kc p) n -> p kc n", p=128)
                w_bb3 = w_bb.rearrange("(kc p) n -> p kc n", p=128)
                w_out3 = w_out.rearrange("(kc p) n -> p kc n", p=128)
                xo = [sb(ph, "xo%d" % i, [128, D], F32) for i in range(2)]
                it = 0
                for ng in range(4):
                    DMA(P, 'pool', wga[:], w_in3[:, :, C_GA + ng * 512:C_GA + (ng + 1) * 512], [], ['wga'])
                    DMA(P, 'pool', wgb[:], w_in3[:, :, C_GB + ng * 512:C_GB + (ng + 1) * 512], [], ['wgb'])
                    DMA(P, 'pool', wa[:], w_ba3[:, :, ng * 512:(ng + 1) * 512], [], ['wa'])
                    DMA(P, 'pool', wb[:], w_bb3[:, :, ng * 512:(ng + 1) * 512], [], ['wb'])
                    for nt in range(4):
                        ns = slice(nt * 128, (nt + 1) * 128)
                        for half in range(2):
                            hs = slice(half * 512, (half + 1) * 512)
                            b0 = 4 * (it % 2)
                            it += 1
                            pga, pgb, pya, pyb = pc[b0], pc[b0 + 1], pc[b0 + 2], pc[b0 + 3]
                            kga, kgb, kya, kyb = ['pc%d' % (b0 + x) for x in range(4)]
                            for kc in range(16):
                                MM(P, pga[:], wga[:, kc, ns], hTo[:, kc, hs], kc == 0, kc == 15, ['wga', 'hTo'], [kga])
                            for kc in range(16):
                                MM(P, pgb[:], wgb[:, kc, ns], hTo[:, kc, hs], kc == 0, kc == 15, ['wgb', 'hTo'], [kgb])
                            for kc in range(8):
                                MM(P, pya[:], wa[:, kc, ns], OT[:, kc, hs], kc == 0, kc == 7, ['wa', 'OT'], [kya])
                            for kc in range(8):
                                MM(P, pyb[:], wb[:, kc, ns], OT[:, 8 + kc, hs], kc == 0, kc == 7, ['wb', 'OT'], [kyb])
                            ACT(P, sga[:], pga[:], AF.Sigmoid, [kga], ['sga'])
                            ACT(P, sgb[:], pgb[:], AF.Sigmoid, [kgb], ['sgb'])
                            TT(P, 'dve', m1[:], sga[:], pya[:], ALU.mult, ['sga', kya], ['m1'])
                            TT(P, 'dve', m2[:], sgb[:], pyb[:], ALU.mult, ['sgb', kyb], ['m2'])
                            TT(P, 'pool', mT[:, ng * 4 + nt, hs], m1[:], m2[:], ALU.add, ['m1', 'm2'], ['mT'])
                wo = sb(ph, "wo", [128, 16, 512], BF16)
                it = 0
                for ng in range(4):
                    DMA(P, 'pool', wo[:], w_out3[:, :, ng * 512:(ng + 1) * 512], [], ['wo'])
                    for tb in range(8):
                        pt = pc[it % 4]
                        pk_ = 'pc%d' % (it % 4)
                        xb_ = xo[it % 2]
                        xk = 'xo%d' % (it % 2)
                        it += 1
                        cs = slice(ng * 512, (ng + 1) * 512)
                        DMA(P, 'sp', xb_[:, 0:512], x_own[tb * 128:(tb + 1) * 128, cs], [], [xk])
                        for kc in range(16):
                            MM(P, pt[:], mT[:, kc, tb * 128:(tb + 1) * 128], wo[:, kc, :], kc == 0, kc == 15,
                               ['mT', 'wo'], [pk_])
                        TT(P, 'dve', xb_[:, 0:512], xb_[:, 0:512], pt[:], ALU.add, [xk, pk_], [xk])
                        DMA(P, 'sp', x1_scr[tb][:, cs], xb_[:, 0:512], [xk], [('x1s', tb, ng)])
                        if DEBUG:
                            DMA(P, 'sp', dbg['x1'][tb][:, cs], xb_[:, 0:512], [xk], [('dbgx1', tb, ng)])
                P.barrier()

        bc.close()
        if STAGE >= 4:
            with ExitStack() as ph:
                gffn = sb(ph, "gffn", [128, D], F32)
                gfin = sb(ph, "gfin", [128, D], F32)
                skb = sb(ph, "skb", [128, 16, 128], BF16)
                rstd2 = sb(ph, "rstd2", [128, 8], F32)
                DMA(P, 'sp', gffn[:], gffn_in[:, :], [], ['gffn'])
                DMA(P, 'sp', gfin[:], gfin_in[:, :], [], ['gfin'])
                DMA(P, 'pool', skb[:], skT.rearrange("ch c n -> c ch n"), [], ['skb'])
                pd = [ps(ph, "pd%d" % i, [128, 512], F32) for i in range(4)]
                jb = sb(ph, "jb", [128, D], BF16)
                x1bs = [sb(ph, "x1b%d" % i, [128, D], F32) for i in range(2)]
                x1b = x1bs[0]
                with ExitStack() as ph1:
                    pdb = [ps(ph1, "pdb%d" % i, [128, 512], BF16) for i in range(2)]
                    h2T = sb(ph1, "h2T", [128, 16, 1024], BF16)
                    qT = sb(ph1, "qT", [128, 16, 1024], BF16)
                    h2b = sb(ph1, "h2b", [128, D], BF16)
                    wqp = sb(ph1, "wqp", [128, 16, 512], BF16)
                    w_q3 = w_query.rearrange("(kc p) n -> p kc n", p=128)
                    ntp = 0
                    for tb in range(8):
                        DMA(P, 'sp', x1b[:], x1_scr[tb], [('x1s', tb)], ['x1b'])
                        ACT(P, jb[:], x1b[:], AF.Square, ['x1b'], ['jb', ('rstd2', tb)],
                            accum_out=rstd2[:, tb:tb + 1])
                        TS(P, 'dve', rstd2[:, tb:tb + 1], rstd2[:, tb:tb + 1], 1.0 / D, EPS, ALU.mult, ALU.add,
                           [('rstd2', tb)], [('rstd2', tb)])
                        ACT(P, rstd2[:, tb:tb + 1], rstd2[:, tb:tb + 1], AF.Sqrt, [('rstd2', tb)], [('rstd2', tb)])
                        P.op('dve', lambda e, tb=tb: e.reciprocal(out=rstd2[:, tb:tb + 1], in_=rstd2[:, tb:tb + 1]),
                             [('rstd2', tb)], [('rstd2', tb)])
                        STT(P, h2b[:], x1b[:], rstd2[:, tb:tb + 1], gffn[:], ALU.mult, ALU.mult,
                            ['x1b', ('rstd2', tb), 'gffn'], ['h2b'])
                        for g in range(4):
                            t = pdb[ntp % 2]
                            tk = 'pdb%d' % (ntp % 2)
                            ntp += 1
                            for q in range(4):
                                kc = 4 * g + q
                                TR(P, t[:, q * 128:(q + 1) * 128], h2b[:, kc * 128:(kc + 1) * 128], ident_b,
                                   ['h2b', 'cstb'], [tk])
                            CP(P, 'act', h2T[:, 4 * g:4 * g + 4, tb * 128:(tb + 1) * 128],
                               t[:].rearrange("p (a b) -> p a b", a=4), [tk], ['h2T'])
                    it = 0
                    for ng in range(4):
                        DMA(P, 'pool', wqp[:], w_q3[:, :, ng * 512:(ng + 1) * 512], [], ['wqp'])
                        for nt in range(4):
                            for half in range(2):
                                pt = pd[it % 4]
                                pk_ = 'pd%d' % (it % 4)
                                it += 1
                                for kc in range(16):
                                    MM(P, pt[:], wqp[:, kc, nt * 128:(nt + 1) * 128],
                                       h2T[:, kc, half * 512:(half + 1) * 512], kc == 0, kc == 15, ['wqp', 'h2T'], [pk_])
                                CP(P, 'act' if it % 2 else 'dve', qT[:, ng * 4 + nt, half * 512:(half + 1) * 512], pt[:],
                                   [pk_], ['qT'])
                    DMA(P, 'sp', q_scr, qT[:], ['qT'], ['q_scr'])
                    P.barrier()
                po = [ps(ph, "po%d" % i, [128, 512], F32) for i in range(4)]
                sc = sb(ph, "sc", [128, 16, 128], F32)
                tmp = sb(ph, "tmp", [128, 256], F32)
                v16 = sb(ph, "v16", [128, 8, 2, 16], F32)
                ix = sb(ph, "ix", [128, 8, 2, 16], U32)
                ixf = sb(ph, "ixf", [128, 8, 2, 16], F32)
                cand = sb(ph, "cand", [128, 8, 16, 16], F32)
                cid = sb(ph, "cid", [128, 8, 16, 16], F32)
                t16 = sb(ph, "t16", [128, 8, 16], F32)
                pos = sb(ph, "pos", [128, 8, 16], U32)
                posf = sb(ph, "posf", [128, 8, 16], F32)
                iot = sb(ph, "iot", [128, 256], F32)
                DMA(P, 'sp', iot[:], iota_in[:, :], [], ['iot'])
                idf = sb(ph, "idf", [128, 128], F32)
                jk2 = sb(ph, "jk2", [128, 256], F32)
                idus = [sb(ph, "idu%d" % i, [128, 128], U32) for i in range(2)]
                gts = [sb(ph, "gt%d" % i, [128, 8, 16], F32) for i in range(2)]
                gs = sb(ph, "gs", [128, 8], F32)
                aa = sb(ph, "aa", [128, 128], F32)
                ww = sb(ph, "ww", [128, 128], F32)
                g1 = sb(ph, "g1", [128, 128], F32)
                h2f = sb(ph, "h2f", [128, D], F32)
                qTbs = [sb(ph, "qTb%d" % i, [128, 16, 128], BF16) for i in range(2)]
                ssf = sb(ph, "ssf", [128, 2], F32)
                GSZ = 8
                dgs = [sb(ph, "dg%d" % i, [128, GSZ, 128], BF16) for i in range(2)]
                NG_, NVR = 5, 14
                gbs = [sb(ph, "gb%d" % i, [128, 2 * D], BF16) for i in range(NG_)]
                vrs = [sb(ph, "vr%d" % i, [128, D], BF16) for i in range(NVR)]
                ngb = [0]

                def topk(tb):
                    idu = idus[tb % 2]
                    ik = 'idu%d' % (tb % 2)
                    gt = gts[tb % 2]
                    gk = 'gt%d' % (tb % 2)
                    qTb = qTbs[tb % 2]
                    qk = 'qTb%d' % (tb % 2)
                    DMA(P, 'sp', qTb[:], q_scr[:, :, tb * 128:(tb + 1) * 128], ['q_scr'], [qk])
                    for g in range(4):
                        for q in range(4):
                            ch = 4 * g + q
                            MM(P, pd[g][:, q * 128:(q + 1) * 128], qTb[:, ch, :], skb[:, ch, :],
                               True, True, [qk, 'skb'], ['pd%d' % g])
                        CP(P, 'act', sc[:, 4 * g:4 * g + 4, :], pd[g][:].rearrange("p (a b) -> p a b", a=4),
                           ['pd%d' % g], ['sc'])
                    for ch in range(16):
                        hh, pp = ch // 2, ch % 2
                        P.op('dve', lambda e, ch=ch, hh=hh, pp=pp: e.max(out=v16[:, hh, pp, 0:8], in_=sc[:, ch, :]),
                             ['sc'], ['v16'])
                        P.op('dve', lambda e, ch=ch, hh=hh, pp=pp: e.max_index(
                            out=ix[:, hh, pp, 0:8], in_max=v16[:, hh, pp, 0:8], in_values=sc[:, ch, :]),
                            ['sc', 'v16'], ['ix'])
                        P.op('dve', lambda e, ch=ch, hh=hh, pp=pp: e.match_replace(
                            out=tmp[:, 0:128], in_to_replace=v16[:, hh, pp, 0:8], in_values=sc[:, ch, :],
                            imm_value=-1e30), ['sc', 'v16'], ['tmp'])
                        P.op('dve', lambda e, ch=ch, hh=hh, pp=pp: e.max(out=v16[:, hh, pp, 8:16], in_=tmp[:, 0:128]),
                             ['tmp'], ['v16'])
                        P.op('dve', lambda e, ch=ch, hh=hh, pp=pp: e.max_index(
                            out=ix[:, hh, pp, 8:16], in_max=v16[:, hh, pp, 8:16], in_values=tmp[:, 0:128]),
                            ['tmp', 'v16'], ['ix'])
                    CP(P, 'dve', ixf[:], ix[:], ['ix'], ['ixf'])
                    TS(P, 'dve', ixf[:, :, 0, :], ixf[:, :, 0, :], 128.0, None, ALU.mult, None, ['ixf'], ['ixf'])
                    TT(P, 'dve', cand[:], v16[:, :, 0, :].unsqueeze(3).to_broadcast([128, 8, 16, 16]),
                       v16[:, :, 1, :].unsqueeze(2).to_broadcast([128, 8, 16, 16]), ALU.add, ['v16'], ['cand'])
                    TT(P, 'dve', cid[:], ixf[:, :, 0, :].unsqueeze(3).to_broadcast([128, 8, 16, 16]),
                       ixf[:, :, 1, :].unsqueeze(2).to_broadcast([128, 8, 16, 16]), ALU.add, ['ixf'], ['cid'])
                    for hh in range(8):
                        cf = cand[:, hh].rearrange("p a b -> p (a b)")
                        P.op('dve', lambda e, hh=hh, cf=cf: e.max(out=t16[:, hh, 0:8], in_=cf), ['cand'], ['t16'])
                        P.op('dve', lambda e, hh=hh, cf=cf: e.max_index(
                            out=pos[:, hh, 0:8], in_max=t16[:, hh, 0:8], in_values=cf), ['cand', 't16'], ['pos'])
                        P.op('dve', lambda e, hh=hh, cf=cf: e.match_replace(
                            out=tmp[:], in_to_replace=t16[:, hh, 0:8], in_values=cf, imm_value=-1e30),
                            ['cand', 't16'], ['tmp'])
                        P.op('dve', lambda e, hh=hh: e.max(out=t16[:, hh, 8:16], in_=tmp[:]), ['tmp'], ['t16'])
                        P.op('dve', lambda e, hh=hh: e.max_index(
                            out=pos[:, hh, 8:16], in_max=t16[:, hh, 8:16], in_values=tmp[:]), ['tmp', 't16'], ['pos'])
                    CP(P, 'dve', posf[:], pos[:], ['pos'], ['posf'])
                    for hh in range(8):
                        cidf = cid[:, hh].rearrange("p a b -> p (a b)")
                        for k in range(16):
                            STT(P, jk2[:], iot[:], posf[:, hh, k:k + 1], cidf, ALU.is_equal, ALU.mult,
                                ['iot', 'cid', 'posf'], [('idf', hh * 16 + k)],
                                accum_out=idf[:, hh * 16 + k:hh * 16 + k + 1])
                    CP(P, 'dve', idu[:], idf[:], ['idf'], [ik])
                    if DEBUG:
                        DMA(P, 'sp', dbg['ids'][:, tb, :], idf[:], ['idf'], [('dbgids', tb)])
                    TT(P, 'dve', gt[:], t16[:], t16[:, :, 0:1].to_broadcast([128, 8, 16]), ALU.subtract, ['t16'], [gk])
                    ACT(P, gt[:], gt[:], AF.Exp, [gk], [gk])
                    P.op('dve', lambda e, gt=gt: e.reduce_sum(out=gs[:], in_=gt[:], axis=AX.X), [gk], ['gs'])
                    P.op('dve', lambda e: e.reciprocal(out=gs[:], in_=gs[:]), ['gs'], ['gs'])
                    TT(P, 'dve', gt[:], gt[:], gs[:].unsqueeze(2).to_broadcast([128, 8, 16]), ALU.mult, [gk, 'gs'], [gk])

                def gather(idu, ik, hk):
                    k = ngb[0]
                    ngb[0] += 1
                    bfr, bk = gbs[k % NG_], 'gb%d' % (k % NG_)
                    vr, vk = vrs[k % NVR], 'vr%d' % (k % NVR)
                    P.dma('pool', lambda e, bfr=bfr, hk=hk: e.indirect_dma_start(
                        out=bfr[:, :], out_offset=None, in_=uv_bf[:, :],
                        in_offset=bass.IndirectOffsetOnAxis(ap=idu[:, hk:hk + 1], axis=0)), [ik, 'uv_bf'], [bk])
                    CP(P, 'act', vr[:], bfr[:, D:2 * D], [bk], [vk])
                    return bfr[:, 0:D], bk, vr, vk

                def u_prep(tb):
                    x1b = x1bs[tb % 2]
                    xk = 'x1b%d' % (tb % 2)
                    DMA(P, 'sp', x1b[:], x1_scr[tb], [('x1s', tb)], [xk])
                    STT(P, h2f[:], x1b[:], rstd2[:, tb:tb + 1], gffn[:], ALU.mult, ALU.mult,
                        [xk, 'rstd2', 'gffn'], ['h2f'])

                ngrp = [0]

                def grp_dots(tb, g, j0, j1, st):
                    idu = idus[tb % 2]
                    ik = 'idu%d' % (tb % 2)
                    for j in range(j0, j1):
                        hk = g * GSZ + j
                        ub, uk, vb, vk = gather(idu, ik, hk)
                        st['v'].append((vb, vk))
                        STT(P, jb[:], ub, 1.0, h2f[:], ALU.mult, ALU.mult, [uk, 'h2f'], [('aa', hk)],
                            accum_out=aa[:, hk:hk + 1])

                def grp_gelu_a(tb, g):
                    hs = slice(g * GSZ, (g + 1) * GSZ)
                    ak = [('aa', g * GSZ + j) for j in range(GSZ)]
                    g1k = ('g1', g % 2)
                    TT(P, 'dve', g1[:, hs], aa[:, hs], aa[:, hs], ALU.mult, ak, [g1k])
                    TS(P, 'dve', g1[:, hs], g1[:, hs], 0.044715, 1.0, ALU.mult, ALU.add, [g1k], [g1k])
                    TT(P, 'dve', g1[:, hs], g1[:, hs], aa[:, hs], ALU.mult, [g1k] + ak, [g1k])
                    ACT(P, g1[:, hs], g1[:, hs], AF.Sigmoid, [g1k], [g1k], scale=2.0 * math.sqrt(2.0 / math.pi))

                def grp_finish(tb, g, st):
                    gt = gts[tb % 2]
                    gk = 'gt%d' % (tb % 2)
                    dg = dgs[ngrp[0] % 2]
                    dk = 'dg%d' % (ngrp[0] % 2)
                    ngrp[0] += 1
                    hs = slice(g * GSZ, (g + 1) * GSZ)
                    ak = [('aa', g * GSZ + j) for j in range(GSZ)]
                    g1k = ('g1', g % 2)
                    TT(P, 'dve', g1[:, hs], g1[:, hs], aa[:, hs], ALU.mult, [g1k] + ak, [g1k])
                    TT(P, 'dve', ww[:, hs], g1[:, hs], gt[:].rearrange("p a b -> p (a b)")[:, hs], ALU.mult,
                       [g1k, gk], [('ww', g % 2)])
                    TT(P, 'dve', dg[:], ident_b.unsqueeze(1).to_broadcast([128, GSZ, 128]),
                       ww[:, hs].unsqueeze(2).to_broadcast([128, GSZ, 128]), ALU.mult, ['cstb', ('ww', g % 2)], [dk])
                    for j in range(GSZ):
                        hk = g * GSZ + j
                        vb, vk = st['v'][j]
                        for c in range(4):
                            MM(P, po[c][:], dg[:, j, :], vb[:, c * 512:(c + 1) * 512], hk == 0, hk == 127,
                               [dk, vk], ['po%d' % c])

                def final(tb):
                    x1b = x1bs[tb % 2]
                    xk = 'x1b%d' % (tb % 2)
                    if DEBUG:
                        DMA(P, 'sp', dbg['gw'][:, tb, :], ww[:], ['ww'], [('dbggw', tb)])
                    for c in range(4):
                        TT(P, 'dve', x1b[:, c * 512:(c + 1) * 512], x1b[:, c * 512:(c + 1) * 512], po[c][:], ALU.add,
                           [xk, 'po%d' % c], [xk])
                    if DEBUG:
                        DMA(P, 'sp', dbg['acc'][tb], x1b[:], [xk], [('dbgacc', tb)])
                    ACT(P, jb[:], x1b[:], AF.Square, [xk], ['ssf'], accum_out=ssf[:, 0:1])
                    TS(P, 'dve', ssf[:, 0:1], ssf[:, 0:1], 1.0 / D, EPS, ALU.mult, ALU.add, ['ssf'], ['ssf'])
                    ACT(P, ssf[:, 0:1], ssf[:, 0:1], AF.Sqrt, ['ssf'], ['ssf'])
                    P.op('dve', lambda e: e.reciprocal(out=ssf[:, 0:1], in_=ssf[:, 0:1]), ['ssf'], ['ssf'])
                    STT(P, x1b[:], x1b[:], ssf[:, 0:1], gfin[:], ALU.mult, ALU.mult, [xk, 'ssf', 'gfin'], [xk])
                    DMA(P, 'sp', out[tb * 128:(tb + 1) * 128, :], x1b[:], [xk], [('out', tb)])

                topk(0)
                for tb in range(8):
                    u_prep(tb)
                    NG = 128 // GSZ
                    prev = None
                    for g in range(NG):
                        st_ = {'v': []}
                        grp_dots(tb, g, 0, GSZ // 2, st_)
                        if prev is not None:
                            grp_finish(tb, g - 1, prev)
                        grp_dots(tb, g, GSZ // 2, GSZ, st_)
                        grp_gelu_a(tb, g)
                        prev = st_
                        if g == 7 and tb + 1 < 8:
                            topk(tb + 1)
                    grp_finish(tb, NG - 1, prev)
                    final(tb)
                P.barrier()
        P.barrier()
        P.emit()
    return nc


def make_core_inputs(inputs):
    x = np.asarray(inputs["x"], dtype=np.float32)
    w_in = np.ascontiguousarray(np.asarray(inputs["w_in"], dtype=np.float32)[0])
    w_ba = np.ascontiguousarray(np.asarray(inputs["w_branch_a"], dtype=np.float32)[0])
    w_bb = np.ascontiguousarray(np.asarray(inputs["w_branch_b"], dtype=np.float32)[0])
    w_out = np.ascontiguousarray(np.asarray(inputs["w_out"], dtype=np.float32)[0])
    w_query = np.ascontiguousarray(np.asarray(inputs["w_query"], dtype=np.float32)[0])
    sk = np.asarray(inputs["sub_keys"], dtype=np.float32)[0]
    skT = np.ascontiguousarray(sk.transpose(0, 1, 3, 2).reshape(16, 128, 128))
    exp_u = np.ascontiguousarray(np.asarray(inputs["expert_u"], dtype=np.float32)[0])
    exp_v = np.ascontiguousarray(np.asarray(inputs["expert_v"], dtype=np.float32)[0])
    gmix = np.ascontiguousarray(np.asarray(inputs["norm_mix_gain"], dtype=np.float32)[0].reshape(16, 128).T)
    gffn = np.ascontiguousarray(np.broadcast_to(np.asarray(inputs["norm_ffn_gain"], dtype=np.float32)[0][None, :], (128, D)))
    gfin = np.ascontiguousarray(np.broadcast_to(np.asarray(inputs["norm_final_gain"], dtype=np.float32)[None, :], (128, D)))
    bfor = np.ascontiguousarray(np.broadcast_to(np.asarray(inputs["b_forget"], dtype=np.float32)[0][None, :], (128, 8)))
    idx = np.arange(128)
    ident = np.eye(128, dtype=np.float32)
    triu = (idx[:, None] <= idx[None, :]).astype(np.float32)
    cst = np.ascontiguousarray(np.concatenate([ident, triu, np.ones((128, 128), np.float32)], axis=1))
    xT = [np.ascontiguousarray(x[b].T) for b in range(2)]
    iota = np.ascontiguousarray(np.broadcast_to(np.arange(256, dtype=np.float32)[None, :], (128, 256)))
    maps = []
    toks = []
    for c in range(8):
        b, j = c // 4, c % 4
        tok = np.concatenate([np.arange((4 * i + j) * 128, (4 * i + j + 1) * 128) for i in range(8)])
        toks.append((b, tok))
        sb01 = np.zeros((128, 4, 128), np.float32)
        fx01 = np.zeros((128, 4, 128), np.float32)
        for k in range(4):
            if k < j:
                sb01[:, k, :] = 1.0
                fx01[:, k, :] = 1.0
            elif k == j:
                sb01[:, k, :] = (idx[None, :] < idx[:, None])
                fx01[:, k, :] = (idx[None, :] <= idx[:, None])
        sbadd = (1.0 - sb01) * NEG
        fxadd = (1.0 - fx01) * NEG
        msk = np.ascontiguousarray(np.concatenate(
            [sb01.reshape(128, 512), sbadd.reshape(128, 512), fxadd.reshape(128, 512), np.zeros((128, 512), np.float32)],
            axis=1).astype(np.float32))
        sel = np.zeros((128, 4), np.float32)
        sel[:, j] = 1.0
        maps.append({
            "xT_full": xT[b], "xT_own": np.ascontiguousarray(xT[b][:, tok]), "x_own": np.ascontiguousarray(x[b][tok]),
            "w_in": w_in, "w_ba": w_ba, "w_bb": w_bb, "w_out": w_out, "w_query": w_query, "skT": skT,
            "exp_u": exp_u, "exp_v": exp_v, "gmix": gmix, "gffn": gffn, "gfin": gfin, "bfor": bfor,
            "cst": cst, "msk": msk, "sel": sel, "iota": iota,
        })
    return maps, toks


def kernel(**inputs):
    maps, toks = make_core_inputs(inputs)
    nc = build_program()
    res = run_bass_kernel_spmd(nc, maps, core_ids=list(range(8)))
    outp = np.zeros((2, 4096, D), np.float32)
    for c in range(8):
        b, tok = toks[c]
        outp[b, tok] = np.asarray(res.results[c]["out"], dtype=np.float32)
    return outp
```

```python
import math
from contextlib import ExitStack

import numpy as np
import concourse.bass as bass
import concourse.mybir as mybir
from concourse.bass_utils import run_bass_kernel_spmd

F32 = mybir.dt.float32
BF16 = mybir.dt.bfloat16
U32 = mybir.dt.uint32
AF = mybir.ActivationFunctionType
ALU = mybir.AluOpType
AX = mybir.AxisListType

ENGS = ['pe', 'act', 'dve', 'pool', 'sp']
NRING = 6
EPS = 1e-6
D = 2048
NEG = -30000.0
STAGE = 99
DEBUG = False


def _conflict(a, b):
    n = min(len(a), len(b))
    return a[:n] == b[:n]


class Prog:
    def __init__(self, nc, stack):
        self.nc = nc
        self.ops = {e: [] for e in ENGS}
        self.ncomp = {e: 0 for e in ENGS}
        self.ndma = {e: 0 for e in ENGS}
        self.waited = {e: {} for e in ENGS}
        self.track = {}
        self.S = {e: stack.enter_context(nc.semaphore('S_' + e)) for e in ['pe', 'act', 'dve', 'pool']}
        self.Dm = {e: [stack.enter_context(nc.semaphore('D_%s%d' % (e, i))) for i in range(NRING)]
                   for e in ['sp', 'act', 'pool']}

    def _sem_of(self, dep):
        kind, e, i = dep
        if kind == 'c':
            return ('S', e), self.S[e], i + 1
        return ('D', e, i % NRING), self.Dm[e][i % NRING], 16 * (i // NRING + 1)

    def _collect(self, reads, writes, me):
        deps = set()
        reads = [r if isinstance(r, tuple) else (r,) for r in reads]
        writes = [w if isinstance(w, tuple) else (w,) for w in writes]
        for r in reads:
            tr = self.track.setdefault(r[0], {})
            for k, (lw, rd) in tr.items():
                if _conflict(k, r) and lw is not None:
                    deps.add(lw)
        for w in writes:
            tr = self.track.setdefault(w[0], {})
            for k, (lw, rd) in tr.items():
                if _conflict(k, w):
                    if lw is not None:
                        deps.add(lw)
                    deps.update(rd)
        for w in writes:
            tr = self.track[w[0]]
            for k in [k for k in tr if _conflict(k, w) and len(k) > len(w)]:
                del tr[k]
            tr[w] = [me, []]
        for r in reads:
            tr = self.track[r[0]]
            if r not in tr:
                lw = None
                for k, (lw2, rd) in tr.items():
                    if _conflict(k, r) and len(k) < len(r) and lw2 is not None:
                        lw = lw2
                tr[r] = [lw, []]
            tr[r][1].append(me)
        deps.discard(me)
        return deps

    def _waits(self, eng, deps, is_dma):
        waits = []
        for dep in sorted(deps):
            kind, e, i = dep
            if kind == 'c' and e == eng and not is_dma and eng == 'pe':
                continue
            key, sem, val = self._sem_of(dep)
            if self.waited[eng].get(key, 0) >= val:
                continue
            self.waited[eng][key] = val
            waits.append((sem, val))
        return waits

    def op(self, eng, fn, reads=(), writes=()):
        me = ('c', eng, self.ncomp[eng])
        deps = self._collect(list(reads), list(writes), me)
        waits = self._waits(eng, deps, False)
        self.ncomp[eng] += 1
        self.ops[eng].append((waits, fn, (self.S[eng], 1)))

    def dma(self, eng, fn, reads=(), writes=()):
        k = self.ndma[eng]
        me = ('d', eng, k)
        deps = self._collect(list(reads), list(writes), me)
        if k >= NRING:
            deps.add(('d', eng, k - NRING))
        waits = self._waits(eng, deps, True)
        self.ndma[eng] += 1
        self.ops[eng].append((waits, fn, (self.Dm[eng][k % NRING], 16)))

    def barrier(self):
        allw = []
        for x in ['pe', 'act', 'dve', 'pool']:
            if self.ncomp[x] > 0:
                allw.append((('S', x), self.S[x], self.ncomp[x]))
        for q in ['sp', 'act', 'pool']:
            for r in range(NRING):
                n = 0 if self.ndma[q] <= r else (self.ndma[q] - r + NRING - 1) // NRING
                if n > 0:
                    allw.append((('D', q, r), self.Dm[q][r], 16 * n))
        for e in ENGS:
            waits = []
            for key, sem, val in allw:
                if self.waited[e].get(key, 0) >= val:
                    continue
                self.waited[e][key] = val
                waits.append((sem, val))
            self.ops[e].append((waits, None, None))

    def emit(self):
        nc = self.nc
        names = {'pe': 'tensor', 'act': 'scalar', 'dve': 'vector', 'pool': 'gpsimd', 'sp': 'sync'}
        with nc.Block() as block:
            for e in ENGS:
                ops = self.ops[e]

                def body(engine, ops=ops):
                    for waits, fn, inc in ops:
                        for sem, val in waits:
                            engine.wait_ge(sem, val)
                        if fn is not None:
                            ins = fn(engine)
                            ins.then_inc(inc[0], inc[1])
                getattr(block, names[e])(body)


def DMA(P, q, out, in_, reads, writes):
    P.dma(q, lambda e: e.dma_start(out=out, in_=in_), reads, writes)


def MM(P, out, lhsT, rhs, start, stop, reads, writes):
    P.op('pe', lambda e: e.matmul(out, lhsT=lhsT, rhs=rhs, start=start, stop=stop), reads, writes)


def TR(P, out, in_, ident, reads, writes):
    P.op('pe', lambda e: e.transpose(out=out, in_=in_, identity=ident), reads, writes)


def ACT(P, out, in_, func, reads, writes, **kw):
    P.op('act', lambda e: e.activation(out=out, in_=in_, func=func, **kw), reads, writes)


def TT(P, eng, out, in0, in1, op, reads, writes):
    P.op(eng, lambda e: e.tensor_tensor(out=out, in0=in0, in1=in1, op=op), reads, writes)


def TS(P, eng, out, in0, s1, s2, op0, op1, reads, writes, **kw):
    if op1 is None:
        P.op(eng, lambda e: e.tensor_scalar(out=out, in0=in0, scalar1=s1, scalar2=None, op0=op0, **kw), reads, writes)
    else:
        P.op(eng, lambda e: e.tensor_scalar(out=out, in0=in0, scalar1=s1, scalar2=s2, op0=op0, op1=op1, **kw),
             reads, writes)


def STT(P, out, in0, scalar, in1, op0, op1, reads, writes, **kw):
    P.op('dve', lambda e: e.scalar_tensor_tensor(out=out, in0=in0, scalar=scalar, in1=in1, op0=op0, op1=op1, **kw),
         reads, writes)


def CP(P, eng, out, in_, reads, writes):
    if eng == 'act':
        P.op('act', lambda e: e.activation(out=out, in_=in_, func=AF.Copy), reads, writes)
    else:
        P.op(eng, lambda e: e.tensor_copy(out=out, in_=in_), reads, writes)


C_QA, C_KA, C_VA, C_QB, C_KB, C_VB, C_F, C_GA, C_GB = 0, 1024, 2048, 3072, 4096, 5120, 6144, 6152, 8200
IN_W = 10248


def build_program():
    nc = bass.Bass("TRN2", target_bir_lowering=False)

    def din(name, shape, dt=F32):
        return nc.dram_tensor(name, shape, dt, kind="ExternalInput").ap()

    xT_full = din("xT_full", [D, 4096])
    xT_own = din("xT_own", [D, 1024])
    x_own = din("x_own", [1024, D])
    w_in = din("w_in", [D, IN_W])
    w_ba = din("w_ba", [1024, D])
    w_bb = din("w_bb", [1024, D])
    w_out = din("w_out", [D, D])
    w_query = din("w_query", [D, D])
    skT = din("skT", [16, 128, 128])
    exp_u = din("exp_u", [16384, D])
    exp_v = din("exp_v", [16384, D])
    gmix_in = din("gmix", [128, 16])
    gffn_in = din("gffn", [128, D])
    gfin_in = din("gfin", [128, D])
    bfor_in = din("bfor", [128, 8])
    cst_in = din("cst", [128, 3 * 128])
    msk_in = din("msk", [128, 4 * 512])
    sel_in = din("sel", [128, 4])
    iota_in = din("iota", [128, 256])
    out = nc.dram_tensor("out", [1024, D], F32, kind="ExternalOutput").ap()
    kt_scr = nc.dram_tensor("kt_scr", [16, 128, 4096], BF16, kind="Internal").ap()
    v_scr = nc.dram_tensor("v_scr", [16, 128, 32, 128], BF16, kind="Internal").ap()
    x1_scr = nc.dram_tensor("x1_scr", [8, 128, D], F32, kind="Internal").ap()
    q_scr = nc.dram_tensor("q_scr", [128, 16, 1024], BF16, kind="Internal").ap()
    uv_bf = nc.dram_tensor("uv_bf", [16384, 2 * D], BF16, kind="Internal").ap()
    dbg = {}
    if DEBUG:
        dbg['OT'] = nc.dram_tensor("dbg_OT", [128, 16, 1024], BF16, kind="ExternalOutput").ap()
        dbg['x1'] = nc.dram_tensor("dbg_x1", [8, 128, D], F32, kind="ExternalOutput").ap()
        dbg['NF'] = nc.dram_tensor("dbg_NF", [128, 32, 8], F32, kind="ExternalOutput").ap()
        dbg['ids'] = nc.dram_tensor("dbg_ids", [128, 8, 128], F32, kind="ExternalOutput").ap()
        dbg['gw'] = nc.dram_tensor("dbg_gw", [128, 8, 128], F32, kind="ExternalOutput").ap()
        dbg['acc'] = nc.dram_tensor("dbg_acc", [8, 128, D], F32, kind="ExternalOutput").ap()

    with ExitStack() as st:
        P = Prog(nc, st)

        def sb(stack, name, shape, dt):
            return stack.enter_context(nc.sbuf_tensor("s_" + name, shape, dt))

        def ps(stack, name, shape, dt):
            return stack.enter_context(nc.psum_tensor("p_" + name, shape, dt))

        cst = sb(st, "cst", [128, 384], F32)
        cstb = sb(st, "cstb", [128, 384], BF16)
        gmix = sb(st, "gmix", [128, 16], F32)
        bfor = sb(st, "bfor", [128, 8], F32)
        sel = sb(st, "sel", [128, 4], F32)
        msk = sb(st, "msk", [128, 2048], F32)
        NFt = sb(st, "NFt", [128, 32, 8], F32)
        nNFo = sb(st, "nNFo", [128, 8, 8], F32)
        DMA(P, 'sp', cst[:], cst_in[:, :], [], ['cst'])
        DMA(P, 'sp', gmix[:], gmix_in[:, :], [], ['gmix'])
        DMA(P, 'sp', bfor[:], bfor_in[:, :], [], ['bfor'])
        DMA(P, 'sp', sel[:], sel_in[:, :], [], ['sel'])
        DMA(P, 'sp', msk[:], msk_in[:, :], [], ['msk'])
        CP(P, 'dve', cstb[:], cst[:], ['cst'], ['cstb'])
        ident_f, triu_f, ones_f = cst[:, 0:128], cst[:, 128:256], cst[:, 256:384]
        ident_b, ones_b = cstb[:, 0:128], cstb[:, 256:384]
        sb01, sbadd, fxadd = msk[:, 0:512], msk[:, 512:1024], msk[:, 1024:1536]

        def normT(ph, src3, nchunks, dst, dkey, pss):
            xss = [sb(ph, "xs%d_" % i + dkey, [128, 16, 256], F32) for i in range(2)]
            sqs = [sb(ph, "sq%d_" % i + dkey, [128, 16, 256], BF16) for i in range(2)]
            rss = [sb(ph, "rs%d_" % i + dkey, [128, 256], F32) for i in range(2)]
            for c in range(nchunks):
                xs, sq, rs = xss[c % 2], sqs[c % 2], rss[c % 2]
                xk, sk_, rk = 'xs%d' % (c % 2), 'sq%d' % (c % 2), 'rs%d' % (c % 2)
                pcs = pss[:, (c % 2) * 256:(c % 2) * 256 + 256]
                pk_ = ('pss', c % 2)
                DMA(P, 'sp', xs[:], src3[:, :, c * 256:(c + 1) * 256], [], [xk])
                ACT(P, sq[:], xs[:], AF.Square, [xk], [sk_])
                for kc in range(16):
                    MM(P, pcs, ones_b, sq[:, kc, :], kc == 0, kc == 15, [sk_, 'cstb'], [pk_])
                TS(P, 'dve', rs[:], pcs, 1.0 / D, EPS, ALU.mult, ALU.add, [pk_], [rk])
                ACT(P, rs[:], rs[:], AF.Sqrt, [rk], [rk])
                P.op('dve', lambda e, rs=rs: e.reciprocal(out=rs[:], in_=rs[:]), [rk], [rk])
                for kc in range(16):
                    STT(P, dst[:, kc, c * 256:(c + 1) * 256], xs[:, kc, :], gmix[:, kc:kc + 1], rs[:],
                        ALU.mult, ALU.mult, [xk, rk, 'gmix'], [(dkey, c // 2, c % 2, kc)])

        w_in3 = w_in.rearrange("(kc p) n -> p kc n", p=128)

        with ExitStack() as ph:
            hTf = sb(ph, "hTf", [128, 16, 4096], BF16)
            pss = ps(ph, "pssA", [128, 512], F32)
            with ExitStack() as ph1:
                normT(ph1, xT_full.rearrange("(kc p) t -> p kc t", p=128), 16, hTf, 'hTf', pss)
                P.barrier()
            wg = [sb(ph, "wg%d" % i, [128, 16, 256], BF16) for i in range(2)]
            wf = sb(ph, "wf", [128, 16, 8], BF16)
            kst = [sb(ph, "kst%d" % i, [128, 512], BF16) for i in range(4)]
            vst = [sb(ph, "vst%d" % i, [128, 256], BF16) for i in range(4)]
            pk = [ps(ph, "pk%d" % i, [128, 512], F32) for i in range(4)]
            pf = ps(ph, "pf", [128, 8], F32)
            flg = sb(ph, "flg", [128, 32, 8], F32)
            gi = 0
            nev = 0
            nk = 0
            for (c0, h0) in [(C_KA + 256 * g, 2 * g) for g in range(4)] + [(C_KB + 256 * g, 8 + 2 * g) for g in range(4)]:
                w = wg[gi % 2]
                wk = 'wg%d' % (gi % 2)
                gi += 1
                DMA(P, 'pool', w[:], w_in3[:, :, c0:c0 + 256], [], [wk])
                for hh in range(2):
                    for c in range(8):
                        pt = pk[nev % 4]
                        pkk = 'pk%d' % (nev % 4)
                        for kc in range(16):
                            MM(P, pt[:], w[:, kc, hh * 128:(hh + 1) * 128], hTf[:, kc, c * 512:(c + 1) * 512],
                               kc == 0, kc == 15, [wk, ('hTf', c)], [pkk])
                        ks = kst[nk % 4]
                        kk = 'kst%d' % (nk % 4)
                        nk += 1
                        CP(P, 'act' if nev % 2 == 0 else 'dve', ks[:], pt[:], [pkk], [kk])
                        nev += 1
                        DMA(P, 'sp', kt_scr[h0 + hh][:, c * 512:(c + 1) * 512], ks[:], [kk], [('kt_scr', h0 + hh, c)])
            nv = 0
            for (c0, h0) in [(C_VA + 256 * g, 2 * g) for g in range(4)] + [(C_VB + 256 * g, 8 + 2 * g) for g in range(4)]:
                w = wg[gi % 2]
                wk = 'wg%d' % (gi % 2)
                gi += 1
                DMA(P, 'pool', w[:], w_in3[:, :, c0:c0 + 256], [], [wk])
                for tb in range(32):
                    pt = pk[nev % 4]
                    pkk = 'pk%d' % (nev % 4)
                    for kc in range(16):
                        MM(P, pt[:, 0:256], hTf[:, kc, tb * 128:(tb + 1) * 128], w[:, kc, :], kc == 0, kc == 15,
                           [wk, ('hTf', tb // 4)], [pkk])
                    vs = vst[nv % 4]
                    vk = 'vst%d' % (nv % 4)
                    nv += 1
                    CP(P, 'act' if nev % 2 == 0 else 'dve', vs[:], pt[:, 0:256], [pkk], [vk])
                    nev += 1
                    DMA(P, 'sp', v_scr[h0:h0 + 2, :, tb, :].rearrange("h p d -> p h d"),
                        vs[:].rearrange("p (h d) -> p h d", h=2), [vk], [('v_scr', h0 // 2, tb)])
            DMA(P, 'pool', wf[:], w_in3[:, :, C_F:C_F + 8], [], ['wf'])
            for tb in range(32):
                for kc in range(16):
                    MM(P, pf[:], hTf[:, kc, tb * 128:(tb + 1) * 128], wf[:, kc, :], kc == 0, kc == 15,
                       ['wf', ('hTf', tb // 4)], ['pf'])
                CP(P, 'dve', flg[:, tb, :], pf[:], ['pf'], [('flg', tb)])
            TT(P, 'dve', flg[:], flg[:], bfor[:].unsqueeze(1).to_broadcast([128, 32, 8]), ALU.add,
               ['flg', 'bfor'], ['flg'])
            ACT(P, flg[:], flg[:], AF.Exp, ['flg'], ['flg'], scale=-1.0)
            ACT(P, flg[:], flg[:], AF.Ln, ['flg'], ['flg'], bias=1.0)
            ppre = pk[0]
            ptot = pk[1]
            flat = flg[:].rearrange("p a b -> p (a b)")
            MM(P, ppre[:, 0:256], triu_f, flat, True, True, ['flg', 'cst'], ['pk0'])
            MM(P, ptot[:, 0:256], ones_f, flat, True, True, ['flg', 'cst'], ['pk1'])
            tot = sb(ph, "tot", [128, 32, 8], F32)
            car = sb(ph, "car", [128, 32, 8], F32)
            CP(P, 'dve', tot[:].rearrange("p a b -> p (a b)"), ptot[:, 0:256], ['pk1'], ['tot'])
            P.op('dve', lambda e: e.memset(car[:, 0, :], 0.0), [], ['car'])
            for bk in range(1, 32):
                TT(P, 'dve', car[:, bk, :], car[:, bk - 1, :], tot[:, bk - 1, :], ALU.add, ['car', 'tot'], ['car'])
            TT(P, 'dve', NFt[:].rearrange("p a b -> p (a b)"), ppre[:, 0:256],
               car[:].rearrange("p a b -> p (a b)"), ALU.add, ['pk0', 'car'], ['NFt'])
            NF4 = NFt[:].rearrange("p (i k) h -> p i k h", k=4)
            TS(P, 'dve', nNFo[:], NF4[:, :, 0, :], sel[:, 0:1], None, ALU.mult, None, ['NFt', 'sel'], ['nNFo'])
            for k in range(1, 4):
                STT(P, nNFo[:], NF4[:, :, k, :], sel[:, k:k + 1], nNFo[:], ALU.mult, ALU.add,
                    ['NFt', 'sel', 'nNFo'], ['nNFo'])
            TS(P, 'dve', nNFo[:], nNFo[:], -1.0, None, ALU.mult, None, ['nNFo'], ['nNFo'])
            if DEBUG:
                DMA(P, 'sp', dbg['NF'], NFt[:], ['NFt'], ['dbgNF'])
            P.barrier()

        bc = ExitStack()
        hTo = sb(bc, "hTo", [128, 16, 1024], BF16)
        OT = sb(bc, "OT", [128, 16, 1024], BF16)
        if STAGE >= 2:
            with ExitStack() as ph:
                pss = ps(ph, "pssB", [128, 512], F32)
                with ExitStack() as ph1:
                    normT(ph1, xT_own.rearrange("(kc p) t -> p kc t", p=128), 4, hTo, 'hTo', pss)
                    P.barrier()
                KTs = [sb(ph, "KT%d" % i, [128, 4096], BF16) for i in range(2)]
                VH_ = sb(ph, "VH", [128, 32, 128], BF16)
                VHs = [VH_, VH_]
                wq = sb(ph, "wq", [128, 16, 128], BF16)
                QH = sb(ph, "QH", [128, 1024], BF16)
                Zs = [sb(ph, "Z%d" % i, [128, 4096], F32) for i in range(2)]
                T1 = sb(ph, "T1", [128, 4096], F32)
                PP = sb(ph, "PP", [128, 4096], F32)
                NFbc = sb(ph, "NFbc", [128, 4096], F32)
                Ws = [sb(ph, "W%d" % i, [128, 4096], BF16) for i in range(2)]
                negt = sb(ph, "negt", [128, 4], F32)
                lpart = sb(ph, "lpart", [128, 2, 8], F32)
                lsum = sb(ph, "lsum", [128, 4], F32)
                WT = [sb(ph, "WT%d" % i, [128, 512], BF16) for i in range(2)]
                Ob = sb(ph, "Ob", [128, 128], BF16)
                small = sb(ph, "small", [128, 8], F32)
                zp = [ps(ph, "zp%d" % i, [128, 512], F32) for i in range(2)]
                tp = [ps(ph, "tp%d" % i, [128, 512], BF16) for i in range(2)]
                op_ = ps(ph, "op", [128, 128], F32)
                otp = ps(ph, "otp", [128, 128], BF16)
                scale = 1.0 / math.sqrt(128.0)
                def load_k(hd):
                    DMA(P, 'sp', KTs[hd % 2][:], kt_scr[hd], [('kt_scr', hd)], ['KT%d' % (hd % 2)])

                def load_v(hd):
                    DMA(P, 'sp', VH_[:], v_scr[hd], [('v_scr', hd // 2)], ['VH'])

                def csl(c):
                    return slice(512 * c, 512 * c + 512)

                cnt = {'nz': 0, 'ntp': 0}

                def prologue(hd):
                    is_sb = hd < 8
                    if hd + 1 < 16:
                        load_k(hd + 1)
                    qc = C_QA + hd * 128 if is_sb else C_QB + (hd - 8) * 128
                    DMA(P, 'pool', wq[:], w_in3[:, :, qc:qc + 128], [], ['wq'])
                    src_t, c0_ = (exp_u, 0) if hd < 8 else (exp_v, D)
                    r0 = (hd % 8) * 2048
                    DMA(P, 'pool', uv_bf[r0:r0 + 2048, c0_:c0_ + D], src_t[r0:r0 + 2048, :], [], [('uv_bf', hd)])
                    for half in range(2):
                        for kc in range(16):
                            MM(P, pss[:], wq[:, kc, :], hTo[:, kc, half * 512:(half + 1) * 512], kc == 0, kc == 15,
                               ['wq', 'hTo'], ['pss'])
                        ACT(P, QH[:, half * 512:(half + 1) * 512], pss[:], AF.Copy, ['pss'], ['QH'], scale=scale)
                    if not is_sb:
                        h = hd - 8
                        dg = T1[:].rearrange("p (a b) -> p a b", b=128)
                        TT(P, 'dve', dg, ident_f.unsqueeze(1).to_broadcast([128, 32, 128]),
                           NFt[:, :, h:h + 1].to_broadcast([128, 32, 128]), ALU.mult, ['cst', 'NFt'], ['T1'])
                        for c in range(8):
                            z = zp[cnt['nz'] % 2]
                            zk = 'zp%d' % (cnt['nz'] % 2)
                            cnt['nz'] += 1
                            MM(P, z[:], ones_f, T1[:, csl(c)], True, True, ['cst', 'T1'], [zk])
                            CP(P, 'act', NFbc[:, csl(c)], z[:], [zk], [('NFbc', c)])

                def stage_q(r, hd, i):
                    is_sb = hd < 8
                    nch = i + 1
                    KT, ktk = KTs[hd % 2], 'KT%d' % (hd % 2)
                    Z, zn = Zs[r % 2], 'Z%d' % (r % 2)
                    for c in range(nch):
                        z = zp[cnt['nz'] % 2]
                        zk = 'zp%d' % (cnt['nz'] % 2)
                        cnt['nz'] += 1
                        MM(P, z[:], QH[:, i * 128:(i + 1) * 128], KT[:, csl(c)], True, True, ['QH', ktk], [zk])
                        if is_sb:
                            CP(P, 'act', Z[:, csl(c)], z[:], [zk], [(zn, c)])
                        else:
                            TT(P, 'dve', Z[:, csl(c)], z[:], NFbc[:, csl(c)], ALU.add, [zk, ('NFbc', c)], [(zn, c)])

                def stage_e(r, hd, i):
                    is_sb = hd < 8
                    nch = i + 1
                    L = 512 * nch
                    Z, zn = Zs[r % 2], 'Z%d' % (r % 2)
                    W, wn = Ws[r % 2], 'W%d' % (r % 2)
                    ngc = r % 4
                    lp = lpart[:, r % 2, :]
                    lpk = ('lpart', r % 2)
                    if is_sb:
                        for c in range(nch):
                            ACT(P, T1[:, csl(c)], Z[:, csl(c)], AF.Exp, [(zn, c)], [('T1', c)])
                        for c in range(nch):
                            ACT(P, T1[:, csl(c)], T1[:, csl(c)], AF.Ln, [('T1', c)], [('T1', c)], bias=1.0)
                        TT(P, 'pool', T1[:, csl(i)], T1[:, csl(i)], sb01, ALU.mult, [('T1', i), 'msk'], [('T1', i)])
                        for c in range(nch):
                            init = 0.0 if c == 0 else PP[:, 512 * c - 1:512 * c]
                            o_ap, d0_ap, d1_ap = PP[:, csl(c)], ones_f[:, 0:1].to_broadcast([128, 512]), T1[:, csl(c)]
                            P.op('dve', lambda e, o_ap=o_ap, d0_ap=d0_ap, d1_ap=d1_ap, init=init: e.tensor_tensor_scan(
                                out=o_ap, data0=d0_ap, data1=d1_ap, initial=init, op0=ALU.mult, op1=ALU.add),
                                [('T1', c), 'cst'] + ([('PP', c - 1)] if c else []), [('PP', c)])
                        TS(P, 'dve', negt[:, ngc:ngc + 1], PP[:, L - 1:L], -1.0, None, ALU.mult, None,
                           [('PP', nch - 1)], [('negt', ngc)])
                        for c in range(nch):
                            if c == 0:
                                TT(P, 'dve', Z[:, 1:512], Z[:, 1:512], PP[:, 0:511], ALU.add,
                                   [(zn, 0), ('PP', 0)], [(zn, 0)])
                            else:
                                TT(P, 'dve', Z[:, csl(c)], Z[:, csl(c)], PP[:, 512 * c - 1:512 * c + 511], ALU.add,
                                   [(zn, c), ('PP', c), ('PP', c - 1)], [(zn, c)])
                        TT(P, 'pool', Z[:, csl(i)], Z[:, csl(i)], sbadd, ALU.add, [(zn, i), 'msk'], [(zn, i)])
                        for c in range(nch):
                            ACT(P, W[:, csl(c)], Z[:, csl(c)], AF.Exp, [(zn, c), ('negt', ngc)], [(wn, c)],
                                bias=negt[:, ngc:ngc + 1])
                    else:
                        TT(P, 'pool', Z[:, csl(i)], Z[:, csl(i)], fxadd, ALU.add, [(zn, i), 'msk'], [(zn, i)])
                        for c in range(nch):
                            ACT(P, W[:, csl(c)], Z[:, csl(c)], AF.Exp, [(zn, c), 'nNFo'], [(wn, c), lpk + (c,)],
                                bias=nNFo[:, i, hd - 8:hd - 7], accum_out=lp[:, c:c + 1])
                        P.op('dve', lambda e, lp=lp, nch=nch, ngc=ngc: e.reduce_sum(
                            out=lsum[:, ngc:ngc + 1], in_=lp[:, 0:nch], axis=AX.X), [lpk], [('lsum', ngc)])
                        P.op('dve', lambda e, ngc=ngc: e.reciprocal(out=lsum[:, ngc:ngc + 1], in_=lsum[:, ngc:ngc + 1]),
                             [('lsum', ngc)], [('lsum', ngc)])

                def stage_b(r, hd, i):
                    is_sb = hd < 8
                    nch = i + 1
                    W, wn = Ws[r % 2], 'W%d' % (r % 2)
                    ngc = r % 4
                    nkb = 4 * nch
                    for g in range(nch):
                        t = tp[cnt['ntp'] % 2]
                        tk = 'tp%d' % (cnt['ntp'] % 2)
                        wt = WT[cnt['ntp'] % 2]
                        wtk = 'WT%d' % (cnt['ntp'] % 2)
                        cnt['ntp'] += 1
                        for q in range(4):
                            kb = 4 * g + q
                            TR(P, t[:, q * 128:(q + 1) * 128], W[:, kb * 128:(kb + 1) * 128], ident_b,
                               [(wn, g), 'cstb'], [tk])
                        CP(P, 'dve', wt[:], t[:], [tk], [wtk])
                        for q in range(4):
                            kb = 4 * g + q
                            MM(P, op_[:], wt[:, q * 128:(q + 1) * 128], VH_[:, kb, :], kb == 0, kb == nkb - 1,
                               [wtk, 'VH'], ['op'])
                    if is_sb:
                        CP(P, 'dve', Ob[:], op_[:], ['op'], ['Ob'])
                    else:
                        TS(P, 'dve', Ob[:], op_[:], lsum[:, ngc:ngc + 1], None, ALU.mult, None,
                           ['op', ('lsum', ngc)], ['Ob'])
                    TR(P, otp[:], Ob[:], ident_b, ['Ob', 'cstb'], ['otp'])
                    CP(P, 'dve', OT[:, hd, i * 128:(i + 1) * 128], otp[:], ['otp'], [('OT', hd)])

                rows = [(hd, i) for hd in range(16) for i in range(8)]
                nr = len(rows)
                load_k(0)
                for it in range(nr + 2):
                    if it < nr:
                        hd, i = rows[it]
                        if i == 0:
                            prologue(hd)
                        stage_q(it, hd, i)
                    if 0 <= it - 2 < nr:
                        hd, i = rows[it - 2]
                        if i == 0:
                            load_v(hd)
                        stage_b(it - 2, hd, i)
                    if 0 <= it - 1 < nr:
                        hd, i = rows[it - 1]
                        stage_e(it - 1, hd, i)
                if DEBUG:
                    DMA(P, 'sp', dbg['OT'], OT[:], ['OT'], ['dbgOT'])
                P.barrier()

        if STAGE >= 3:
            with ExitStack() as ph:
                mT = sb(ph, "mT", [128, 16, 1024], BF16)
                wga = sb(ph, "wga", [128, 16, 512], BF16)
                wgb = sb(ph, "wgb", [128, 16, 512], BF16)
                wa = sb(ph, "wa", [128, 8, 512], BF16)
                wb = sb(ph, "wb", [128, 8, 512], BF16)
                sga = sb(ph, "sga", [128, 512], F32)
                sgb = sb(ph, "sgb", [128, 512], F32)
                m1 = sb(ph, "m1", [128, 512], F32)
                m2 = sb(ph, "m2", [128, 512], F32)
                pc = [ps(ph, "pc%d" % i, [128, 512], F32) for i in range(8)]
                w_ba3 = w_ba.rearrange("(kc p) n -> p kc n", p=128)
                w_bb3 = w_bb.rearrange("(kc p) n -> p kc n", p=128)
                w_out3 = w_out.rearrange("(kc p) n -> p kc n", p=128)
                xo = [sb(ph, "xo%d" % i, [128, D], F32) for i in range(2)]
                it = 0
                for ng in range(4):
                    DMA(P, 'pool', wga[:], w_in3[:, :, C_GA + ng * 512:C_GA + (ng + 1) * 512], [], ['wga'])
                    DMA(P, 'pool', wgb[:], w_in3[:, :, C_GB + ng * 512:C_GB + (ng + 1) * 512], [], ['wgb'])
                    DMA(P, 'pool', wa[:], w_ba3[:, :, ng * 512:(ng + 1) * 512], [], ['wa'])
                    DMA(P, 'pool', wb[:], w_bb3[:, :, ng * 512:(ng + 1) * 512], [], ['wb'])
                    for nt in range(4):
                        ns = slice(nt * 128, (nt + 1) * 128)
                        for half in range(2):
                            hs = slice(half * 512, (half + 1) * 512)
                            b0 = 4 * (it % 2)
                            it += 1
                            pga, pgb, pya, pyb = pc[b0], pc[b0 + 1], pc[b0 + 2], pc[b0 + 3]
                            kga, kgb, kya, kyb = ['pc%d' % (b0 + x) for x in range(4)]
                            for kc in range(16):
                                MM(P, pga[:], wga[:, kc, ns], hTo[:, kc, hs], kc == 0, kc == 15, ['wga', 'hTo'], [kga])
                            for kc in range(16):
                                MM(P, pgb[:], wgb[:, kc, ns], hTo[:, kc, hs], kc == 0, kc == 15, ['wgb', 'hTo'], [kgb])
                            for kc in range(8):
                                MM(P, pya[:], wa[:, kc, ns], OT[:, kc, hs], kc == 0, kc == 7, ['wa', 'OT'], [kya])
                            for kc in range(8):
                                MM(P, pyb[:], wb[:, kc, ns], OT[:, 8 + kc, hs], kc == 0, kc == 7, ['wb', 'OT'], [kyb])
                            ACT(P, sga[:], pga[:], AF.Sigmoid, [kga], ['sga'])
                            ACT(P, sgb[:], pgb[:], AF.Sigmoid, [kgb], ['sgb'])
                            TT(P, 'dve', m1[:], sga[:], pya[:], ALU.mult, ['sga', kya], ['m1'])
                            TT(P, 'dve', m2[:], sgb[:], pyb[:], ALU.mult, ['sgb', kyb], ['m2'])
                            TT(P, 'pool', mT[:, ng * 4 + nt, hs], m1[:], m2[:], ALU.add, ['m1', 'm2'], ['mT'])
                wo = sb(ph, "wo", [128, 16, 512], BF16)
                it = 0
                for ng in range(4):
                    DMA(P, 'pool', wo[:], w_out3[:, :, ng * 512:(ng + 1) * 512], [], ['wo'])
                    for tb in range(8):
                        pt = pc[it % 4]
                        pk_ = 'pc%d' % (it % 4)
                        xb_ = xo[it % 2]
                        xk = 'xo%d' % (it % 2)
                        it += 1
                        cs = slice(ng * 512, (ng + 1) * 512)
                        DMA(P, 'sp', xb_[:, 0:512], x_own[tb * 128:(tb + 1) * 128, cs], [], [xk])
                        for kc in range(16):
                            MM(P, pt[:], mT[:, kc, tb * 128:(tb + 1) * 128], wo[:, kc, :], kc == 0, kc == 15,
                               ['mT', 'wo'], [pk_])
                        TT(P, 'dve', xb_[:, 0:512], xb_[:, 0:512], pt[:], ALU.add, [xk, pk_], [xk])
                        DMA(P, 'sp', x1_scr[tb][:, cs], xb_[:, 0:512], [xk], [('x1s', tb, ng)])
                        if DEBUG:
                            DMA(P, 'sp', dbg['x1'][tb][:, cs], xb_[:, 0:512], [xk], [('dbgx1', tb, ng)])
                P.barrier()

        bc.close()
        if STAGE >= 4:
            with ExitStack() as ph:
                gffn = sb(ph, "gffn", [128, D], F32)
                gfin = sb(ph, "gfin", [128, D], F32)
                skb = sb(ph, "skb", [128, 16, 128], BF16)
                rstd2 = sb(ph, "rstd2", [128, 8], F32)
                DMA(P, 'sp', gffn[:], gffn_in[:, :], [], ['gffn'])
                DMA(P, 'sp', gfin[:], gfin_in[:, :], [], ['gfin'])
                DMA(P, 'pool', skb[:], skT.rearrange("ch c n -> c ch n"), [], ['skb'])
                pd = [ps(ph, "pd%d" % i, [128, 512], F32) for i in range(4)]
                jb = sb(ph, "jb", [128, D], BF16)
                x1bs = [sb(ph, "x1b%d" % i, [128, D], F32) for i in range(2)]
                x1b = x1bs[0]
                with ExitStack() as ph1:
                    pdb = [ps(ph1, "pdb%d" % i, [128, 512], BF16) for i in range(2)]
                    h2T = sb(ph1, "h2T", [128, 16, 1024], BF16)
                    qT = sb(ph1, "qT", [128, 16, 1024], BF16)
                    h2b = sb(ph1, "h2b", [128, D], BF16)
                    wqp = sb(ph1, "wqp", [128, 16, 512], BF16)
                    w_q3 = w_query.rearrange("(kc p) n -> p kc n", p=128)
                    ntp = 0
                    for tb in range(8):
                        DMA(P, 'sp', x1b[:], x1_scr[tb], [('x1s', tb)], ['x1b'])
                        ACT(P, jb[:], x1b[:], AF.Square, ['x1b'], ['jb', ('rstd2', tb)],
                            accum_out=rstd2[:, tb:tb + 1])
                        TS(P, 'dve', rstd2[:, tb:tb + 1], rstd2[:, tb:tb + 1], 1.0 / D, EPS, ALU.mult, ALU.add,
                           [('rstd2', tb)], [('rstd2', tb)])
                        ACT(P, rstd2[:, tb:tb + 1], rstd2[:, tb:tb + 1], AF.Sqrt, [('rstd2', tb)], [('rstd2', tb)])
                        P.op('dve', lambda e, tb=tb: e.reciprocal(out=rstd2[:, tb:tb + 1], in_=rstd2[:, tb:tb + 1]),
                             [('rstd2', tb)], [('rstd2', tb)])
                        STT(P, h2b[:], x1b[:], rstd2[:, tb:tb + 1], gffn[:], ALU.mult, ALU.mult,
                            ['x1b', ('rstd2', tb), 'gffn'], ['h2b'])
                        for g in range(4):
                            t = pdb[ntp % 2]
                            tk = 'pdb%d' % (ntp % 2)
                            ntp += 1
                            for q in range(4):
                                kc = 4 * g + q
                                TR(P, t[:, q * 128:(q + 1) * 128], h2b[:, kc * 128:(kc + 1) * 128], ident_b,
                                   ['h2b', 'cstb'], [tk])
                            CP(P, 'act', h2T[:, 4 * g:4 * g + 4, tb * 128:(tb + 1) * 128],
                               t[:].rearrange("p (a b) -> p a b", a=4), [tk], ['h2T'])
                    it = 0
                    for ng in range(4):
                        DMA(P, 'pool', wqp[:], w_q3[:, :, ng * 512:(ng + 1) * 512], [], ['wqp'])
                        for nt in range(4):
                            for half in range(2):
                                pt = pd[it % 4]
                                pk_ = 'pd%d' % (it % 4)
                                it += 1
                                for kc in range(16):
                                    MM(P, pt[:], wqp[:, kc, nt * 128:(nt + 1) * 128],
                                       h2T[:, kc, half * 512:(half + 1) * 512], kc == 0, kc == 15, ['wqp', 'h2T'], [pk_])
                                CP(P, 'act' if it % 2 else 'dve', qT[:, ng * 4 + nt, half * 512:(half + 1) * 512], pt[:],
                                   [pk_], ['qT'])
                    DMA(P, 'sp', q_scr, qT[:], ['qT'], ['q_scr'])
                    P.barrier()
                po = [ps(ph, "po%d" % i, [128, 512], F32) for i in range(4)]
                sc = sb(ph, "sc", [128, 16, 128], F32)
                tmp = sb(ph, "tmp", [128, 256], F32)
                v16 = sb(ph, "v16", [128, 8, 2, 16], F32)
                ix = sb(ph, "ix", [128, 8, 2, 16], U32)
                ixf = sb(ph, "ixf", [128, 8, 2, 16], F32)
                cand = sb(ph, "cand", [128, 8, 16, 16], F32)
                cid = sb(ph, "cid", [128, 8, 16, 16], F32)
                t16 = sb(ph, "t16", [128, 8, 16], F32)
                pos = sb(ph, "pos", [128, 8, 16], U32)
                posf = sb(ph, "posf", [128, 8, 16], F32)
                iot = sb(ph, "iot", [128, 256], F32)
                DMA(P, 'sp', iot[:], iota_in[:, :], [], ['iot'])
                idf = sb(ph, "idf", [128, 128], F32)
                jk2 = sb(ph, "jk2", [128, 256], F32)
                idus = [sb(ph, "idu%d" % i, [128, 128], U32) for i in range(2)]
                gts = [sb(ph, "gt%d" % i, [128, 8, 16], F32) for i in range(2)]
                gs = sb(ph, "gs", [128, 8], F32)
                aa = sb(ph, "aa", [128, 128], F32)
                ww = sb(ph, "ww", [128, 128], F32)
                g1 = sb(ph, "g1", [128, 128], F32)
                h2f = sb(ph, "h2f", [128, D], F32)
                qTbs = [sb(ph, "qTb%d" % i, [128, 16, 128], BF16) for i in range(2)]
                ssf = sb(ph, "ssf", [128, 2], F32)
                GSZ = 8
                dgs = [sb(ph, "dg%d" % i, [128, GSZ, 128], BF16) for i in range(2)]
                NG_, NVR = 5, 14
                gbs = [sb(ph, "gb%d" % i, [128, 2 * D], BF16) for i in range(NG_)]
                vrs = [sb(ph, "vr%d" % i, [128, D], BF16) for i in range(NVR)]
                ngb = [0]

                def topk(tb):
                    idu = idus[tb % 2]
                    ik = 'idu%d' % (tb % 2)
                    gt = gts[tb % 2]
                    gk = 'gt%d' % (tb % 2)
                    qTb = qTbs[tb % 2]
                    qk = 'qTb%d' % (tb % 2)
                    DMA(P, 'sp', qTb[:], q_scr[:, :, tb * 128:(tb + 1) * 128], ['q_scr'], [qk])
                    for g in range(4):
                        for q in range(4):
                            ch = 4 * g + q
                            MM(P, pd[g][:, q * 128:(q + 1) * 128], qTb[:, ch, :], skb[:, ch, :],
                               True, True, [qk, 'skb'], ['pd%d' % g])
                        CP(P, 'act', sc[:, 4 * g:4 * g + 4, :], pd[g][:].rearrange("p (a b) -> p a b", a=4),
                           ['pd%d' % g], ['sc'])
                    for ch in range(16):
                        hh, pp = ch // 2, ch % 2
                        P.op('dve', lambda e, ch=ch, hh=hh, pp=pp: e.max(out=v16[:, hh, pp, 0:8], in_=sc[:, ch, :]),
                             ['sc'], ['v16'])
                        P.op('dve', lambda e, ch=ch, hh=hh, pp=pp: e.max_index(
                            out=ix[:, hh, pp, 0:8], in_max=v16[:, hh, pp, 0:8], in_values=sc[:, ch, :]),
                            ['sc', 'v16'], ['ix'])
                        P.op('dve', lambda e, ch=ch, hh=hh, pp=pp: e.match_replace(
                            out=tmp[:, 0:128], in_to_replace=v16[:, hh, pp, 0:8], in_values=sc[:, ch, :],
                            imm_value=-1e30), ['sc', 'v16'], ['tmp'])
                        P.op('dve', lambda e, ch=ch, hh=hh, pp=pp: e.max(out=v16[:, hh, pp, 8:16], in_=tmp[:, 0:128]),
                             ['tmp'], ['v16'])
                        P.op('dve', lambda e, ch=ch, hh=hh, pp=pp: e.max_index(
                            out=ix[:, hh, pp, 8:16], in_max=v16[:, hh, pp, 8:16], in_values=tmp[:, 0:128]),
                            ['tmp', 'v16'], ['ix'])
                    CP(P, 'dve', ixf[:], ix[:], ['ix'], ['ixf'])
                    TS(P, 'dve', ixf[:, :, 0, :], ixf[:, :, 0, :], 128.0, None, ALU.mult, None, ['ixf'], ['ixf'])
                    TT(P, 'dve', cand[:], v16[:, :, 0, :].unsqueeze(3).to_broadcast([128, 8, 16, 16]),
                       v16[:, :, 1, :].unsqueeze(2).to_broadcast([128, 8, 16, 16]), ALU.add, ['v16'], ['cand'])
                    TT(P, 'dve', cid[:], ixf[:, :, 0, :].unsqueeze(3).to_broadcast([128, 8, 16, 16]),
                       ixf[:, :, 1, :].unsqueeze(2).to_broadcast([128, 8, 16, 16]), ALU.add, ['ixf'], ['cid'])
                    for hh in range(8):
                        cf = cand[:, hh].rearrange("p a b -> p (a b)")
                        P.op('dve', lambda e, hh=hh, cf=cf: e.max(out=t16[:, hh, 0:8], in_=cf), ['cand'], ['t16'])
                        P.op('dve', lambda e, hh=hh, cf=cf: e.max_index(
                            out=pos[:, hh, 0:8], in_max=t16[:, hh, 0:8], in_values=cf), ['cand', 't16'], ['pos'])
                        P.op('dve', lambda e, hh=hh, cf=cf: e.match_replace(
                            out=tmp[:], in_to_replace=t16[:, hh, 0:8], in_values=cf, imm_value=-1e30),
                            ['cand', 't16'], ['tmp'])
                        P.op('dve', lambda e, hh=hh: e.max(out=t16[:, hh, 8:16], in_=tmp[:]), ['tmp'], ['t16'])
                        P.op('dve', lambda e, hh=hh: e.max_index(
                            out=pos[:, hh, 8:16], in_max=t16[:, hh, 8:16], in_values=tmp[:]), ['tmp', 't16'], ['pos'])
                    CP(P, 'dve', posf[:], pos[:], ['pos'], ['posf'])
                    for hh in range(8):
                        cidf = cid[:, hh].rearrange("p a b -> p (a b)")
                        for k in range(16):
                            STT(P, jk2[:], iot[:], posf[:, hh, k:k + 1], cidf, ALU.is_equal, ALU.mult,
                                ['iot', 'cid', 'posf'], [('idf', hh * 16 + k)],
                                accum_out=idf[:, hh * 16 + k:hh * 16 + k + 1])
                    CP(P, 'dve', idu[:], idf[:], ['idf'], [ik])
                    if DEBUG:
                        DMA(P, 'sp', dbg['ids'][:, tb, :], idf[:], ['idf'], [('dbgids', tb)])
                    TT(P, 'dve', gt[:], t16[:], t16[:, :, 0:1].to_broadcast([128, 8, 16]), ALU.subtract, ['t16'], [gk])
                    ACT(P, gt[:], gt[:], AF.Exp, [gk], [gk])
                    P.op('dve', lambda e, gt=gt: e.reduce_sum(out=gs[:], in_=gt[:], axis=AX.X), [gk], ['gs'])
                    P.op('dve', lambda e: e.reciprocal(out=gs[:], in_=gs[:]), ['gs'], ['gs'])
                    TT(P, 'dve', gt[:], gt[:], gs[:].unsqueeze(2).to_broadcast([128, 8, 16]), ALU.mult, [gk, 'gs'], [gk])

                def gather(idu, ik, hk):
                    k = ngb[0]
                    ngb[0] += 1
                    bfr, bk = gbs[k % NG_], 'gb%d' % (k % NG_)
                    vr, vk = vrs[k % NVR], 'vr%d' % (k % NVR)
                    P.dma('pool', lambda e, bfr=bfr, hk=hk: e.indirect_dma_start(
                        out=bfr[:, :], out_offset=None, in_=uv_bf[:, :],
                        in_offset=bass.IndirectOffsetOnAxis(ap=idu[:, hk:hk + 1], axis=0)), [ik, 'uv_bf'], [bk])
                    CP(P, 'act', vr[:], bfr[:, D:2 * D], [bk], [vk])
                    return bfr[:, 0:D], bk, vr, vk

                def u_prep(tb):
                    x1b = x1bs[tb % 2]
                    xk = 'x1b%d' % (tb % 2)
                    DMA(P, 'sp', x1b[:], x1_scr[tb], [('x1s', tb)], [xk])
                    STT(P, h2f[:], x1b[:], rstd2[:, tb:tb + 1], gffn[:], ALU.mult, ALU.mult,
                        [xk, 'rstd2', 'gffn'], ['h2f'])

                ngrp = [0]

                def grp_dots(tb, g, j0, j1, st):
                    idu = idus[tb % 2]
                    ik = 'idu%d' % (tb % 2)
                    for j in range(j0, j1):
                        hk = g * GSZ + j
                        ub, uk, vb, vk = gather(idu, ik, hk)
                        st['v'].append((vb, vk))
                        STT(P, jb[:], ub, 1.0, h2f[:], ALU.mult, ALU.mult, [uk, 'h2f'], [('aa', hk)],
                            accum_out=aa[:, hk:hk + 1])

                def grp_gelu_a(tb, g):
                    hs = slice(g * GSZ, (g + 1) * GSZ)
                    ak = [('aa', g * GSZ + j) for j in range(GSZ)]
                    g1k = ('g1', g % 2)
                    TT(P, 'dve', g1[:, hs], aa[:, hs], aa[:, hs], ALU.mult, ak, [g1k])
                    TS(P, 'dve', g1[:, hs], g1[:, hs], 0.044715, 1.0, ALU.mult, ALU.add, [g1k], [g1k])
                    TT(P, 'dve', g1[:, hs], g1[:, hs], aa[:, hs], ALU.mult, [g1k] + ak, [g1k])
                    ACT(P, g1[:, hs], g1[:, hs], AF.Sigmoid, [g1k], [g1k], scale=2.0 * math.sqrt(2.0 / math.pi))

                def grp_finish(tb, g, st):
                    gt = gts[tb % 2]
                    gk = 'gt%d' % (tb % 2)
                    dg = dgs[ngrp[0] % 2]
                    dk = 'dg%d' % (ngrp[0] % 2)
                    ngrp[0] += 1
                    hs = slice(g * GSZ, (g + 1) * GSZ)
                    ak = [('aa', g * GSZ + j) for j in range(GSZ)]
                    g1k = ('g1', g % 2)
                    TT(P, 'dve', g1[:, hs], g1[:, hs], aa[:, hs], ALU.mult, [g1k] + ak, [g1k])
                    TT(P, 'dve', ww[:, hs], g1[:, hs], gt[:].rearrange("p a b -> p (a b)")[:, hs], ALU.mult,
                       [g1k, gk], [('ww', g % 2)])
                    TT(P, 'dve', dg[:], ident_b.unsqueeze(1).to_broadcast([128, GSZ, 128]),
                       ww[:, hs].unsqueeze(2).to_broadcast([128, GSZ, 128]), ALU.mult, ['cstb', ('ww', g % 2)], [dk])
                    for j in range(GSZ):
                        hk = g * GSZ + j
                        vb, vk = st['v'][j]
                        for c in range(4):
                            MM(P, po[c][:], dg[:, j, :], vb[:, c * 512:(c + 1) * 512], hk == 0, hk == 127,
                               [dk, vk], ['po%d' % c])

                def final(tb):
                    x1b = x1bs[tb % 2]
                    xk = 'x1b%d' % (tb % 2)
                    if DEBUG:
                        DMA(P, 'sp', dbg['gw'][:, tb, :], ww[:], ['ww'], [('dbggw', tb)])
                    for c in range(4):
                        TT(P, 'dve', x1b[:, c * 512:(c + 1) * 512], x1b[:, c * 512:(c + 1) * 512], po[c][:], ALU.add,
                           [xk, 'po%d' % c], [xk])
                    if DEBUG:
                        DMA(P, 'sp', dbg['acc'][tb], x1b[:], [xk], [('dbgacc', tb)])
                    ACT(P, jb[:], x1b[:], AF.Square, [xk], ['ssf'], accum_out=ssf[:, 0:1])
                    TS(P, 'dve', ssf[:, 0:1], ssf[:, 0:1], 1.0 / D, EPS, ALU.mult, ALU.add, ['ssf'], ['ssf'])
                    ACT(P, ssf[:, 0:1], ssf[:, 0:1], AF.Sqrt, ['ssf'], ['ssf'])
                    P.op('dve', lambda e: e.reciprocal(out=ssf[:, 0:1], in_=ssf[:, 0:1]), ['ssf'], ['ssf'])
                    STT(P, x1b[:], x1b[:], ssf[:, 0:1], gfin[:], ALU.mult, ALU.mult, [xk, 'ssf', 'gfin'], [xk])
                    DMA(P, 'sp', out[tb * 128:(tb + 1) * 128, :], x1b[:], [xk], [('out', tb)])

                topk(0)
                for tb in range(8):
                    u_prep(tb)
                    NG = 128 // GSZ
                    prev = None
                    for g in range(NG):
                        st_ = {'v': []}
                        grp_dots(tb, g, 0, GSZ // 2, st_)
                        if prev is not None:
                            grp_finish(tb, g - 1, prev)
                        grp_dots(tb, g, GSZ // 2, GSZ, st_)
                        grp_gelu_a(tb, g)
                        prev = st_
                        if g == 7 and tb + 1 < 8:
                            topk(tb + 1)
                    grp_finish(tb, NG - 1, prev)
                    final(tb)
                P.barrier()
        P.barrier()
        P.emit()
    return nc


def make_core_inputs(inputs):
    x = np.asarray(inputs["x"], dtype=np.float32)
    w_in = np.ascontiguousarray(np.asarray(inputs["w_in"], dtype=np.float32)[0])
    w_ba = np.ascontiguousarray(np.asarray(inputs["w_branch_a"], dtype=np.float32)[0])
    w_bb = np.ascontiguousarray(np.asarray(inputs["w_branch_b"], dtype=np.float32)[0])
    w_out = np.ascontiguousarray(np.asarray(inputs["w_out"], dtype=np.float32)[0])
    w_query = np.ascontiguousarray(np.asarray(inputs["w_query"], dtype=np.float32)[0])
    sk = np.asarray(inputs["sub_keys"], dtype=np.float32)[0]
    skT = np.ascontiguousarray(sk.transpose(0, 1, 3, 2).reshape(16, 128, 128))
    exp_u = np.ascontiguousarray(np.asarray(inputs["expert_u"], dtype=np.float32)[0])
    exp_v = np.ascontiguousarray(np.asarray(inputs["expert_v"], dtype=np.float32)[0])
    gmix = np.ascontiguousarray(np.asarray(inputs["norm_mix_gain"], dtype=np.float32)[0].reshape(16, 128).T)
    gffn = np.ascontiguousarray(np.broadcast_to(np.asarray(inputs["norm_ffn_gain"], dtype=np.float32)[0][None, :], (128, D)))
    gfin = np.ascontiguousarray(np.broadcast_to(np.asarray(inputs["norm_final_gain"], dtype=np.float32)[None, :], (128, D)))
    bfor = np.ascontiguousarray(np.broadcast_to(np.asarray(inputs["b_forget"], dtype=np.float32)[0][None, :], (128, 8)))
    idx = np.arange(128)
    ident = np.eye(128, dtype=np.float32)
    triu = (idx[:, None] <= idx[None, :]).astype(np.float32)
    cst = np.ascontiguousarray(np.concatenate([ident, triu, np.ones((128, 128), np.float32)], axis=1))
    xT = [np.ascontiguousarray(x[b].T) for b in range(2)]
    iota = np.ascontiguousarray(np.broadcast_to(np.arange(256, dtype=np.float32)[None, :], (128, 256)))
    maps = []
    toks = []
    for c in range(8):
        b, j = c // 4, c % 4
        tok = np.concatenate([np.arange((4 * i + j) * 128, (4 * i + j + 1) * 128) for i in range(8)])
        toks.append((b, tok))
        sb01 = np.zeros((128, 4, 128), np.float32)
        fx01 = np.zeros((128, 4, 128), np.float32)
        for k in range(4):
            if k < j:
                sb01[:, k, :] = 1.0
                fx01[:, k, :] = 1.0
            elif k == j:
                sb01[:, k, :] = (idx[None, :] < idx[:, None])
                fx01[:, k, :] = (idx[None, :] <= idx[:, None])
        sbadd = (1.0 - sb01) * NEG
        fxadd = (1.0 - fx01) * NEG
        msk = np.ascontiguousarray(np.concatenate(
            [sb01.reshape(128, 512), sbadd.reshape(128, 512), fxadd.reshape(128, 512), np.zeros((128, 512), np.float32)],
            axis=1).astype(np.float32))
        sel = np.zeros((128, 4), np.float32)
        sel[:, j] = 1.0
        maps.append({
            "xT_full": xT[b], "xT_own": np.ascontiguousarray(xT[b][:, tok]), "x_own": np.ascontiguousarray(x[b][tok]),
            "w_in": w_in, "w_ba": w_ba, "w_bb": w_bb, "w_out": w_out, "w_query": w_query, "skT": skT,
            "exp_u": exp_u, "exp_v": exp_v, "gmix": gmix, "gffn": gffn, "gfin": gfin, "bfor": bfor,
            "cst": cst, "msk": msk, "sel": sel, "iota": iota,
        })
    return maps, toks


def kernel(**inputs):
    maps, toks = make_core_inputs(inputs)
    nc = build_program()
    res = run_bass_kernel_spmd(nc, maps, core_ids=list(range(8)))
    outp = np.zeros((2, 4096, D), np.float32)
    for c in range(8):
        b, tok = toks[c]
        outp[b, tok] = np.asarray(res.results[c]["out"], dtype=np.float32)
    return outp
```

```python
import math
from contextlib import ExitStack

import numpy as np
import concourse.bass as bass
import concourse.mybir as mybir
from concourse.bass_utils import run_bass_kernel_spmd

F32 = mybir.dt.float32
BF16 = mybir.dt.bfloat16
U32 = mybir.dt.uint32
AF = mybir.ActivationFunctionType
ALU = mybir.AluOpType
AX = mybir.AxisListType

ENGS = ['pe', 'act', 'dve', 'pool', 'sp']
NRING = 6
EPS = 1e-6
D = 2048
NEG = -30000.0
STAGE = 99
DEBUG = False


def _conflict(a, b):
    n = min(len(a), len(b))
    return a[:n] == b[:n]


class Prog:
    def __init__(self, nc, stack):
        self.nc = nc
        self.ops = {e: [] for e in ENGS}
        self.ncomp = {e: 0 for e in ENGS}
        self.ndma = {e: 0 for e in ENGS}
        self.waited = {e: {} for e in ENGS}
        self.track = {}
        self.S = {e: stack.enter_context(nc.semaphore('S_' + e)) for e in ['pe', 'act', 'dve', 'pool']}
        self.Dm = {e: [stack.enter_context(nc.semaphore('D_%s%d' % (e, i))) for i in range(NRING)]
                   for e in ['sp', 'act', 'pool']}

    def _sem_of(self, dep):
        kind, e, i = dep
        if kind == 'c':
            return ('S', e), self.S[e], i + 1
        return ('D', e, i % NRING), self.Dm[e][i % NRING], 16 * (i // NRING + 1)

    def _collect(self, reads, writes, me):
        deps = set()
        reads = [r if isinstance(r, tuple) else (r,) for r in reads]
        writes = [w if isinstance(w, tuple) else (w,) for w in writes]
        for r in reads:
            tr = self.track.setdefault(r[0], {})
            for k, (lw, rd) in tr.items():
                if _conflict(k, r) and lw is not None:
                    deps.add(lw)
        for w in writes:
            tr = self.track.setdefault(w[0], {})
            for k, (lw, rd) in tr.items():
                if _conflict(k, w):
                    if lw is not None:
                        deps.add(lw)
                    deps.update(rd)
        for w in writes:
            tr = self.track[w[0]]
            for k in [k for k in tr if _conflict(k, w) and len(k) > len(w)]:
                del tr[k]
            tr[w] = [me, []]
        for r in reads:
            tr = self.track[r[0]]
            if r not in tr:
                lw = None
                for k, (lw2, rd) in tr.items():
                    if _conflict(k, r) and len(k) < len(r) and lw2 is not None:
                        lw = lw2
                tr[r] = [lw, []]
            tr[r][1].append(me)
        deps.discard(me)
        return deps

    def _waits(self, eng, deps, is_dma):
        waits = []
        for dep in sorted(deps):
            kind, e, i = dep
            if kind == 'c' and e == eng and not is_dma and eng == 'pe':
                continue
            key, sem, val = self._sem_of(dep)
            if self.waited[eng].get(key, 0) >= val:
                continue
            self.waited[eng][key] = val
            waits.append((sem, val))
        return waits

    def op(self, eng, fn, reads=(), writes=()):
        me = ('c', eng, self.ncomp[eng])
        deps = self._collect(list(reads), list(writes), me)
        waits = self._waits(eng, deps, False)
        self.ncomp[eng] += 1
        self.ops[eng].append((waits, fn, (self.S[eng], 1)))

    def dma(self, eng, fn, reads=(), writes=()):
        k = self.ndma[eng]
        me = ('d', eng, k)
        deps = self._collect(list(reads), list(writes), me)
        if k >= NRING:
            deps.add(('d', eng, k - NRING))
        waits = self._waits(eng, deps, True)
        self.ndma[eng] += 1
        self.ops[eng].append((waits, fn, (self.Dm[eng][k % NRING], 16)))

    def barrier(self):
        allw = []
        for x in ['pe', 'act', 'dve', 'pool']:
            if self.ncomp[x] > 0:
                allw.append((('S', x), self.S[x], self.ncomp[x]))
        for q in ['sp', 'act', 'pool']:
            for r in range(NRING):
                n = 0 if self.ndma[q] <= r else (self.ndma[q] - r + NRING - 1) // NRING
                if n > 0:
                    allw.append((('D', q, r), self.Dm[q][r], 16 * n))
        for e in ENGS:
            waits = []
            for key, sem, val in allw:
                if self.waited[e].get(key, 0) >= val:
                    continue
                self.waited[e][key] = val
                waits.append((sem, val))
            self.ops[e].append((waits, None, None))

    def emit(self):
        nc = self.nc
        names = {'pe': 'tensor', 'act': 'scalar', 'dve': 'vector', 'pool': 'gpsimd', 'sp': 'sync'}
        with nc.Block() as block:
            for e in ENGS:
                ops = self.ops[e]

                def body(engine, ops=ops):
                    for waits, fn, inc in ops:
                        for sem, val in waits:
                            engine.wait_ge(sem, val)
                        if fn is not None:
                            ins = fn(engine)
                            ins.then_inc(inc[0], inc[1])
                getattr(block, names[e])(body)


def DMA(P, q, out, in_, reads, writes):
    P.dma(q, lambda e: e.dma_start(out=out, in_=in_), reads, writes)


def MM(P, out, lhsT, rhs, start, stop, reads, writes):
    P.op('pe', lambda e: e.matmul(out, lhsT=lhsT, rhs=rhs, start=start, stop=stop), reads, writes)


def TR(P, out, in_, ident, reads, writes):
    P.op('pe', lambda e: e.transpose(out=out, in_=in_, identity=ident), reads, writes)


def ACT(P, out, in_, func, reads, writes, **kw):
    P.op('act', lambda e: e.activation(out=out, in_=in_, func=func, **kw), reads, writes)


def TT(P, eng, out, in0, in1, op, reads, writes):
    P.op(eng, lambda e: e.tensor_tensor(out=out, in0=in0, in1=in1, op=op), reads, writes)


def TS(P, eng, out, in0, s1, s2, op0, op1, reads, writes, **kw):
    if op1 is None:
        P.op(eng, lambda e: e.tensor_scalar(out=out, in0=in0, scalar1=s1, scalar2=None, op0=op0, **kw), reads, writes)
    else:
        P.op(eng, lambda e: e.tensor_scalar(out=out, in0=in0, scalar1=s1, scalar2=s2, op0=op0, op1=op1, **kw),
             reads, writes)


def STT(P, out, in0, scalar, in1, op0, op1, reads, writes, **kw):
    P.op('dve', lambda e: e.scalar_tensor_tensor(out=out, in0=in0, scalar=scalar, in1=in1, op0=op0, op1=op1, **kw),
         reads, writes)


def CP(P, eng, out, in_, reads, writes):
    if eng == 'act':
        P.op('act', lambda e: e.activation(out=out, in_=in_, func=AF.Copy), reads, writes)
    else:
        P.op(eng, lambda e: e.tensor_copy(out=out, in_=in_), reads, writes)


C_QA, C_KA, C_VA, C_QB, C_KB, C_VB, C_F, C_GA, C_GB = 0, 1024, 2048, 3072, 4096, 5120, 6144, 6152, 8200
IN_W = 10248


def build_program():
    nc = bass.Bass("TRN2", target_bir_lowering=False)

    def din(name, shape, dt=F32):
        return nc.dram_tensor(name, shape, dt, kind="ExternalInput").ap()

    xT_full = din("xT_full", [D, 4096])
    xT_own = din("xT_own", [D, 1024])
    x_own = din("x_own", [1024, D])
    w_in = din("w_in", [D, IN_W])
    w_ba = din("w_ba", [1024, D])
    w_bb = din("w_bb", [1024, D])
    w_out = din("w_out", [D, D])
    w_query = din("w_query", [D, D])
    skT = din("skT", [16, 128, 128])
    exp_u = din("exp_u", [16384, D])
    exp_v = din("exp_v", [16384, D])
    gmix_in = din("gmix", [128, 16])
    gffn_in = din("gffn", [128, D])
    gfin_in = din("gfin", [128, D])
    bfor_in = din("bfor", [128, 8])
    cst_in = din("cst", [128, 3 * 128])
    msk_in = din("msk", [128, 4 * 512])
    sel_in = din("sel", [128, 4])
    iota_in = din("iota", [128, 256])
    out = nc.dram_tensor("out", [1024, D], F32, kind="ExternalOutput").ap()
    kt_scr = nc.dram_tensor("kt_scr", [16, 128, 4096], BF16, kind="Internal").ap()
    v_scr = nc.dram_tensor("v_scr", [16, 128, 32, 128], BF16, kind="Internal").ap()
    x1_scr = nc.dram_tensor("x1_scr", [8, 128, D], F32, kind="Internal").ap()
    q_scr = nc.dram_tensor("q_scr", [128, 16, 1024], BF16, kind="Internal").ap()
    uv_bf = nc.dram_tensor("uv_bf", [16384, 2 * D], BF16, kind="Internal").ap()
    dbg = {}
    if DEBUG:
        dbg['OT'] = nc.dram_tensor("dbg_OT", [128, 16, 1024], BF16, kind="ExternalOutput").ap()
        dbg['x1'] = nc.dram_tensor("dbg_x1", [8, 128, D], F32, kind="ExternalOutput").ap()
        dbg['NF'] = nc.dram_tensor("dbg_NF", [128, 32, 8], F32, kind="ExternalOutput").ap()
        dbg['ids'] = nc.dram_tensor("dbg_ids", [128, 8, 128], F32, kind="ExternalOutput").ap()
        dbg['gw'] = nc.dram_tensor("dbg_gw", [128, 8, 128], F32, kind="ExternalOutput").ap()
        dbg['acc'] = nc.dram_tensor("dbg_acc", [8, 128, D], F32, kind="ExternalOutput").ap()

    with ExitStack() as st:
        P = Prog(nc, st)

        def sb(stack, name, shape, dt):
            return stack.enter_context(nc.sbuf_tensor("s_" + name, shape, dt))

        def ps(stack, name, shape, dt):
            return stack.enter_context(nc.psum_tensor("p_" + name, shape, dt))

        cst = sb(st, "cst", [128, 384], F32)
        cstb = sb(st, "cstb", [128, 384], BF16)
        gmix = sb(st, "gmix", [128, 16], F32)
        bfor = sb(st, "bfor", [128, 8], F32)
        sel = sb(st, "sel", [128, 4], F32)
        msk = sb(st, "msk", [128, 2048], F32)
        NFt = sb(st, "NFt", [128, 32, 8], F32)
        nNFo = sb(st, "nNFo", [128, 8, 8], F32)
        DMA(P, 'sp', cst[:], cst_in[:, :], [], ['cst'])
        DMA(P, 'sp', gmix[:], gmix_in[:, :], [], ['gmix'])
        DMA(P, 'sp', bfor[:], bfor_in[:, :], [], ['bfor'])
        DMA(P, 'sp', sel[:], sel_in[:, :], [], ['sel'])
        DMA(P, 'sp', msk[:], msk_in[:, :], [], ['msk'])
        CP(P, 'dve', cstb[:], cst[:], ['cst'], ['cstb'])
        ident_f, triu_f, ones_f = cst[:, 0:128], cst[:, 128:256], cst[:, 256:384]
        ident_b, ones_b = cstb[:, 0:128], cstb[:, 256:384]
        sb01, sbadd, fxadd = msk[:, 0:512], msk[:, 512:1024], msk[:, 1024:1536]

        def normT(ph, src3, nchunks, dst, dkey, pss):
            xss = [sb(ph, "xs%d_" % i + dkey, [128, 16, 256], F32) for i in range(2)]
            sqs = [sb(ph, "sq%d_" % i + dkey, [128, 16, 256], BF16) for i in range(2)]
            rss = [sb(ph, "rs%d_" % i + dkey, [128, 256], F32) for i in range(2)]
            for c in range(nchunks):
                xs, sq, rs = xss[c % 2], sqs[c % 2], rss[c % 2]
                xk, sk_, rk = 'xs%d' % (c % 2), 'sq%d' % (c % 2), 'rs%d' % (c % 2)
                pcs = pss[:, (c % 2) * 256:(c % 2) * 256 + 256]
                pk_ = ('pss', c % 2)
                DMA(P, 'sp', xs[:], src3[:, :, c * 256:(c + 1) * 256], [], [xk])
                ACT(P, sq[:], xs[:], AF.Square, [xk], [sk_])
                for kc in range(16):
                    MM(P, pcs, ones_b, sq[:, kc, :], kc == 0, kc == 15, [sk_, 'cstb'], [pk_])
                TS(P, 'dve', rs[:], pcs, 1.0 / D, EPS, ALU.mult, ALU.add, [pk_], [rk])
                ACT(P, rs[:], rs[:], AF.Sqrt, [rk], [rk])
                P.op('dve', lambda e, rs=rs: e.reciprocal(out=rs[:], in_=rs[:]), [rk], [rk])
                for kc in range(16):
                    STT(P, dst[:, kc, c * 256:(c + 1) * 256], xs[:, kc, :], gmix[:, kc:kc + 1], rs[:],
                        ALU.mult, ALU.mult, [xk, rk, 'gmix'], [(dkey, c // 2, c % 2, kc)])

        w_in3 = w_in.rearrange("(kc p) n -> p kc n", p=128)

        with ExitStack() as ph:
            hTf = sb(ph, "hTf", [128, 16, 4096], BF16)
            pss = ps(ph, "pssA", [128, 512], F32)
            with ExitStack() as ph1:
                normT(ph1, xT_full.rearrange("(kc p) t -> p kc t", p=128), 16, hTf, 'hTf', pss)
                P.barrier()
            wg = [sb(ph, "wg%d" % i, [128, 16, 256], BF16) for i in range(2)]
            wf = sb(ph, "wf", [128, 16, 8], BF16)
            kst = [sb(ph, "kst%d" % i, [128, 512], BF16) for i in range(4)]
            vst = [sb(ph, "vst%d" % i, [128, 256], BF16) for i in range(4)]
            pk = [ps(ph, "pk%d" % i, [128, 512], F32) for i in range(4)]
            pf = ps(ph, "pf", [128, 8], F32)
            flg = sb(ph, "flg", [128, 32, 8], F32)
            gi = 0
            nev = 0
            nk = 0
            for (c0, h0) in [(C_KA + 256 * g, 2 * g) for g in range(4)] + [(C_KB + 256 * g, 8 + 2 * g) for g in range(4)]:
                w = wg[gi % 2]
                wk = 'wg%d' % (gi % 2)
                gi += 1
                DMA(P, 'pool', w[:], w_in3[:, :, c0:c0 + 256], [], [wk])
                for hh in range(2):
                    for c in range(8):
                        pt = pk[nev % 4]
                        pkk = 'pk%d' % (nev % 4)
                        for kc in range(16):
                            MM(P, pt[:], w[:, kc, hh * 128:(hh + 1) * 128], hTf[:, kc, c * 512:(c + 1) * 512],
                               kc == 0, kc == 15, [wk, ('hTf', c)], [pkk])
                        ks = kst[nk % 4]
                        kk = 'kst%d' % (nk % 4)
                        nk += 1
                        CP(P, 'act' if nev % 2 == 0 else 'dve', ks[:], pt[:], [pkk], [kk])
                        nev += 1
                        DMA(P, 'sp', kt_scr[h0 + hh][:, c * 512:(c + 1) * 512], ks[:], [kk], [('kt_scr', h0 + hh, c)])
            nv = 0
            for (c0, h0) in [(C_VA + 256 * g, 2 * g) for g in range(4)] + [(C_VB + 256 * g, 8 + 2 * g) for g in range(4)]:
                w = wg[gi % 2]
                wk = 'wg%d' % (gi % 2)
                gi += 1
                DMA(P, 'pool', w[:], w_in3[:, :, c0:c0 + 256], [], [wk])
                for tb in range(32):
                    pt = pk[nev % 4]
                    pkk = 'pk%d' % (nev % 4)
                    for kc in range(16):
                        MM(P, pt[:, 0:256], hTf[:, kc, tb * 128:(tb + 1) * 128], w[:, kc, :], kc == 0, kc == 15,
                           [wk, ('hTf', tb // 4)], [pkk])
                    vs = vst[nv % 4]
                    vk = 'vst%d' % (nv % 4)
                    nv += 1
                    CP(P, 'act' if nev % 2 == 0 else 'dve', vs[:], pt[:, 0:256], [pkk], [vk])
                    nev += 1
                    DMA(P, 'sp', v_scr[h0:h0 + 2, :, tb, :].rearrange("h p d -> p h d"),
                        vs[:].rearrange("p (h d) -> p h d", h=2), [vk], [('v_scr', h0 // 2, tb)])
            DMA(P, 'pool', wf[:], w_in3[:, :, C_F:C_F + 8], [], ['wf'])
            for tb in range(32):
                for kc in range(16):
                    MM(P, pf[:], hTf[:, kc, tb * 128:(tb + 1) * 128], wf[:, kc, :], kc == 0, kc == 15,
                       ['wf', ('hTf', tb // 4)], ['pf'])
                CP(P, 'dve', flg[:, tb, :], pf[:], ['pf'], [('flg', tb)])
            TT(P, 'dve', flg[:], flg[:], bfor[:].unsqueeze(1).to_broadcast([128, 32, 8]), ALU.add,
               ['flg', 'bfor'], ['flg'])
            ACT(P, flg[:], flg[:], AF.Exp, ['flg'], ['flg'], scale=-1.0)
            ACT(P, flg[:], flg[:], AF.Ln, ['flg'], ['flg'], bias=1.0)
            ppre = pk[0]
            ptot = pk[1]
            flat = flg[:].rearrange("p a b -> p (a b)")
            MM(P, ppre[:, 0:256], triu_f, flat, True, True, ['flg', 'cst'], ['pk0'])
            MM(P, ptot[:, 0:256], ones_f, flat, True, True, ['flg', 'cst'], ['pk1'])
            tot = sb(ph, "tot", [128, 32, 8], F32)
            car = sb(ph, "car", [128, 32, 8], F32)
            CP(P, 'dve', tot[:].rearrange("p a b -> p (a b)"), ptot[:, 0:256], ['pk1'], ['tot'])
            P.op('dve', lambda e: e.memset(car[:, 0, :], 0.0), [], ['car'])
            for bk in range(1, 32):
                TT(P, 'dve', car[:, bk, :], car[:, bk - 1, :], tot[:, bk - 1, :], ALU.add, ['car', 'tot'], ['car'])
            TT(P, 'dve', NFt[:].rearrange("p a b -> p (a b)"), ppre[:, 0:256],
               car[:].rearrange("p a b -> p (a b)"), ALU.add, ['pk0', 'car'], ['NFt'])
            NF4 = NFt[:].rearrange("p (i k) h -> p i k h", k=4)
            TS(P, 'dve', nNFo[:], NF4[:, :, 0, :], sel[:, 0:1], None, ALU.mult, None, ['NFt', 'sel'], ['nNFo'])
            for k in range(1, 4):
                STT(P, nNFo[:], NF4[:, :, k, :], sel[:, k:k + 1], nNFo[:], ALU.mult, ALU.add,
                    ['NFt', 'sel', 'nNFo'], ['nNFo'])
            TS(P, 'dve', nNFo[:], nNFo[:], -1.0, None, ALU.mult, None, ['nNFo'], ['nNFo'])
            if DEBUG:
                DMA(P, 'sp', dbg['NF'], NFt[:], ['NFt'], ['dbgNF'])
            P.barrier()

        bc = ExitStack()
        hTo = sb(bc, "hTo", [128, 16, 1024], BF16)
        OT = sb(bc, "OT", [128, 16, 1024], BF16)
        if STAGE >= 2:
            with ExitStack() as ph:
                pss = ps(ph, "pssB", [128, 512], F32)
                with ExitStack() as ph1:
                    normT(ph1, xT_own.rearrange("(kc p) t -> p kc t", p=128), 4, hTo, 'hTo', pss)
                    P.barrier()
                KTs = [sb(ph, "KT%d" % i, [128, 4096], BF16) for i in range(2)]
                VH_ = sb(ph, "VH", [128, 32, 128], BF16)
                VHs = [VH_, VH_]
                wq = sb(ph, "wq", [128, 16, 128], BF16)
                QH = sb(ph, "QH", [128, 1024], BF16)
                Zs = [sb(ph, "Z%d" % i, [128, 4096], F32) for i in range(2)]
                T1 = sb(ph, "T1", [128, 4096], F32)
                PP = sb(ph, "PP", [128, 4096], F32)
                NFbc = sb(ph, "NFbc", [128, 4096], F32)
                Ws = [sb(ph, "W%d" % i, [128, 4096], BF16) for i in range(2)]
                negt = sb(ph, "negt", [128, 4], F32)
                lpart = sb(ph, "lpart", [128, 2, 8], F32)
                lsum = sb(ph, "lsum", [128, 4], F32)
                WT = [sb(ph, "WT%d" % i, [128, 512], BF16) for i in range(2)]
                Ob = sb(ph, "Ob", [128, 128], BF16)
                small = sb(ph, "small", [128, 8], F32)
                zp = [ps(ph, "zp%d" % i, [128, 512], F32) for i in range(2)]
                tp = [ps(ph, "tp%d" % i, [128, 512], BF16) for i in range(2)]
                op_ = ps(ph, "op", [128, 128], F32)
                otp = ps(ph, "otp", [128, 128], BF16)
                scale = 1.0 / math.sqrt(128.0)
                def load_k(hd):
                    DMA(P, 'sp', KTs[hd % 2][:], kt_scr[hd], [('kt_scr', hd)], ['KT%d' % (hd % 2)])

                def load_v(hd):
                    DMA(P, 'sp', VH_[:], v_scr[hd], [('v_scr', hd // 2)], ['VH'])

                def csl(c):
                    return slice(512 * c, 512 * c + 512)

                cnt = {'nz': 0, 'ntp': 0}

                def prologue(hd):
                    is_sb = hd < 8
                    if hd + 1 < 16:
                        load_k(hd + 1)
                    qc = C_QA + hd * 128 if is_sb else C_QB + (hd - 8) * 128
                    DMA(P, 'pool', wq[:], w_in3[:, :, qc:qc + 128], [], ['wq'])
                    src_t, c0_ = (exp_u, 0) if hd < 8 else (exp_v, D)
                    r0 = (hd % 8) * 2048
                    DMA(P, 'pool', uv_bf[r0:r0 + 2048, c0_:c0_ + D], src_t[r0:r0 + 2048, :], [], [('uv_bf', hd)])
                    for half in range(2):
                        for kc in range(16):
                            MM(P, pss[:], wq[:, kc, :], hTo[:, kc, half * 512:(half + 1) * 512], kc == 0, kc == 15,
                               ['wq', 'hTo'], ['pss'])
                        ACT(P, QH[:, half * 512:(half + 1) * 512], pss[:], AF.Copy, ['pss'], ['QH'], scale=scale)
                    if not is_sb:
                        h = hd - 8
                        dg = T1[:].rearrange("p (a b) -> p a b", b=128)
                        TT(P, 'dve', dg, ident_f.unsqueeze(1).to_broadcast([128, 32, 128]),
                           NFt[:, :, h:h + 1].to_broadcast([128, 32, 128]), ALU.mult, ['cst', 'NFt'], ['T1'])
                        for c in range(8):
                            z = zp[cnt['nz'] % 2]
                            zk = 'zp%d' % (cnt['nz'] % 2)
                            cnt['nz'] += 1
                            MM(P, z[:], ones_f, T1[:, csl(c)], True, True, ['cst', 'T1'], [zk])
                            CP(P, 'act', NFbc[:, csl(c)], z[:], [zk], [('NFbc', c)])

                def stage_q(r, hd, i):
                    is_sb = hd < 8
                    nch = i + 1
                    KT, ktk = KTs[hd % 2], 'KT%d' % (hd % 2)
                    Z, zn = Zs[r % 2], 'Z%d' % (r % 2)
                    for c in range(nch):
                        z = zp[cnt['nz'] % 2]
                        zk = 'zp%d' % (cnt['nz'] % 2)
                        cnt['nz'] += 1
                        MM(P, z[:], QH[:, i * 128:(i + 1) * 128], KT[:, csl(c)], True, True, ['QH', ktk], [zk])
                        if is_sb:
                            CP(P, 'act', Z[:, csl(c)], z[:], [zk], [(zn, c)])
                        else:
                            TT(P, 'dve', Z[:, csl(c)], z[:], NFbc[:, csl(c)], ALU.add, [zk, ('NFbc', c)], [(zn, c)])

                def stage_e(r, hd, i):
                    is_sb = hd < 8
                    nch = i + 1
                    L = 512 * nch
                    Z, zn = Zs[r % 2], 'Z%d' % (r % 2)
                    W, wn = Ws[r % 2], 'W%d' % (r % 2)
                    ngc = r % 4
                    lp = lpart[:, r % 2, :]
                    lpk = ('lpart', r % 2)
                    if is_sb:
                        for c in range(nch):
                            ACT(P, T1[:, csl(c)], Z[:, csl(c)], AF.Exp, [(zn, c)], [('T1', c)])
                        for c in range(nch):
                            ACT(P, T1[:, csl(c)], T1[:, csl(c)], AF.Ln, [('T1', c)], [('T1', c)], bias=1.0)
                        TT(P, 'pool', T1[:, csl(i)], T1[:, csl(i)], sb01, ALU.mult, [('T1', i), 'msk'], [('T1', i)])
                        for c in range(nch):
                            init = 0.0 if c == 0 else PP[:, 512 * c - 1:512 * c]
                            o_ap, d0_ap, d1_ap = PP[:, csl(c)], ones_f[:, 0:1].to_broadcast([128, 512]), T1[:, csl(c)]
                            P.op('dve', lambda e, o_ap=o_ap, d0_ap=d0_ap, d1_ap=d1_ap, init=init: e.tensor_tensor_scan(
                                out=o_ap, data0=d0_ap, data1=d1_ap, initial=init, op0=ALU.mult, op1=ALU.add),
                                [('T1', c), 'cst'] + ([('PP', c - 1)] if c else []), [('PP', c)])
                        TS(P, 'dve', negt[:, ngc:ngc + 1], PP[:, L - 1:L], -1.0, None, ALU.mult, None,
                           [('PP', nch - 1)], [('negt', ngc)])
                        for c in range(nch):
                            if c == 0:
                                TT(P, 'dve', Z[:, 1:512], Z[:, 1:512], PP[:, 0:511], ALU.add,
                                   [(zn, 0), ('PP', 0)], [(zn, 0)])
                            else:
                                TT(P, 'dve', Z[:, csl(c)], Z[:, csl(c)], PP[:, 512 * c - 1:512 * c + 511], ALU.add,
                                   [(zn, c), ('PP', c), ('PP', c - 1)], [(zn, c)])
                        TT(P, 'pool', Z[:, csl(i)], Z[:, csl(i)], sbadd, ALU.add, [(zn, i), 'msk'], [(zn, i)])
                    else:
                        TT(P, 'pool', Z[:, csl(i)], Z[:, csl(i)], fxadd, ALU.add, [(zn, i), 'msk'], [(zn, i)])

                def stage_e2(r, hd, i):
                    is_sb = hd < 8
                    nch = i + 1
                    Z, zn = Zs[r % 2], 'Z%d' % (r % 2)
                    W, wn = Ws[r % 2], 'W%d' % (r % 2)
                    ngc = r % 4
                    lp = lpart[:, r % 2, :]
                    lpk = ('lpart', r % 2)
                    if is_sb:
                        for c in range(nch):
                            ACT(P, W[:, csl(c)], Z[:, csl(c)], AF.Exp, [(zn, c), ('negt', ngc)], [(wn, c)],
                                bias=negt[:, ngc:ngc + 1])
                    else:
                        for c in range(nch):
                            ACT(P, W[:, csl(c)], Z[:, csl(c)], AF.Exp, [(zn, c), 'nNFo'], [(wn, c), lpk + (c,)],
                                bias=nNFo[:, i, hd - 8:hd - 7], accum_out=lp[:, c:c + 1])
                        P.op('dve', lambda e, lp=lp, nch=nch, ngc=ngc: e.reduce_sum(
                            out=lsum[:, ngc:ngc + 1], in_=lp[:, 0:nch], axis=AX.X), [lpk], [('lsum', ngc)])
                        P.op('dve', lambda e, ngc=ngc: e.reciprocal(out=lsum[:, ngc:ngc + 1], in_=lsum[:, ngc:ngc + 1]),
                             [('lsum', ngc)], [('lsum', ngc)])

                def stage_b(r, hd, i):
                    is_sb = hd < 8
                    nch = i + 1
                    W, wn = Ws[r % 2], 'W%d' % (r % 2)
                    ngc = r % 4
                    nkb = 4 * nch
                    slots = []

                    def tr_group(g):
                        t = tp[cnt['ntp'] % 2]
                        tk = 'tp%d' % (cnt['ntp'] % 2)
                        wt = WT[cnt['ntp'] % 2]
                        wtk = 'WT%d' % (cnt['ntp'] % 2)
                        cnt['ntp'] += 1
                        for q in range(4):
                            kb = 4 * g + q
                            TR(P, t[:, q * 128:(q + 1) * 128], W[:, kb * 128:(kb + 1) * 128], ident_b,
                               [(wn, g), 'cstb'], [tk])
                        CP(P, 'dve', wt[:], t[:], [tk], [wtk])
                        slots.append((wt, wtk))

                    def pv_group(g):
                        wt, wtk = slots[g]
                        for q in range(4):
                            kb = 4 * g + q
                            MM(P, op_[:], wt[:, q * 128:(q + 1) * 128], VH_[:, kb, :], kb == 0, kb == nkb - 1,
                               [wtk, 'VH'], ['op'])
                    tr_group(0)
                    for g in range(nch):
                        if g + 1 < nch:
                            tr_group(g + 1)
                        pv_group(g)
                    if is_sb:
                        CP(P, 'dve', Ob[:], op_[:], ['op'], ['Ob'])
                    else:
                        TS(P, 'dve', Ob[:], op_[:], lsum[:, ngc:ngc + 1], None, ALU.mult, None,
                           ['op', ('lsum', ngc)], ['Ob'])
                    TR(P, otp[:], Ob[:], ident_b, ['Ob', 'cstb'], ['otp'])
                    CP(P, 'dve', OT[:, hd, i * 128:(i + 1) * 128], otp[:], ['otp'], [('OT', hd)])

                rows = [(hd, i) for hd in range(16) for i in range(8)]
                nr = len(rows)
                load_k(0)
                for it in range(nr + 2):
                    if 0 <= it - 1 < nr:
                        hd, i = rows[it - 1]
                        stage_e(it - 1, hd, i)
                    if it < nr:
                        hd, i = rows[it]
                        if i == 0:
                            prologue(hd)
                        stage_q(it, hd, i)
                    if 0 <= it - 2 < nr:
                        hd, i = rows[it - 2]
                        if i == 0:
                            load_v(hd)
                        stage_b(it - 2, hd, i)
                    if 0 <= it - 1 < nr:
                        hd, i = rows[it - 1]
                        stage_e2(it - 1, hd, i)
                if DEBUG:
                    DMA(P, 'sp', dbg['OT'], OT[:], ['OT'], ['dbgOT'])
                P.barrier()

        if STAGE >= 3:
            with ExitStack() as ph:
                mT = sb(ph, "mT", [128, 16, 1024], BF16)
                wga = sb(ph, "wga", [128, 16, 512], BF16)
                wgb = sb(ph, "wgb", [128, 16, 512], BF16)
                wa = sb(ph, "wa", [128, 8, 512], BF16)
                wb = sb(ph, "wb", [128, 8, 512], BF16)
                sga = sb(ph, "sga", [128, 512], F32)
                sgb = sb(ph, "sgb", [128, 512], F32)
                m1 = sb(ph, "m1", [128, 512], F32)
                m2 = sb(ph, "m2", [128, 512], F32)
                pc = [ps(ph, "pc%d" % i, [128, 512], F32) for i in range(8)]
                w_ba3 = w_ba.rearrange("(kc p) n -> p kc n", p=128)
                w_bb3 = w_bb.rearrange("(kc p) n -> p kc n", p=128)
                w_out3 = w_out.rearrange("(kc p) n -> p kc n", p=128)
                xo = [sb(ph, "xo%d" % i, [128, D], F32) for i in range(2)]
                it = 0
                for ng in range(4):
                    DMA(P, 'pool', wga[:], w_in3[:, :, C_GA + ng * 512:C_GA + (ng + 1) * 512], [], ['wga'])
                    DMA(P, 'pool', wgb[:], w_in3[:, :, C_GB + ng * 512:C_GB + (ng + 1) * 512], [], ['wgb'])
                    DMA(P, 'pool', wa[:], w_ba3[:, :, ng * 512:(ng + 1) * 512], [], ['wa'])
                    DMA(P, 'pool', wb[:], w_bb3[:, :, ng * 512:(ng + 1) * 512], [], ['wb'])
                    for nt in range(4):
                        ns = slice(nt * 128, (nt + 1) * 128)
                        for half in range(2):
                            hs = slice(half * 512, (half + 1) * 512)
                            b0 = 4 * (it % 2)
                            it += 1
                            pga, pgb, pya, pyb = pc[b0], pc[b0 + 1], pc[b0 + 2], pc[b0 + 3]
                            kga, kgb, kya, kyb = ['pc%d' % (b0 + x) for x in range(4)]
                            for kc in range(16):
                                MM(P, pga[:], wga[:, kc, ns], hTo[:, kc, hs], kc == 0, kc == 15, ['wga', 'hTo'], [kga])
                            for kc in range(16):
                                MM(P, pgb[:], wgb[:, kc, ns], hTo[:, kc, hs], kc == 0, kc == 15, ['wgb', 'hTo'], [kgb])
                            for kc in range(8):
                                MM(P, pya[:], wa[:, kc, ns], OT[:, kc, hs], kc == 0, kc == 7, ['wa', 'OT'], [kya])
                            for kc in range(8):
                                MM(P, pyb[:], wb[:, kc, ns], OT[:, 8 + kc, hs], kc == 0, kc == 7, ['wb', 'OT'], [kyb])
                            ACT(P, sga[:], pga[:], AF.Sigmoid, [kga], ['sga'])
                            ACT(P, sgb[:], pgb[:], AF.Sigmoid, [kgb], ['sgb'])
                            TT(P, 'dve', m1[:], sga[:], pya[:], ALU.mult, ['sga', kya], ['m1'])
                            TT(P, 'dve', m2[:], sgb[:], pyb[:], ALU.mult, ['sgb', kyb], ['m2'])
                            TT(P, 'pool', mT[:, ng * 4 + nt, hs], m1[:], m2[:], ALU.add, ['m1', 'm2'], ['mT'])
                wo = sb(ph, "wo", [128, 16, 512], BF16)
                it = 0
                for ng in range(4):
                    DMA(P, 'pool', wo[:], w_out3[:, :, ng * 512:(ng + 1) * 512], [], ['wo'])
                    for tb in range(8):
                        pt = pc[it % 4]
                        pk_ = 'pc%d' % (it % 4)
                        xb_ = xo[it % 2]
                        xk = 'xo%d' % (it % 2)
                        it += 1
                        cs = slice(ng * 512, (ng + 1) * 512)
                        DMA(P, 'sp', xb_[:, 0:512], x_own[tb * 128:(tb + 1) * 128, cs], [], [xk])
                        for kc in range(16):
                            MM(P, pt[:], mT[:, kc, tb * 128:(tb + 1) * 128], wo[:, kc, :], kc == 0, kc == 15,
                               ['mT', 'wo'], [pk_])
                        TT(P, 'dve', xb_[:, 0:512], xb_[:, 0:512], pt[:], ALU.add, [xk, pk_], [xk])
                        DMA(P, 'sp', x1_scr[tb][:, cs], xb_[:, 0:512], [xk], [('x1s', tb, ng)])
                        if DEBUG:
                            DMA(P, 'sp', dbg['x1'][tb][:, cs], xb_[:, 0:512], [xk], [('dbgx1', tb, ng)])
                P.barrier()

        bc.close()
        if STAGE >= 4:
            with ExitStack() as ph:
                gffn = sb(ph, "gffn", [128, D], F32)
                gfin = sb(ph, "gfin", [128, D], F32)
                skb = sb(ph, "skb", [128, 16, 128], BF16)
                rstd2 = sb(ph, "rstd2", [128, 8], F32)
                DMA(P, 'sp', gffn[:], gffn_in[:, :], [], ['gffn'])
                DMA(P, 'sp', gfin[:], gfin_in[:, :], [], ['gfin'])
                DMA(P, 'pool', skb[:], skT.rearrange("ch c n -> c ch n"), [], ['skb'])
                pd = [ps(ph, "pd%d" % i, [128, 512], F32) for i in range(4)]
                jb = sb(ph, "jb", [128, D], BF16)
                x1bs = [sb(ph, "x1b%d" % i, [128, D], F32) for i in range(2)]
                x1b = x1bs[0]
                with ExitStack() as ph1:
                    pdb = [ps(ph1, "pdb%d" % i, [128, 512], BF16) for i in range(2)]
                    h2T = sb(ph1, "h2T", [128, 16, 1024], BF16)
                    qT = sb(ph1, "qT", [128, 16, 1024], BF16)
                    h2b = sb(ph1, "h2b", [128, D], BF16)
                    wqp = sb(ph1, "wqp", [128, 16, 512], BF16)
                    w_q3 = w_query.rearrange("(kc p) n -> p kc n", p=128)
                    ntp = 0
                    for tb in range(8):
                        DMA(P, 'sp', x1b[:], x1_scr[tb], [('x1s', tb)], ['x1b'])
                        ACT(P, jb[:], x1b[:], AF.Square, ['x1b'], ['jb', ('rstd2', tb)],
                            accum_out=rstd2[:, tb:tb + 1])
                        TS(P, 'dve', rstd2[:, tb:tb + 1], rstd2[:, tb:tb + 1], 1.0 / D, EPS, ALU.mult, ALU.add,
                           [('rstd2', tb)], [('rstd2', tb)])
                        ACT(P, rstd2[:, tb:tb + 1], rstd2[:, tb:tb + 1], AF.Sqrt, [('rstd2', tb)], [('rstd2', tb)])
                        P.op('dve', lambda e, tb=tb: e.reciprocal(out=rstd2[:, tb:tb + 1], in_=rstd2[:, tb:tb + 1]),
                             [('rstd2', tb)], [('rstd2', tb)])
                        STT(P, h2b[:], x1b[:], rstd2[:, tb:tb + 1], gffn[:], ALU.mult, ALU.mult,
                            ['x1b', ('rstd2', tb), 'gffn'], ['h2b'])
                        for g in range(4):
                            t = pdb[ntp % 2]
                            tk = 'pdb%d' % (ntp % 2)
                            ntp += 1
                            for q in range(4):
                                kc = 4 * g + q
                                TR(P, t[:, q * 128:(q + 1) * 128], h2b[:, kc * 128:(kc + 1) * 128], ident_b,
                                   ['h2b', 'cstb'], [tk])
                            CP(P, 'act', h2T[:, 4 * g:4 * g + 4, tb * 128:(tb + 1) * 128],
                               t[:].rearrange("p (a b) -> p a b", a=4), [tk], ['h2T'])
                    it = 0
                    for ng in range(4):
                        DMA(P, 'pool', wqp[:], w_q3[:, :, ng * 512:(ng + 1) * 512], [], ['wqp'])
                        for nt in range(4):
                            for half in range(2):
                                pt = pd[it % 4]
                                pk_ = 'pd%d' % (it % 4)
                                it += 1
                                for kc in range(16):
                                    MM(P, pt[:], wqp[:, kc, nt * 128:(nt + 1) * 128],
                                       h2T[:, kc, half * 512:(half + 1) * 512], kc == 0, kc == 15, ['wqp', 'h2T'], [pk_])
                                CP(P, 'act' if it % 2 else 'dve', qT[:, ng * 4 + nt, half * 512:(half + 1) * 512], pt[:],
                                   [pk_], ['qT'])
                    DMA(P, 'sp', q_scr, qT[:], ['qT'], ['q_scr'])
                    P.barrier()
                po = [ps(ph, "po%d" % i, [128, 512], F32) for i in range(4)]
                sc = sb(ph, "sc", [128, 16, 128], F32)
                tmp = sb(ph, "tmp", [128, 256], F32)
                v16 = sb(ph, "v16", [128, 8, 2, 16], F32)
                ix = sb(ph, "ix", [128, 8, 2, 16], U32)
                ixf = sb(ph, "ixf", [128, 8, 2, 16], F32)
                cand = sb(ph, "cand", [128, 8, 16, 16], F32)
                cid = sb(ph, "cid", [128, 8, 16, 16], F32)
                t16 = sb(ph, "t16", [128, 8, 16], F32)
                pos = sb(ph, "pos", [128, 8, 16], U32)
                posf = sb(ph, "posf", [128, 8, 16], F32)
                iot = sb(ph, "iot", [128, 256], F32)
                DMA(P, 'sp', iot[:], iota_in[:, :], [], ['iot'])
                idf = sb(ph, "idf", [128, 128], F32)
                jk2 = sb(ph, "jk2", [128, 256], F32)
                idus = [sb(ph, "idu%d" % i, [128, 128], U32) for i in range(2)]
                gts = [sb(ph, "gt%d" % i, [128, 8, 16], F32) for i in range(2)]
                gs = sb(ph, "gs", [128, 8], F32)
                aa = sb(ph, "aa", [128, 128], F32)
                ww = sb(ph, "ww", [128, 128], F32)
                g1 = sb(ph, "g1", [128, 128], F32)
                h2f = sb(ph, "h2f", [128, D], F32)
                qTbs = [sb(ph, "qTb%d" % i, [128, 16, 128], BF16) for i in range(2)]
                ssf = sb(ph, "ssf", [128, 2], F32)
                GSZ = 8
                dgs = [sb(ph, "dg%d" % i, [128, GSZ, 128], BF16) for i in range(2)]
                NG_, NVR = 5, 14
                gbs = [sb(ph, "gb%d" % i, [128, 2 * D], BF16) for i in range(NG_)]
                vrs = [sb(ph, "vr%d" % i, [128, D], BF16) for i in range(NVR)]
                ngb = [0]

                def topk(tb):
                    idu = idus[tb % 2]
                    ik = 'idu%d' % (tb % 2)
                    gt = gts[tb % 2]
                    gk = 'gt%d' % (tb % 2)
                    qTb = qTbs[tb % 2]
                    qk = 'qTb%d' % (tb % 2)
                    DMA(P, 'sp', qTb[:], q_scr[:, :, tb * 128:(tb + 1) * 128], ['q_scr'], [qk])
                    for g in range(4):
                        for q in range(4):
                            ch = 4 * g + q
                            MM(P, pd[g][:, q * 128:(q + 1) * 128], qTb[:, ch, :], skb[:, ch, :],
                               True, True, [qk, 'skb'], ['pd%d' % g])
                        CP(P, 'act', sc[:, 4 * g:4 * g + 4, :], pd[g][:].rearrange("p (a b) -> p a b", a=4),
                           ['pd%d' % g], ['sc'])
                    for ch in range(16):
                        hh, pp = ch // 2, ch % 2
                        P.op('dve', lambda e, ch=ch, hh=hh, pp=pp: e.max(out=v16[:, hh, pp, 0:8], in_=sc[:, ch, :]),
                             ['sc'], ['v16'])
                        P.op('dve', lambda e, ch=ch, hh=hh, pp=pp: e.max_index(
                            out=ix[:, hh, pp, 0:8], in_max=v16[:, hh, pp, 0:8], in_values=sc[:, ch, :]),
                            ['sc', 'v16'], ['ix'])
                        P.op('dve', lambda e, ch=ch, hh=hh, pp=pp: e.match_replace(
                            out=tmp[:, 0:128], in_to_replace=v16[:, hh, pp, 0:8], in_values=sc[:, ch, :],
                            imm_value=-1e30), ['sc', 'v16'], ['tmp'])
                        P.op('dve', lambda e, ch=ch, hh=hh, pp=pp: e.max(out=v16[:, hh, pp, 8:16], in_=tmp[:, 0:128]),
                             ['tmp'], ['v16'])
                        P.op('dve', lambda e, ch=ch, hh=hh, pp=pp: e.max_index(
                            out=ix[:, hh, pp, 8:16], in_max=v16[:, hh, pp, 8:16], in_values=tmp[:, 0:128]),
                            ['tmp', 'v16'], ['ix'])
                    CP(P, 'dve', ixf[:], ix[:], ['ix'], ['ixf'])
                    TS(P, 'dve', ixf[:, :, 0, :], ixf[:, :, 0, :], 128.0, None, ALU.mult, None, ['ixf'], ['ixf'])
                    TT(P, 'dve', cand[:], v16[:, :, 0, :].unsqueeze(3).to_broadcast([128, 8, 16, 16]),
                       v16[:, :, 1, :].unsqueeze(2).to_broadcast([128, 8, 16, 16]), ALU.add, ['v16'], ['cand'])
                    TT(P, 'dve', cid[:], ixf[:, :, 0, :].unsqueeze(3).to_broadcast([128, 8, 16, 16]),
                       ixf[:, :, 1, :].unsqueeze(2).to_broadcast([128, 8, 16, 16]), ALU.add, ['ixf'], ['cid'])
                    for hh in range(8):
                        cf = cand[:, hh].rearrange("p a b -> p (a b)")
                        P.op('dve', lambda e, hh=hh, cf=cf: e.max(out=t16[:, hh, 0:8], in_=cf), ['cand'], ['t16'])
                        P.op('dve', lambda e, hh=hh, cf=cf: e.max_index(
                            out=pos[:, hh, 0:8], in_max=t16[:, hh, 0:8], in_values=cf), ['cand', 't16'], ['pos'])
                        P.op('dve', lambda e, hh=hh, cf=cf: e.match_replace(
                            out=tmp[:], in_to_replace=t16[:, hh, 0:8], in_values=cf, imm_value=-1e30),
                            ['cand', 't16'], ['tmp'])
                        P.op('dve', lambda e, hh=hh: e.max(out=t16[:, hh, 8:16], in_=tmp[:]), ['tmp'], ['t16'])
                        P.op('dve', lambda e, hh=hh: e.max_index(
                            out=pos[:, hh, 8:16], in_max=t16[:, hh, 8:16], in_values=tmp[:]), ['tmp', 't16'], ['pos'])
                    CP(P, 'dve', posf[:], pos[:], ['pos'], ['posf'])
                    for hh in range(8):
                        cidf = cid[:, hh].rearrange("p a b -> p (a b)")
                        for k in range(16):
                            STT(P, jk2[:], iot[:], posf[:, hh, k:k + 1], cidf, ALU.is_equal, ALU.mult,
                                ['iot', 'cid', 'posf'], [('idf', hh * 16 + k)],
                                accum_out=idf[:, hh * 16 + k:hh * 16 + k + 1])
                    CP(P, 'dve', idu[:], idf[:], ['idf'], [ik])
                    if DEBUG:
                        DMA(P, 'sp', dbg['ids'][:, tb, :], idf[:], ['idf'], [('dbgids', tb)])
                    TT(P, 'dve', gt[:], t16[:], t16[:, :, 0:1].to_broadcast([128, 8, 16]), ALU.subtract, ['t16'], [gk])
                    ACT(P, gt[:], gt[:], AF.Exp, [gk], [gk])
                    P.op('dve', lambda e, gt=gt: e.reduce_sum(out=gs[:], in_=gt[:], axis=AX.X), [gk], ['gs'])
                    P.op('dve', lambda e: e.reciprocal(out=gs[:], in_=gs[:]), ['gs'], ['gs'])
                    TT(P, 'dve', gt[:], gt[:], gs[:].unsqueeze(2).to_broadcast([128, 8, 16]), ALU.mult, [gk, 'gs'], [gk])

                def gather(idu, ik, hk):
                    k = ngb[0]
                    ngb[0] += 1
                    bfr, bk = gbs[k % NG_], 'gb%d' % (k % NG_)
                    vr, vk = vrs[k % NVR], 'vr%d' % (k % NVR)
                    P.dma('pool', lambda e, bfr=bfr, hk=hk: e.indirect_dma_start(
                        out=bfr[:, :], out_offset=None, in_=uv_bf[:, :],
                        in_offset=bass.IndirectOffsetOnAxis(ap=idu[:, hk:hk + 1], axis=0)), [ik, 'uv_bf'], [bk])
                    CP(P, 'act', vr[:], bfr[:, D:2 * D], [bk], [vk])
                    return bfr[:, 0:D], bk, vr, vk

                def u_prep(tb):
                    x1b = x1bs[tb % 2]
                    xk = 'x1b%d' % (tb % 2)
                    DMA(P, 'sp', x1b[:], x1_scr[tb], [('x1s', tb)], [xk])
                    STT(P, h2f[:], x1b[:], rstd2[:, tb:tb + 1], gffn[:], ALU.mult, ALU.mult,
                        [xk, 'rstd2', 'gffn'], ['h2f'])

                ngrp = [0]

                def grp_dots(tb, g, j0, j1, st):
                    idu = idus[tb % 2]
                    ik = 'idu%d' % (tb % 2)
                    for j in range(j0, j1):
                        hk = g * GSZ + j
                        ub, uk, vb, vk = gather(idu, ik, hk)
                        st['v'].append((vb, vk))
                        STT(P, jb[:], ub, 1.0, h2f[:], ALU.mult, ALU.mult, [uk, 'h2f'], [('aa', hk)],
                            accum_out=aa[:, hk:hk + 1])

                def grp_gelu_a(tb, g):
                    hs = slice(g * GSZ, (g + 1) * GSZ)
                    ak = [('aa', g * GSZ + j) for j in range(GSZ)]
                    g1k = ('g1', g % 2)
                    TT(P, 'dve', g1[:, hs], aa[:, hs], aa[:, hs], ALU.mult, ak, [g1k])
                    TS(P, 'dve', g1[:, hs], g1[:, hs], 0.044715, 1.0, ALU.mult, ALU.add, [g1k], [g1k])
                    TT(P, 'dve', g1[:, hs], g1[:, hs], aa[:, hs], ALU.mult, [g1k] + ak, [g1k])
                    ACT(P, g1[:, hs], g1[:, hs], AF.Sigmoid, [g1k], [g1k], scale=2.0 * math.sqrt(2.0 / math.pi))

                def grp_finish(tb, g, st):
                    gt = gts[tb % 2]
                    gk = 'gt%d' % (tb % 2)
                    dg = dgs[ngrp[0] % 2]
                    dk = 'dg%d' % (ngrp[0] % 2)
                    ngrp[0] += 1
                    hs = slice(g * GSZ, (g + 1) * GSZ)
                    ak = [('aa', g * GSZ + j) for j in range(GSZ)]
                    g1k = ('g1', g % 2)
                    TT(P, 'dve', g1[:, hs], g1[:, hs], aa[:, hs], ALU.mult, [g1k] + ak, [g1k])
                    TT(P, 'dve', ww[:, hs], g1[:, hs], gt[:].rearrange("p a b -> p (a b)")[:, hs], ALU.mult,
                       [g1k, gk], [('ww', g % 2)])
                    TT(P, 'dve', dg[:], ident_b.unsqueeze(1).to_broadcast([128, GSZ, 128]),
                       ww[:, hs].unsqueeze(2).to_broadcast([128, GSZ, 128]), ALU.mult, ['cstb', ('ww', g % 2)], [dk])
                    for j in range(GSZ):
                        hk = g * GSZ + j
                        vb, vk = st['v'][j]
                        for c in range(4):
                            MM(P, po[c][:], dg[:, j, :], vb[:, c * 512:(c + 1) * 512], hk == 0, hk == 127,
                               [dk, vk], ['po%d' % c])

                def final(tb):
                    x1b = x1bs[tb % 2]
                    xk = 'x1b%d' % (tb % 2)
                    if DEBUG:
                        DMA(P, 'sp', dbg['gw'][:, tb, :], ww[:], ['ww'], [('dbggw', tb)])
                    for c in range(4):
                        TT(P, 'dve', x1b[:, c * 512:(c + 1) * 512], x1b[:, c * 512:(c + 1) * 512], po[c][:], ALU.add,
                           [xk, 'po%d' % c], [xk])
                    if DEBUG:
                        DMA(P, 'sp', dbg['acc'][tb], x1b[:], [xk], [('dbgacc', tb)])
                    ACT(P, jb[:], x1b[:], AF.Square, [xk], ['ssf'], accum_out=ssf[:, 0:1])
                    TS(P, 'dve', ssf[:, 0:1], ssf[:, 0:1], 1.0 / D, EPS, ALU.mult, ALU.add, ['ssf'], ['ssf'])
                    ACT(P, ssf[:, 0:1], ssf[:, 0:1], AF.Sqrt, ['ssf'], ['ssf'])
                    P.op('dve', lambda e: e.reciprocal(out=ssf[:, 0:1], in_=ssf[:, 0:1]), ['ssf'], ['ssf'])
                    STT(P, x1b[:], x1b[:], ssf[:, 0:1], gfin[:], ALU.mult, ALU.mult, [xk, 'ssf', 'gfin'], [xk])
                    DMA(P, 'sp', out[tb * 128:(tb + 1) * 128, :], x1b[:], [xk], [('out', tb)])

                topk(0)
                for tb in range(8):
                    u_prep(tb)
                    NG = 128 // GSZ
                    prev = None
                    for g in range(NG):
                        st_ = {'v': []}
                        grp_dots(tb, g, 0, GSZ // 2, st_)
                        if prev is not None:
                            grp_finish(tb, g - 1, prev)
                        grp_dots(tb, g, GSZ // 2, GSZ, st_)
                        grp_gelu_a(tb, g)
                        prev = st_
                        if g == 7 and tb + 1 < 8:
                            topk(tb + 1)
                    grp_finish(tb, NG - 1, prev)
                    final(tb)
                P.barrier()
        P.barrier()
        P.emit()
    return nc


def make_core_inputs(inputs):
    x = np.asarray(inputs["x"], dtype=np.float32)
    w_in = np.ascontiguousarray(np.asarray(inputs["w_in"], dtype=np.float32)[0])
    w_ba = np.ascontiguousarray(np.asarray(inputs["w_branch_a"], dtype=np.float32)[0])
    w_bb = np.ascontiguousarray(np.asarray(inputs["w_branch_b"], dtype=np.float32)[0])
    w_out = np.ascontiguousarray(np.asarray(inputs["w_out"], dtype=np.float32)[0])
    w_query = np.ascontiguousarray(np.asarray(inputs["w_query"], dtype=np.float32)[0])
    sk = np.asarray(inputs["sub_keys"], dtype=np.float32)[0]
    skT = np.ascontiguousarray(sk.transpose(0, 1, 3, 2).reshape(16, 128, 128))
    exp_u = np.ascontiguousarray(np.asarray(inputs["expert_u"], dtype=np.float32)[0])
    exp_v = np.ascontiguousarray(np.asarray(inputs["expert_v"], dtype=np.float32)[0])
    gmix = np.ascontiguousarray(np.asarray(inputs["norm_mix_gain"], dtype=np.float32)[0].reshape(16, 128).T)
    gffn = np.ascontiguousarray(np.broadcast_to(np.asarray(inputs["norm_ffn_gain"], dtype=np.float32)[0][None, :], (128, D)))
    gfin = np.ascontiguousarray(np.broadcast_to(np.asarray(inputs["norm_final_gain"], dtype=np.float32)[None, :], (128, D)))
    bfor = np.ascontiguousarray(np.broadcast_to(np.asarray(inputs["b_forget"], dtype=np.float32)[0][None, :], (128, 8)))
    idx = np.arange(128)
    ident = np.eye(128, dtype=np.float32)
    triu = (idx[:, None] <= idx[None, :]).astype(np.float32)
    cst = np.ascontiguousarray(np.concatenate([ident, triu, np.ones((128, 128), np.float32)], axis=1))
    xT = [np.ascontiguousarray(x[b].T) for b in range(2)]
    iota = np.ascontiguousarray(np.broadcast_to(np.arange(256, dtype=np.float32)[None, :], (128, 256)))
    maps = []
    toks = []
    for c in range(8):
        b, j = c // 4, c % 4
        tok = np.concatenate([np.arange((4 * i + j) * 128, (4 * i + j + 1) * 128) for i in range(8)])
        toks.append((b, tok))
        sb01 = np.zeros((128, 4, 128), np.float32)
        fx01 = np.zeros((128, 4, 128), np.float32)
        for k in range(4):
            if k < j:
                sb01[:, k, :] = 1.0
                fx01[:, k, :] = 1.0
            elif k == j:
                sb01[:, k, :] = (idx[None, :] < idx[:, None])
                fx01[:, k, :] = (idx[None, :] <= idx[:, None])
        sbadd = (1.0 - sb01) * NEG
        fxadd = (1.0 - fx01) * NEG
        msk = np.ascontiguousarray(np.concatenate(
            [sb01.reshape(128, 512), sbadd.reshape(128, 512), fxadd.reshape(128, 512), np.zeros((128, 512), np.float32)],
            axis=1).astype(np.float32))
        sel = np.zeros((128, 4), np.float32)
        sel[:, j] = 1.0
        maps.append({
            "xT_full": xT[b], "xT_own": np.ascontiguousarray(xT[b][:, tok]), "x_own": np.ascontiguousarray(x[b][tok]),
            "w_in": w_in, "w_ba": w_ba, "w_bb": w_bb, "w_out": w_out, "w_query": w_query, "skT": skT,
            "exp_u": exp_u, "exp_v": exp_v, "gmix": gmix, "gffn": gffn, "gfin": gfin, "bfor": bfor,
            "cst": cst, "msk": msk, "sel": sel, "iota": iota,
        })
    return maps, toks


def kernel(**inputs):
    maps, toks = make_core_inputs(inputs)
    nc = build_program()
    res = run_bass_kernel_spmd(nc, maps, core_ids=list(range(8)))
    outp = np.zeros((2, 4096, D), np.float32)
    for c in range(8):
        b, tok = toks[c]
        outp[b, tok] = np.asarray(res.results[c]["out"], dtype=np.float32)
    return outp
```

```python
import math
from contextlib import ExitStack

import numpy as np
import concourse.bass as bass
import concourse.mybir as mybir
from concourse.bass_utils import run_bass_kernel_spmd

F32 = mybir.dt.float32
BF16 = mybir.dt.bfloat16
U32 = mybir.dt.uint32
AF = mybir.ActivationFunctionType
ALU = mybir.AluOpType
AX = mybir.AxisListType

ENGS = ['pe', 'act', 'dve', 'pool', 'sp']
NRING = 6
EPS = 1e-6
D = 2048
NEG = -30000.0
STAGE = 99
DEBUG = False


def _conflict(a, b):
    n = min(len(a), len(b))
    return a[:n] == b[:n]


class Prog:
    def __init__(self, nc, stack):
        self.nc = nc
        self.ops = {e: [] for e in ENGS}
        self.ncomp = {e: 0 for e in ENGS}
        self.ndma = {e: 0 for e in ENGS}
        self.waited = {e: {} for e in ENGS}
        self.track = {}
        self.S = {e: stack.enter_context(nc.semaphore('S_' + e)) for e in ['pe', 'act', 'dve', 'pool']}
        self.Dm = {e: [stack.enter_context(nc.semaphore('D_%s%d' % (e, i))) for i in range(NRING)]
                   for e in ['sp', 'act', 'pool']}

    def _sem_of(self, dep):
        kind, e, i = dep
        if kind == 'c':
            return ('S', e), self.S[e], i + 1
        return ('D', e, i % NRING), self.Dm[e][i % NRING], 16 * (i // NRING + 1)

    def _collect(self, reads, writes, me):
        deps = set()
        reads = [r if isinstance(r, tuple) else (r,) for r in reads]
        writes = [w if isinstance(w, tuple) else (w,) for w in writes]
        for r in reads:
            tr = self.track.setdefault(r[0], {})
            for k, (lw, rd) in tr.items():
                if _conflict(k, r) and lw is not None:
                    deps.add(lw)
        for w in writes:
            tr = self.track.setdefault(w[0], {})
            for k, (lw, rd) in tr.items():
                if _conflict(k, w):
                    if lw is not None:
                        deps.add(lw)
                    deps.update(rd)
        for w in writes:
            tr = self.track[w[0]]
            for k in [k for k in tr if _conflict(k, w) and len(k) > len(w)]:
                del tr[k]
            tr[w] = [me, []]
        for r in reads:
            tr = self.track[r[0]]
            if r not in tr:
                lw = None
                for k, (lw2, rd) in tr.items():
                    if _conflict(k, r) and len(k) < len(r) and lw2 is not None:
                        lw = lw2
                tr[r] = [lw, []]
            tr[r][1].append(me)
        deps.discard(me)
        return deps

    def _waits(self, eng, deps, is_dma):
        waits = []
        for dep in sorted(deps):
            kind, e, i = dep
            if kind == 'c' and e == eng and not is_dma and eng == 'pe':
                continue
            key, sem, val = self._sem_of(dep)
            if self.waited[eng].get(key, 0) >= val:
                continue
            self.waited[eng][key] = val
            waits.append((sem, val))
        return waits

    def op(self, eng, fn, reads=(), writes=()):
        me = ('c', eng, self.ncomp[eng])
        deps = self._collect(list(reads), list(writes), me)
        waits = self._waits(eng, deps, False)
        self.ncomp[eng] += 1
        self.ops[eng].append((waits, fn, (self.S[eng], 1)))

    def dma(self, eng, fn, reads=(), writes=()):
        k = self.ndma[eng]
        me = ('d', eng, k)
        deps = self._collect(list(reads), list(writes), me)
        if k >= NRING:
            deps.add(('d', eng, k - NRING))
        waits = self._waits(eng, deps, True)
        self.ndma[eng] += 1
        self.ops[eng].append((waits, fn, (self.Dm[eng][k % NRING], 16)))

    def barrier(self):
        allw = []
        for x in ['pe', 'act', 'dve', 'pool']:
            if self.ncomp[x] > 0:
                allw.append((('S', x), self.S[x], self.ncomp[x]))
        for q in ['sp', 'act', 'pool']:
            for r in range(NRING):
                n = 0 if self.ndma[q] <= r else (self.ndma[q] - r + NRING - 1) // NRING
                if n > 0:
                    allw.append((('D', q, r), self.Dm[q][r], 16 * n))
        for e in ENGS:
            waits = []
            for key, sem, val in allw:
                if self.waited[e].get(key, 0) >= val:
                    continue
                self.waited[e][key] = val
                waits.append((sem, val))
            self.ops[e].append((waits, None, None))

    def emit(self):
        nc = self.nc
        names = {'pe': 'tensor', 'act': 'scalar', 'dve': 'vector', 'pool': 'gpsimd', 'sp': 'sync'}
        with nc.Block() as block:
            for e in ENGS:
                ops = self.ops[e]

                def body(engine, ops=ops):
                    for waits, fn, inc in ops:
                        for sem, val in waits:
                            engine.wait_ge(sem, val)
                        if fn is not None:
                            ins = fn(engine)
                            ins.then_inc(inc[0], inc[1])
                getattr(block, names[e])(body)


def DMA(P, q, out, in_, reads, writes):
    P.dma(q, lambda e: e.dma_start(out=out, in_=in_), reads, writes)


def MM(P, out, lhsT, rhs, start, stop, reads, writes):
    P.op('pe', lambda e: e.matmul(out, lhsT=lhsT, rhs=rhs, start=start, stop=stop), reads, writes)


def TR(P, out, in_, ident, reads, writes):
    P.op('pe', lambda e: e.transpose(out=out, in_=in_, identity=ident), reads, writes)


def ACT(P, out, in_, func, reads, writes, **kw):
    P.op('act', lambda e: e.activation(out=out, in_=in_, func=func, **kw), reads, writes)


def TT(P, eng, out, in0, in1, op, reads, writes):
    P.op(eng, lambda e: e.tensor_tensor(out=out, in0=in0, in1=in1, op=op), reads, writes)


def TS(P, eng, out, in0, s1, s2, op0, op1, reads, writes, **kw):
    if op1 is None:
        P.op(eng, lambda e: e.tensor_scalar(out=out, in0=in0, scalar1=s1, scalar2=None, op0=op0, **kw), reads, writes)
    else:
        P.op(eng, lambda e: e.tensor_scalar(out=out, in0=in0, scalar1=s1, scalar2=s2, op0=op0, op1=op1, **kw),
             reads, writes)


def STT(P, out, in0, scalar, in1, op0, op1, reads, writes, **kw):
    P.op('dve', lambda e: e.scalar_tensor_tensor(out=out, in0=in0, scalar=scalar, in1=in1, op0=op0, op1=op1, **kw),
         reads, writes)


def CP(P, eng, out, in_, reads, writes):
    if eng == 'act':
        P.op('act', lambda e: e.activation(out=out, in_=in_, func=AF.Copy), reads, writes)
    else:
        P.op(eng, lambda e: e.tensor_copy(out=out, in_=in_), reads, writes)


C_QA, C_KA, C_VA, C_QB, C_KB, C_VB, C_F, C_GA, C_GB = 0, 1024, 2048, 3072, 4096, 5120, 6144, 6152, 8200
IN_W = 10248


def build_program():
    nc = bass.Bass("TRN2", target_bir_lowering=False)

    def din(name, shape, dt=F32):
        return nc.dram_tensor(name, shape, dt, kind="ExternalInput").ap()

    xT_full = din("xT_full", [D, 4096])
    xT_own = din("xT_own", [D, 1024])
    x_own = din("x_own", [1024, D])
    w_in = din("w_in", [D, IN_W])
    w_ba = din("w_ba", [1024, D])
    w_bb = din("w_bb", [1024, D])
    w_out = din("w_out", [D, D])
    w_query = din("w_query", [D, D])
    skT = din("skT", [16, 128, 128])
    exp_u = din("exp_u", [16384, D])
    exp_v = din("exp_v", [16384, D])
    gmix_in = din("gmix", [128, 16])
    gffn_in = din("gffn", [128, D])
    gfin_in = din("gfin", [128, D])
    bfor_in = din("bfor", [128, 8])
    cst_in = din("cst", [128, 3 * 128])
    msk_in = din("msk", [128, 4 * 512])
    sel_in = din("sel", [128, 4])
    iota_in = din("iota", [128, 256])
    out = nc.dram_tensor("out", [1024, D], F32, kind="ExternalOutput").ap()
    kt_scr = nc.dram_tensor("kt_scr", [16, 128, 4096], BF16, kind="Internal").ap()
    v_scr = nc.dram_tensor("v_scr", [16, 128, 32, 128], BF16, kind="Internal").ap()
    x1_scr = nc.dram_tensor("x1_scr", [8, 128, D], F32, kind="Internal").ap()
    q_scr = nc.dram_tensor("q_scr", [128, 16, 1024], BF16, kind="Internal").ap()
    uv_bf = nc.dram_tensor("uv_bf", [16384, 2 * D], BF16, kind="Internal").ap()
    dbg = {}
    if DEBUG:
        dbg['OT'] = nc.dram_tensor("dbg_OT", [128, 16, 1024], BF16, kind="ExternalOutput").ap()
        dbg['x1'] = nc.dram_tensor("dbg_x1", [8, 128, D], F32, kind="ExternalOutput").ap()
        dbg['NF'] = nc.dram_tensor("dbg_NF", [128, 32, 8], F32, kind="ExternalOutput").ap()
        dbg['ids'] = nc.dram_tensor("dbg_ids", [128, 8, 128], F32, kind="ExternalOutput").ap()
        dbg['gw'] = nc.dram_tensor("dbg_gw", [128, 8, 128], F32, kind="ExternalOutput").ap()
        dbg['acc'] = nc.dram_tensor("dbg_acc", [8, 128, D], F32, kind="ExternalOutput").ap()

    with ExitStack() as st:
        P = Prog(nc, st)

        def sb(stack, name, shape, dt):
            return stack.enter_context(nc.sbuf_tensor("s_" + name, shape, dt))

        def ps(stack, name, shape, dt):
            return stack.enter_context(nc.psum_tensor("p_" + name, shape, dt))

        cst = sb(st, "cst", [128, 384], F32)
        cstb = sb(st, "cstb", [128, 384], BF16)
        gmix = sb(st, "gmix", [128, 16], F32)
        bfor = sb(st, "bfor", [128, 8], F32)
        sel = sb(st, "sel", [128, 4], F32)
        msk = sb(st, "msk", [128, 2048], F32)
        NFt = sb(st, "NFt", [128, 32, 8], F32)
        nNFo = sb(st, "nNFo", [128, 8, 8], F32)
        DMA(P, 'sp', cst[:], cst_in[:, :], [], ['cst'])
        DMA(P, 'sp', gmix[:], gmix_in[:, :], [], ['gmix'])
        DMA(P, 'sp', bfor[:], bfor_in[:, :], [], ['bfor'])
        DMA(P, 'sp', sel[:], sel_in[:, :], [], ['sel'])
        DMA(P, 'sp', msk[:], msk_in[:, :], [], ['msk'])
        CP(P, 'dve', cstb[:], cst[:], ['cst'], ['cstb'])
        ident_f, triu_f, ones_f = cst[:, 0:128], cst[:, 128:256], cst[:, 256:384]
        ident_b, ones_b = cstb[:, 0:128], cstb[:, 256:384]
        sb01, sbadd, fxadd = msk[:, 0:512], msk[:, 512:1024], msk[:, 1024:1536]

        def normT(ph, src3, nchunks, dst, dkey, pss):
            xss = [sb(ph, "xs%d_" % i + dkey, [128, 16, 256], F32) for i in range(2)]
            sqs = [sb(ph, "sq%d_" % i + dkey, [128, 16, 256], BF16) for i in range(2)]
            rss = [sb(ph, "rs%d_" % i + dkey, [128, 256], F32) for i in range(2)]
            for c in range(nchunks):
                xs, sq, rs = xss[c % 2], sqs[c % 2], rss[c % 2]
                xk, sk_, rk = 'xs%d' % (c % 2), 'sq%d' % (c % 2), 'rs%d' % (c % 2)
                pcs = pss[:, (c % 2) * 256:(c % 2) * 256 + 256]
                pk_ = ('pss', c % 2)
                DMA(P, 'sp', xs[:], src3[:, :, c * 256:(c + 1) * 256], [], [xk])
                ACT(P, sq[:], xs[:], AF.Square, [xk], [sk_])
                for kc in range(16):
                    MM(P, pcs, ones_b, sq[:, kc, :], kc == 0, kc == 15, [sk_, 'cstb'], [pk_])
                TS(P, 'dve', rs[:], pcs, 1.0 / D, EPS, ALU.mult, ALU.add, [pk_], [rk])
                ACT(P, rs[:], rs[:], AF.Sqrt, [rk], [rk])
                P.op('dve', lambda e, rs=rs: e.reciprocal(out=rs[:], in_=rs[:]), [rk], [rk])
                for kc in range(16):
                    STT(P, dst[:, kc, c * 256:(c + 1) * 256], xs[:, kc, :], gmix[:, kc:kc + 1], rs[:],
                        ALU.mult, ALU.mult, [xk, rk, 'gmix'], [(dkey, c // 2, c % 2, kc)])

        w_in3 = w_in.rearrange("(kc p) n -> p kc n", p=128)

        with ExitStack() as ph:
            hTf = sb(ph, "hTf", [128, 16, 4096], BF16)
            pss = ps(ph, "pssA", [128, 512], F32)
            with ExitStack() as ph1:
                normT(ph1, xT_full.rearrange("(kc p) t -> p kc t", p=128), 16, hTf, 'hTf', pss)
                P.barrier()
            wg = [sb(ph, "wg%d" % i, [128, 16, 256], BF16) for i in range(2)]
            wf = sb(ph, "wf", [128, 16, 8], BF16)
            kst = [sb(ph, "kst%d" % i, [128, 512], BF16) for i in range(4)]
            vst = [sb(ph, "vst%d" % i, [128, 256], BF16) for i in range(4)]
            pk = [ps(ph, "pk%d" % i, [128, 512], F32) for i in range(4)]
            pf = ps(ph, "pf", [128, 8], F32)
            flg = sb(ph, "flg", [128, 32, 8], F32)
            gi = 0
            nev = 0
            nk = 0
            for (c0, h0) in [(C_KA + 256 * g, 2 * g) for g in range(4)] + [(C_KB + 256 * g, 8 + 2 * g) for g in range(4)]:
                w = wg[gi % 2]
                wk = 'wg%d' % (gi % 2)
                gi += 1
                DMA(P, 'pool', w[:], w_in3[:, :, c0:c0 + 256], [], [wk])
                for hh in range(2):
                    for c in range(8):
                        pt = pk[nev % 4]
                        pkk = 'pk%d' % (nev % 4)
                        for kc in range(16):
                            MM(P, pt[:], w[:, kc, hh * 128:(hh + 1) * 128], hTf[:, kc, c * 512:(c + 1) * 512],
                               kc == 0, kc == 15, [wk, ('hTf', c)], [pkk])
                        ks = kst[nk % 4]
                        kk = 'kst%d' % (nk % 4)
                        nk += 1
                        CP(P, 'act' if nev % 2 == 0 else 'dve', ks[:], pt[:], [pkk], [kk])
                        nev += 1
                        DMA(P, 'sp', kt_scr[h0 + hh][:, c * 512:(c + 1) * 512], ks[:], [kk], [('kt_scr', h0 + hh, c)])
            nv = 0
            for (c0, h0) in [(C_VA + 256 * g, 2 * g) for g in range(4)] + [(C_VB + 256 * g, 8 + 2 * g) for g in range(4)]:
                w = wg[gi % 2]
                wk = 'wg%d' % (gi % 2)
                gi += 1
                DMA(P, 'pool', w[:], w_in3[:, :, c0:c0 + 256], [], [wk])
                for tb in range(32):
                    pt = pk[nev % 4]
                    pkk = 'pk%d' % (nev % 4)
                    for kc in range(16):
                        MM(P, pt[:, 0:256], hTf[:, kc, tb * 128:(tb + 1) * 128], w[:, kc, :], kc == 0, kc == 15,
                           [wk, ('hTf', tb // 4)], [pkk])
                    vs = vst[nv % 4]
                    vk = 'vst%d' % (nv % 4)
                    nv += 1
                    CP(P, 'act' if nev % 2 == 0 else 'dve', vs[:], pt[:, 0:256], [pkk], [vk])
                    nev += 1
                    DMA(P, 'sp', v_scr[h0:h0 + 2, :, tb, :].rearrange("h p d -> p h d"),
                        vs[:].rearrange("p (h d) -> p h d", h=2), [vk], [('v_scr', h0 // 2, tb)])
            DMA(P, 'pool', wf[:], w_in3[:, :, C_F:C_F + 8], [], ['wf'])
            for tb in range(32):
                for kc in range(16):
                    MM(P, pf[:], hTf[:, kc, tb * 128:(tb + 1) * 128], wf[:, kc, :], kc == 0, kc == 15,
                       ['wf', ('hTf', tb // 4)], ['pf'])
                CP(P, 'dve', flg[:, tb, :], pf[:], ['pf'], [('flg', tb)])
            TT(P, 'dve', flg[:], flg[:], bfor[:].unsqueeze(1).to_broadcast([128, 32, 8]), ALU.add,
               ['flg', 'bfor'], ['flg'])
            ACT(P, flg[:], flg[:], AF.Exp, ['flg'], ['flg'], scale=-1.0)
            ACT(P, flg[:], flg[:], AF.Ln, ['flg'], ['flg'], bias=1.0)
            ppre = pk[0]
            ptot = pk[1]
            flat = flg[:].rearrange("p a b -> p (a b)")
            MM(P, ppre[:, 0:256], triu_f, flat, True, True, ['flg', 'cst'], ['pk0'])
            MM(P, ptot[:, 0:256], ones_f, flat, True, True, ['flg', 'cst'], ['pk1'])
            tot = sb(ph, "tot", [128, 32, 8], F32)
            car = sb(ph, "car", [128, 32, 8], F32)
            CP(P, 'dve', tot[:].rearrange("p a b -> p (a b)"), ptot[:, 0:256], ['pk1'], ['tot'])
            P.op('dve', lambda e: e.memset(car[:, 0, :], 0.0), [], ['car'])
            for bk in range(1, 32):
                TT(P, 'dve', car[:, bk, :], car[:, bk - 1, :], tot[:, bk - 1, :], ALU.add, ['car', 'tot'], ['car'])
            TT(P, 'dve', NFt[:].rearrange("p a b -> p (a b)"), ppre[:, 0:256],
               car[:].rearrange("p a b -> p (a b)"), ALU.add, ['pk0', 'car'], ['NFt'])
            NF4 = NFt[:].rearrange("p (i k) h -> p i k h", k=4)
            TS(P, 'dve', nNFo[:], NF4[:, :, 0, :], sel[:, 0:1], None, ALU.mult, None, ['NFt', 'sel'], ['nNFo'])
            for k in range(1, 4):
                STT(P, nNFo[:], NF4[:, :, k, :], sel[:, k:k + 1], nNFo[:], ALU.mult, ALU.add,
                    ['NFt', 'sel', 'nNFo'], ['nNFo'])
            TS(P, 'dve', nNFo[:], nNFo[:], -1.0, None, ALU.mult, None, ['nNFo'], ['nNFo'])
            if DEBUG:
                DMA(P, 'sp', dbg['NF'], NFt[:], ['NFt'], ['dbgNF'])
            P.barrier()

        bc = ExitStack()
        hTo = sb(bc, "hTo", [128, 16, 1024], BF16)
        OT = sb(bc, "OT", [128, 16, 1024], BF16)
        if STAGE >= 2:
            with ExitStack() as ph:
                pss = ps(ph, "pssB", [128, 512], F32)
                with ExitStack() as ph1:
                    normT(ph1, xT_own.rearrange("(kc p) t -> p kc t", p=128), 4, hTo, 'hTo', pss)
                    P.barrier()
                KTs = [sb(ph, "KT%d" % i, [128, 4096], BF16) for i in range(2)]
                VH_ = sb(ph, "VH", [128, 32, 128], BF16)
                VHs = [VH_, VH_]
                wq = sb(ph, "wq", [128, 16, 128], BF16)
                QH = sb(ph, "QH", [128, 1024], BF16)
                Zs = [sb(ph, "Z%d" % i, [128, 4096], F32) for i in range(2)]
                T1 = sb(ph, "T1", [128, 4096], F32)
                PP = sb(ph, "PP", [128, 4096], F32)
                NFbc = sb(ph, "NFbc", [128, 4096], F32)
                Ws = [sb(ph, "W%d" % i, [128, 4096], BF16) for i in range(2)]
                negt = sb(ph, "negt", [128, 4], F32)
                lpart = sb(ph, "lpart", [128, 2, 8], F32)
                lsum = sb(ph, "lsum", [128, 4], F32)
                WT = [sb(ph, "WT%d" % i, [128, 512], BF16) for i in range(2)]
                Ob = sb(ph, "Ob", [128, 128], BF16)
                small = sb(ph, "small", [128, 8], F32)
                zp = [ps(ph, "zp%d" % i, [128, 512], F32) for i in range(2)]
                tp = [ps(ph, "tp%d" % i, [128, 512], BF16) for i in range(2)]
                op_ = ps(ph, "op", [128, 128], F32)
                otp = ps(ph, "otp", [128, 128], BF16)
                scale = 1.0 / math.sqrt(128.0)
                def load_k(hd):
                    DMA(P, 'sp', KTs[hd % 2][:], kt_scr[hd], [('kt_scr', hd)], ['KT%d' % (hd % 2)])

                def load_v(hd):
                    DMA(P, 'sp', VH_[:], v_scr[hd], [('v_scr', hd // 2)], ['VH'])

                def csl(c):
                    return slice(512 * c, 512 * c + 512)

                cnt = {'nz': 0, 'ntp': 0}

                def prologue(hd):
                    is_sb = hd < 8
                    if hd + 1 < 16:
                        load_k(hd + 1)
                    qc = C_QA + hd * 128 if is_sb else C_QB + (hd - 8) * 128
                    DMA(P, 'pool', wq[:], w_in3[:, :, qc:qc + 128], [], ['wq'])
                    src_t, c0_ = (exp_u, 0) if hd < 8 else (exp_v, D)
                    r0 = (hd % 8) * 2048
                    DMA(P, 'pool', uv_bf[r0:r0 + 2048, c0_:c0_ + D], src_t[r0:r0 + 2048, :], [], [('uv_bf', hd)])
                    for half in range(2):
                        for kc in range(16):
                            MM(P, pss[:], wq[:, kc, :], hTo[:, kc, half * 512:(half + 1) * 512], kc == 0, kc == 15,
                               ['wq', 'hTo'], ['pss'])
                        ACT(P, QH[:, half * 512:(half + 1) * 512], pss[:], AF.Copy, ['pss'], ['QH'], scale=scale)
                    if not is_sb:
                        h = hd - 8
                        dg = T1[:].rearrange("p (a b) -> p a b", b=128)
                        TT(P, 'dve', dg, ident_f.unsqueeze(1).to_broadcast([128, 32, 128]),
                           NFt[:, :, h:h + 1].to_broadcast([128, 32, 128]), ALU.mult, ['cst', 'NFt'], ['T1'])
                        for c in range(8):
                            z = zp[cnt['nz'] % 2]
                            zk = 'zp%d' % (cnt['nz'] % 2)
                            cnt['nz'] += 1
                            MM(P, z[:], ones_f, T1[:, csl(c)], True, True, ['cst', 'T1'], [zk])
                            CP(P, 'act', NFbc[:, csl(c)], z[:], [zk], [('NFbc', c)])

                def stage_q(r, hd, i):
                    is_sb = hd < 8
                    nch = i + 1
                    KT, ktk = KTs[hd % 2], 'KT%d' % (hd % 2)
                    Z, zn = Zs[r % 2], 'Z%d' % (r % 2)
                    for c in range(nch):
                        z = zp[cnt['nz'] % 2]
                        zk = 'zp%d' % (cnt['nz'] % 2)
                        cnt['nz'] += 1
                        MM(P, z[:], QH[:, i * 128:(i + 1) * 128], KT[:, csl(c)], True, True, ['QH', ktk], [zk])
                        if is_sb:
                            CP(P, 'act', Z[:, csl(c)], z[:], [zk], [(zn, c)])
                        else:
                            TT(P, 'dve', Z[:, csl(c)], z[:], NFbc[:, csl(c)], ALU.add, [zk, ('NFbc', c)], [(zn, c)])

                def stage_e(r, hd, i):
                    is_sb = hd < 8
                    nch = i + 1
                    L = 512 * nch
                    Z, zn = Zs[r % 2], 'Z%d' % (r % 2)
                    W, wn = Ws[r % 2], 'W%d' % (r % 2)
                    ngc = r % 4
                    lp = lpart[:, r % 2, :]
                    lpk = ('lpart', r % 2)
                    if is_sb:
                        for c in range(nch):
                            ACT(P, T1[:, csl(c)], Z[:, csl(c)], AF.Exp, [(zn, c)], [('T1', c)])
                        for c in range(nch):
                            ACT(P, T1[:, csl(c)], T1[:, csl(c)], AF.Ln, [('T1', c)], [('T1', c)], bias=1.0)
                        TT(P, 'pool', T1[:, csl(i)], T1[:, csl(i)], sb01, ALU.mult, [('T1', i), 'msk'], [('T1', i)])
                        for c in range(nch):
                            init = 0.0 if c == 0 else PP[:, 512 * c - 1:512 * c]
                            o_ap, d0_ap, d1_ap = PP[:, csl(c)], ones_f[:, 0:1].to_broadcast([128, 512]), T1[:, csl(c)]
                            P.op('dve', lambda e, o_ap=o_ap, d0_ap=d0_ap, d1_ap=d1_ap, init=init: e.tensor_tensor_scan(
                                out=o_ap, data0=d0_ap, data1=d1_ap, initial=init, op0=ALU.mult, op1=ALU.add),
                                [('T1', c), 'cst'] + ([('PP', c - 1)] if c else []), [('PP', c)])
                        TS(P, 'dve', negt[:, ngc:ngc + 1], PP[:, L - 1:L], -1.0, None, ALU.mult, None,
                           [('PP', nch - 1)], [('negt', ngc)])
                        for c in range(nch):
                            if c == 0:
                                TT(P, 'dve', Z[:, 1:512], Z[:, 1:512], PP[:, 0:511], ALU.add,
                                   [(zn, 0), ('PP', 0)], [(zn, 0)])
                            else:
                                TT(P, 'dve', Z[:, csl(c)], Z[:, csl(c)], PP[:, 512 * c - 1:512 * c + 511], ALU.add,
                                   [(zn, c), ('PP', c), ('PP', c - 1)], [(zn, c)])
                        TT(P, 'pool', Z[:, csl(i)], Z[:, csl(i)], sbadd, ALU.add, [(zn, i), 'msk'], [(zn, i)])
                    else:
                        TT(P, 'pool', Z[:, csl(i)], Z[:, csl(i)], fxadd, ALU.add, [(zn, i), 'msk'], [(zn, i)])

                def stage_e2(r, hd, i):
                    is_sb = hd < 8
                    nch = i + 1
                    Z, zn = Zs[r % 2], 'Z%d' % (r % 2)
                    W, wn = Ws[r % 2], 'W%d' % (r % 2)
                    ngc = r % 4
                    lp = lpart[:, r % 2, :]
                    lpk = ('lpart', r % 2)
                    if is_sb:
                        for c in range(nch):
                            ACT(P, W[:, csl(c)], Z[:, csl(c)], AF.Exp, [(zn, c), ('negt', ngc)], [(wn, c)],
                                bias=negt[:, ngc:ngc + 1])
                    else:
                        for c in range(nch):
                            ACT(P, W[:, csl(c)], Z[:, csl(c)], AF.Exp, [(zn, c), 'nNFo'], [(wn, c), lpk + (c,)],
                                bias=nNFo[:, i, hd - 8:hd - 7], accum_out=lp[:, c:c + 1])
                        P.op('dve', lambda e, lp=lp, nch=nch, ngc=ngc: e.reduce_sum(
                            out=lsum[:, ngc:ngc + 1], in_=lp[:, 0:nch], axis=AX.X), [lpk], [('lsum', ngc)])
                        P.op('dve', lambda e, ngc=ngc: e.reciprocal(out=lsum[:, ngc:ngc + 1], in_=lsum[:, ngc:ngc + 1]),
                             [('lsum', ngc)], [('lsum', ngc)])

                def stage_b(r, hd, i):
                    is_sb = hd < 8
                    nch = i + 1
                    W, wn = Ws[r % 2], 'W%d' % (r % 2)
                    ngc = r % 4
                    nkb = 4 * nch
                    slots = []

                    def tr_group(g):
                        t = tp[cnt['ntp'] % 2]
                        tk = 'tp%d' % (cnt['ntp'] % 2)
                        wt = WT[cnt['ntp'] % 2]
                        wtk = 'WT%d' % (cnt['ntp'] % 2)
                        cnt['ntp'] += 1
                        for q in range(4):
                            kb = 4 * g + q
                            TR(P, t[:, q * 128:(q + 1) * 128], W[:, kb * 128:(kb + 1) * 128], ident_b,
                               [(wn, g), 'cstb'], [tk])
                        CP(P, 'dve', wt[:], t[:], [tk], [wtk])
                        slots.append((wt, wtk))

                    def pv_group(g):
                        wt, wtk = slots[g]
                        for q in range(4):
                            kb = 4 * g + q
                            MM(P, op_[:], wt[:, q * 128:(q + 1) * 128], VH_[:, kb, :], kb == 0, kb == nkb - 1,
                               [wtk, 'VH'], ['op'])
                    tr_group(0)
                    for g in range(nch):
                        if g + 1 < nch:
                            tr_group(g + 1)
                        pv_group(g)
                    if is_sb:
                        CP(P, 'dve', Ob[:], op_[:], ['op'], ['Ob'])
                    else:
                        TS(P, 'dve', Ob[:], op_[:], lsum[:, ngc:ngc + 1], None, ALU.mult, None,
                           ['op', ('lsum', ngc)], ['Ob'])
                    TR(P, otp[:], Ob[:], ident_b, ['Ob', 'cstb'], ['otp'])
                    CP(P, 'dve', OT[:, hd, i * 128:(i + 1) * 128], otp[:], ['otp'], [('OT', hd)])

                rows = [(hd, i) for hd in range(16) for i in range(8)]
                nr = len(rows)
                load_k(0)
                for it in range(nr + 2):
                    if 0 <= it - 1 < nr:
                        hd, i = rows[it - 1]
                        stage_e(it - 1, hd, i)
                    if it < nr:
                        hd, i = rows[it]
                        if i == 0:
                            prologue(hd)
                        stage_q(it, hd, i)
                    if 0 <= it - 2 < nr:
                        hd, i = rows[it - 2]
                        if i == 0:
                            load_v(hd)
                        stage_b(it - 2, hd, i)
                    if 0 <= it - 1 < nr:
                        hd, i = rows[it - 1]
                        stage_e2(it - 1, hd, i)
                if DEBUG:
                    DMA(P, 'sp', dbg['OT'], OT[:], ['OT'], ['dbgOT'])
                P.barrier()

        if STAGE >= 3:
            with ExitStack() as ph:
                mT = sb(ph, "mT", [128, 16, 1024], BF16)
                wga = sb(ph, "wga", [128, 16, 512], BF16)
                wgb = sb(ph, "wgb", [128, 16, 512], BF16)
                wa = sb(ph, "wa", [128, 8, 512], BF16)
                wb = sb(ph, "wb", [128, 8, 512], BF16)
                sga = sb(ph, "sga", [128, 512], F32)
                sgb = sb(ph, "sgb", [128, 512], F32)
                m1 = sb(ph, "m1", [128, 512], F32)
                m2 = sb(ph, "m2", [128, 512], F32)
                pc = [ps(ph, "pc%d" % i, [128, 512], F32) for i in range(8)]
                w_ba3 = w_ba.rearrange("(kc p) n -> p kc n", p=128)
                w_bb3 = w_bb.rearrange("(kc p) n -> p kc n", p=128)
                w_out3 = w_out.rearrange("(kc p) n -> p kc n", p=128)
                xo = [sb(ph, "xo%d" % i, [128, D], F32) for i in range(2)]
                it = 0
                for ng in range(4):
                    DMA(P, 'pool', wga[:], w_in3[:, :, C_GA + ng * 512:C_GA + (ng + 1) * 512], [], ['wga'])
                    DMA(P, 'pool', wgb[:], w_in3[:, :, C_GB + ng * 512:C_GB + (ng + 1) * 512], [], ['wgb'])
                    DMA(P, 'pool', wa[:], w_ba3[:, :, ng * 512:(ng + 1) * 512], [], ['wa'])
                    DMA(P, 'pool', wb[:], w_bb3[:, :, ng * 512:(ng + 1) * 512], [], ['wb'])
                    for nt in range(4):
                        ns = slice(nt * 128, (nt + 1) * 128)
                        for half in range(2):
                            hs = slice(half * 512, (half + 1) * 512)
                            b0 = 4 * (it % 2)
                            it += 1
                            pga, pgb, pya, pyb = pc[b0], pc[b0 + 1], pc[b0 + 2], pc[b0 + 3]
                            kga, kgb, kya, kyb = ['pc%d' % (b0 + x) for x in range(4)]
                            for kc in range(16):
                                MM(P, pga[:], wga[:, kc, ns], hTo[:, kc, hs], kc == 0, kc == 15, ['wga', 'hTo'], [kga])
                            for kc in range(16):
                                MM(P, pgb[:], wgb[:, kc, ns], hTo[:, kc, hs], kc == 0, kc == 15, ['wgb', 'hTo'], [kgb])
                            for kc in range(8):
                                MM(P, pya[:], wa[:, kc, ns], OT[:, kc, hs], kc == 0, kc == 7, ['wa', 'OT'], [kya])
                            for kc in range(8):
                                MM(P, pyb[:], wb[:, kc, ns], OT[:, 8 + kc, hs], kc == 0, kc == 7, ['wb', 'OT'], [kyb])
                            ACT(P, sga[:], pga[:], AF.Sigmoid, [kga], ['sga'])
                            ACT(P, sgb[:], pgb[:], AF.Sigmoid, [kgb], ['sgb'])
                            TT(P, 'dve', m1[:], sga[:], pya[:], ALU.mult, ['sga', kya], ['m1'])
                            TT(P, 'dve', m2[:], sgb[:], pyb[:], ALU.mult, ['sgb', kyb], ['m2'])
                            TT(P, 'pool', mT[:, ng * 4 + nt, hs], m1[:], m2[:], ALU.add, ['m1', 'm2'], ['mT'])
                wo = sb(ph, "wo", [128, 16, 512], BF16)
                it = 0
                for ng in range(4):
                    DMA(P, 'pool', wo[:], w_out3[:, :, ng * 512:(ng + 1) * 512], [], ['wo'])
                    for tb in range(8):
                        pt = pc[it % 4]
                        pk_ = 'pc%d' % (it % 4)
                        xb_ = xo[it % 2]
                        xk = 'xo%d' % (it % 2)
                        it += 1
                        cs = slice(ng * 512, (ng + 1) * 512)
                        DMA(P, 'sp', xb_[:, 0:512], x_own[tb * 128:(tb + 1) * 128, cs], [], [xk])
                        for kc in range(16):
                            MM(P, pt[:], mT[:, kc, tb * 128:(tb + 1) * 128], wo[:, kc, :], kc == 0, kc == 15,
                               ['mT', 'wo'], [pk_])
                        TT(P, 'dve', xb_[:, 0:512], xb_[:, 0:512], pt[:], ALU.add, [xk, pk_], [xk])
                        DMA(P, 'sp', x1_scr[tb][:, cs], xb_[:, 0:512], [xk], [('x1s', tb, ng)])
                        if DEBUG:
                            DMA(P, 'sp', dbg['x1'][tb][:, cs], xb_[:, 0:512], [xk], [('dbgx1', tb, ng)])
                P.barrier()

        bc.close()
        if STAGE >= 4:
            with ExitStack() as ph:
                gffn = sb(ph, "gffn", [128, D], F32)
                gfin = sb(ph, "gfin", [128, D], F32)
                skb = sb(ph, "skb", [128, 16, 128], BF16)
                rstd2 = sb(ph, "rstd2", [128, 8], F32)
                DMA(P, 'sp', gffn[:], gffn_in[:, :], [], ['gffn'])
                DMA(P, 'sp', gfin[:], gfin_in[:, :], [], ['gfin'])
                DMA(P, 'pool', skb[:], skT.rearrange("ch c n -> c ch n"), [], ['skb'])
                pd = [ps(ph, "pd%d" % i, [128, 512], F32) for i in range(4)]
                jb = sb(ph, "jb", [128, D], BF16)
                x1bs = [sb(ph, "x1b%d" % i, [128, D], F32) for i in range(2)]
                x1b = x1bs[0]
                with ExitStack() as ph1:
                    pdb = [ps(ph1, "pdb%d" % i, [128, 512], BF16) for i in range(2)]
                    h2T = sb(ph1, "h2T", [128, 16, 1024], BF16)
                    qT = sb(ph1, "qT", [128, 16, 1024], BF16)
                    h2b = sb(ph1, "h2b", [128, D], BF16)
                    wqp = sb(ph1, "wqp", [128, 16, 512], BF16)
                    w_q3 = w_query.rearrange("(kc p) n -> p kc n", p=128)
                    ntp = 0
                    for tb in range(8):
                        DMA(P, 'sp', x1b[:], x1_scr[tb], [('x1s', tb)], ['x1b'])
                        ACT(P, jb[:], x1b[:], AF.Square, ['x1b'], ['jb', ('rstd2', tb)],
                            accum_out=rstd2[:, tb:tb + 1])
                        TS(P, 'dve', rstd2[:, tb:tb + 1], rstd2[:, tb:tb + 1], 1.0 / D, EPS, ALU.mult, ALU.add,
                           [('rstd2', tb)], [('rstd2', tb)])
                        ACT(P, rstd2[:, tb:tb + 1], rstd2[:, tb:tb + 1], AF.Sqrt, [('rstd2', tb)], [('rstd2', tb)])
                        P.op('dve', lambda e, tb=tb: e.reciprocal(out=rstd2[:, tb:tb + 1], in_=rstd2[:, tb:tb + 1]),
                             [('rstd2', tb)], [('rstd2', tb)])
                        STT(P, h2b[:], x1b[:], rstd2[:, tb:tb + 1], gffn[:], ALU.mult, ALU.mult,
                            ['x1b', ('rstd2', tb), 'gffn'], ['h2b'])
                        for g in range(4):
                            t = pdb[ntp % 2]
                            tk = 'pdb%d' % (ntp % 2)
                            ntp += 1
                            for q in range(4):
                                kc = 4 * g + q
                                TR(P, t[:, q * 128:(q + 1) * 128], h2b[:, kc * 128:(kc + 1) * 128], ident_b,
                                   ['h2b', 'cstb'], [tk])
                            CP(P, 'act', h2T[:, 4 * g:4 * g + 4, tb * 128:(tb + 1) * 128],
                               t[:].rearrange("p (a b) -> p a b", a=4), [tk], ['h2T'])
                    it = 0
                    for ng in range(4):
                        DMA(P, 'pool', wqp[:], w_q3[:, :, ng * 512:(ng + 1) * 512], [], ['wqp'])
                        for nt in range(4):
                            for half in range(2):
                                pt = pd[it % 4]
                                pk_ = 'pd%d' % (it % 4)
                                it += 1
                                for kc in range(16):
                                    MM(P, pt[:], wqp[:, kc, nt * 128:(nt + 1) * 128],
                                       h2T[:, kc, half * 512:(half + 1) * 512], kc == 0, kc == 15, ['wqp', 'h2T'], [pk_])
                                CP(P, 'act' if it % 2 else 'dve', qT[:, ng * 4 + nt, half * 512:(half + 1) * 512], pt[:],
                                   [pk_], ['qT'])
                    DMA(P, 'sp', q_scr, qT[:], ['qT'], ['q_scr'])
                    P.barrier()
                po = [ps(ph, "po%d" % i, [128, 512], F32) for i in range(4)]
                sc = sb(ph, "sc", [128, 16, 128], F32)
                tmp = sb(ph, "tmp", [128, 256], F32)
                v16 = sb(ph, "v16", [128, 8, 2, 16], F32)
                ix = sb(ph, "ix", [128, 8, 2, 16], U32)
                ixf = sb(ph, "ixf", [128, 8, 2, 16], F32)
                cand = sb(ph, "cand", [128, 8, 16, 16], F32)
                cid = sb(ph, "cid", [128, 8, 16, 16], F32)
                t16 = sb(ph, "t16", [128, 8, 16], F32)
                pos = sb(ph, "pos", [128, 8, 16], U32)
                posf = sb(ph, "posf", [128, 8, 16], F32)
                iot = sb(ph, "iot", [128, 256], F32)
                DMA(P, 'sp', iot[:], iota_in[:, :], [], ['iot'])
                idf = sb(ph, "idf", [128, 128], F32)
                jk2 = sb(ph, "jk2", [128, 256], F32)
                idus = [sb(ph, "idu%d" % i, [128, 128], U32) for i in range(2)]
                gts = [sb(ph, "gt%d" % i, [128, 8, 16], F32) for i in range(2)]
                gs = sb(ph, "gs", [128, 8], F32)
                aa = sb(ph, "aa", [128, 128], F32)
                ww = sb(ph, "ww", [128, 128], F32)
                g1 = sb(ph, "g1", [128, 128], F32)
                h2f = sb(ph, "h2f", [128, D], F32)
                qTbs = [sb(ph, "qTb%d" % i, [128, 16, 128], BF16) for i in range(2)]
                ssf = sb(ph, "ssf", [128, 2], F32)
                GSZ = 8
                dgs = [sb(ph, "dg%d" % i, [128, GSZ, 128], BF16) for i in range(2)]
                NG_, NVR = 5, 14
                gbs = [sb(ph, "gb%d" % i, [128, 2 * D], BF16) for i in range(NG_)]
                vrs = [sb(ph, "vr%d" % i, [128, D], BF16) for i in range(NVR)]
                ngb = [0]

                def topk(tb):
                    idu = idus[tb % 2]
                    ik = 'idu%d' % (tb % 2)
                    gt = gts[tb % 2]
                    gk = 'gt%d' % (tb % 2)
                    qTb = qTbs[tb % 2]
                    qk = 'qTb%d' % (tb % 2)
                    DMA(P, 'sp', qTb[:], q_scr[:, :, tb * 128:(tb + 1) * 128], ['q_scr'], [qk])
                    for g in range(4):
                        for q in range(4):
                            ch = 4 * g + q
                            MM(P, pd[g][:, q * 128:(q + 1) * 128], qTb[:, ch, :], skb[:, ch, :],
                               True, True, [qk, 'skb'], ['pd%d' % g])
                        CP(P, 'act', sc[:, 4 * g:4 * g + 4, :], pd[g][:].rearrange("p (a b) -> p a b", a=4),
                           ['pd%d' % g], ['sc'])
                    for ch in range(16):
                        hh, pp = ch // 2, ch % 2
                        P.op('dve', lambda e, ch=ch, hh=hh, pp=pp: e.max(out=v16[:, hh, pp, 0:8], in_=sc[:, ch, :]),
                             ['sc'], ['v16'])
                        P.op('dve', lambda e, ch=ch, hh=hh, pp=pp: e.max_index(
                            out=ix[:, hh, pp, 0:8], in_max=v16[:, hh, pp, 0:8], in_values=sc[:, ch, :]),
                            ['sc', 'v16'], ['ix'])
                        P.op('dve', lambda e, ch=ch, hh=hh, pp=pp: e.match_replace(
                            out=tmp[:, 0:128], in_to_replace=v16[:, hh, pp, 0:8], in_values=sc[:, ch, :],
                            imm_value=-1e30), ['sc', 'v16'], ['tmp'])
                        P.op('dve', lambda e, ch=ch, hh=hh, pp=pp: e.max(out=v16[:, hh, pp, 8:16], in_=tmp[:, 0:128]),
                             ['tmp'], ['v16'])
                        P.op('dve', lambda e, ch=ch, hh=hh, pp=pp: e.max_index(
                            out=ix[:, hh, pp, 8:16], in_max=v16[:, hh, pp, 8:16], in_values=tmp[:, 0:128]),
                            ['tmp', 'v16'], ['ix'])
                    CP(P, 'dve', ixf[:], ix[:], ['ix'], ['ixf'])
                    TS(P, 'dve', ixf[:, :, 0, :], ixf[:, :, 0, :], 128.0, None, ALU.mult, None, ['ixf'], ['ixf'])
                    TT(P, 'dve', cand[:], v16[:, :, 0, :].unsqueeze(3).to_broadcast([128, 8, 16, 16]),
                       v16[:, :, 1, :].unsqueeze(2).to_broadcast([128, 8, 16, 16]), ALU.add, ['v16'], ['cand'])
                    TT(P, 'dve', cid[:], ixf[:, :, 0, :].unsqueeze(3).to_broadcast([128, 8, 16, 16]),
                       ixf[:, :, 1, :].unsqueeze(2).to_broadcast([128, 8, 16, 16]), ALU.add, ['ixf'], ['cid'])
                    for hh in range(8):
                        cf = cand[:, hh].rearrange("p a b -> p (a b)")
                        P.op('dve', lambda e, hh=hh, cf=cf: e.max(out=t16[:, hh, 0:8], in_=cf), ['cand'], ['t16'])
                        P.op('dve', lambda e, hh=hh, cf=cf: e.max_index(
                            out=pos[:, hh, 0:8], in_max=t16[:, hh, 0:8], in_values=cf), ['cand', 't16'], ['pos'])
                        P.op('dve', lambda e, hh=hh, cf=cf: e.match_replace(
                            out=tmp[:], in_to_replace=t16[:, hh, 0:8], in_values=cf, imm_value=-1e30),
                            ['cand', 't16'], ['tmp'])
                        P.op('dve', lambda e, hh=hh: e.max(out=t16[:, hh, 8:16], in_=tmp[:]), ['tmp'], ['t16'])
                        P.op('dve', lambda e, hh=hh: e.max_index(
                            out=pos[:, hh, 8:16], in_max=t16[:, hh, 8:16], in_values=tmp[:]), ['tmp', 't16'], ['pos'])
                    CP(P, 'dve', posf[:], pos[:], ['pos'], ['posf'])
                    for hh in range(8):
                        cidf = cid[:, hh].rearrange("p a b -> p (a b)")
                        for k in range(16):
                            STT(P, jk2[:], iot[:], posf[:, hh, k:k + 1], cidf, ALU.is_equal, ALU.mult,
                                ['iot', 'cid', 'posf'], [('idf', hh * 16 + k)],
                                accum_out=idf[:, hh * 16 + k:hh * 16 + k + 1])
                    CP(P, 'dve', idu[:], idf[:], ['idf'], [ik])
                    if DEBUG:
                        DMA(P, 'sp', dbg['ids'][:, tb, :], idf[:], ['idf'], [('dbgids', tb)])
                    TT(P, 'dve', gt[:], t16[:], t16[:, :, 0:1].to_broadcast([128, 8, 16]), ALU.subtract, ['t16'], [gk])
                    ACT(P, gt[:], gt[:], AF.Exp, [gk], [gk])
                    P.op('dve', lambda e, gt=gt: e.reduce_sum(out=gs[:], in_=gt[:], axis=AX.X), [gk], ['gs'])
                    P.op('dve', lambda e: e.reciprocal(out=gs[:], in_=gs[:]), ['gs'], ['gs'])
                    TT(P, 'dve', gt[:], gt[:], gs[:].unsqueeze(2).to_broadcast([128, 8, 16]), ALU.mult, [gk, 'gs'], [gk])

                def gather(idu, ik, hk):
                    k = ngb[0]
                    ngb[0] += 1
                    bfr, bk = gbs[k % NG_], 'gb%d' % (k % NG_)
                    vr, vk = vrs[k % NVR], 'vr%d' % (k % NVR)
                    P.dma('pool', lambda e, bfr=bfr, hk=hk: e.indirect_dma_start(
                        out=bfr[:, :], out_offset=None, in_=uv_bf[:, :],
                        in_offset=bass.IndirectOffsetOnAxis(ap=idu[:, hk:hk + 1], axis=0)), [ik, 'uv_bf'], [bk])
                    CP(P, 'act', vr[:], bfr[:, D:2 * D], [bk], [vk])
                    return bfr[:, 0:D], bk, vr, vk

                def u_prep(tb):
                    x1b = x1bs[tb % 2]
                    xk = 'x1b%d' % (tb % 2)
                    DMA(P, 'sp', x1b[:], x1_scr[tb], [('x1s', tb)], [xk])
                    STT(P, h2f[:], x1b[:], rstd2[:, tb:tb + 1], gffn[:], ALU.mult, ALU.mult,
                        [xk, 'rstd2', 'gffn'], ['h2f'])

                ngrp = [0]

                def grp_dots(tb, g, j0, j1, st):
                    idu = idus[tb % 2]
                    ik = 'idu%d' % (tb % 2)
                    for j in range(j0, j1):
                        hk = g * GSZ + j
                        ub, uk, vb, vk = gather(idu, ik, hk)
                        st['v'].append((vb, vk))
                        STT(P, jb[:], ub, 1.0, h2f[:], ALU.mult, ALU.mult, [uk, 'h2f'], [('aa', hk)],
                            accum_out=aa[:, hk:hk + 1])

                def grp_gelu_a(tb, g):
                    hs = slice(g * GSZ, (g + 1) * GSZ)
                    ak = [('aa', g * GSZ + j) for j in range(GSZ)]
                    g1k = ('g1', g % 2)
                    TT(P, 'dve', g1[:, hs], aa[:, hs], aa[:, hs], ALU.mult, ak, [g1k])
                    TS(P, 'dve', g1[:, hs], g1[:, hs], 0.044715, 1.0, ALU.mult, ALU.add, [g1k], [g1k])
                    TT(P, 'dve', g1[:, hs], g1[:, hs], aa[:, hs], ALU.mult, [g1k] + ak, [g1k])
                    ACT(P, g1[:, hs], g1[:, hs], AF.Sigmoid, [g1k], [g1k], scale=2.0 * math.sqrt(2.0 / math.pi))

                def grp_finish(tb, g, st):
                    gt = gts[tb % 2]
                    gk = 'gt%d' % (tb % 2)
                    dg = dgs[ngrp[0] % 2]
                    dk = 'dg%d' % (ngrp[0] % 2)
                    ngrp[0] += 1
                    hs = slice(g * GSZ, (g + 1) * GSZ)
                    ak = [('aa', g * GSZ + j) for j in range(GSZ)]
                    g1k = ('g1', g % 2)
                    TT(P, 'dve', g1[:, hs], g1[:, hs], aa[:, hs], ALU.mult, [g1k] + ak, [g1k])
                    TT(P, 'dve', ww[:, hs], g1[:, hs], gt[:].rearrange("p a b -> p (a b)")[:, hs], ALU.mult,
                       [g1k, gk], [('ww', g % 2)])
                    for j in range(GSZ):
                        hk = g * GSZ + j
                        ACT(P, dg[:, j, :], ident_b, AF.Copy, ['cstb', ('ww', g % 2)], [(dk, j)],
                            scale=ww[:, hk:hk + 1])
                    for j in range(GSZ):
                        hk = g * GSZ + j
                        vb, vk = st['v'][j]
                        for c in range(4):
                            MM(P, po[c][:], dg[:, j, :], vb[:, c * 512:(c + 1) * 512], hk == 0, hk == 127,
                               [(dk, j), vk], ['po%d' % c])

                def final(tb):
                    x1b = x1bs[tb % 2]
                    xk = 'x1b%d' % (tb % 2)
                    if DEBUG:
                        DMA(P, 'sp', dbg['gw'][:, tb, :], ww[:], ['ww'], [('dbggw', tb)])
                    for c in range(4):
                        TT(P, 'dve', x1b[:, c * 512:(c + 1) * 512], x1b[:, c * 512:(c + 1) * 512], po[c][:], ALU.add,
                           [xk, 'po%d' % c], [xk])
                    if DEBUG:
                        DMA(P, 'sp', dbg['acc'][tb], x1b[:], [xk], [('dbgacc', tb)])
                    ACT(P, jb[:], x1b[:], AF.Square, [xk], ['ssf'], accum_out=ssf[:, 0:1])
                    TS(P, 'dve', ssf[:, 0:1], ssf[:, 0:1], 1.0 / D, EPS, ALU.mult, ALU.add, ['ssf'], ['ssf'])
                    ACT(P, ssf[:, 0:1], ssf[:, 0:1], AF.Sqrt, ['ssf'], ['ssf'])
                    P.op('dve', lambda e: e.reciprocal(out=ssf[:, 0:1], in_=ssf[:, 0:1]), ['ssf'], ['ssf'])
                    STT(P, x1b[:], x1b[:], ssf[:, 0:1], gfin[:], ALU.mult, ALU.mult, [xk, 'ssf', 'gfin'], [xk])
                    DMA(P, 'sp', out[tb * 128:(tb + 1) * 128, :], x1b[:], [xk], [('out', tb)])

                topk(0)
                for tb in range(8):
                    u_prep(tb)
                    NG = 128 // GSZ
                    prev = None
                    for g in range(NG):
                        st_ = {'v': []}
                        grp_dots(tb, g, 0, GSZ // 2, st_)
                        if prev is not None:
                            grp_finish(tb, g - 1, prev)
                        grp_dots(tb, g, GSZ // 2, GSZ, st_)
                        grp_gelu_a(tb, g)
                        prev = st_
                        if g == 7 and tb + 1 < 8:
                            topk(tb + 1)
                    grp_finish(tb, NG - 1, prev)
                    final(tb)
                P.barrier()
        P.barrier()
        P.emit()
    return nc


def make_core_inputs(inputs):
    x = np.asarray(inputs["x"], dtype=np.float32)
    w_in = np.ascontiguousarray(np.asarray(inputs["w_in"], dtype=np.float32)[0])
    w_ba = np.ascontiguousarray(np.asarray(inputs["w_branch_a"], dtype=np.float32)[0])
    w_bb = np.ascontiguousarray(np.asarray(inputs["w_branch_b"], dtype=np.float32)[0])
    w_out = np.ascontiguousarray(np.asarray(inputs["w_out"], dtype=np.float32)[0])
    w_query = np.ascontiguousarray(np.asarray(inputs["w_query"], dtype=np.float32)[0])
    sk = np.asarray(inputs["sub_keys"], dtype=np.float32)[0]
    skT = np.ascontiguousarray(sk.transpose(0, 1, 3, 2).reshape(16, 128, 128))
    exp_u = np.ascontiguousarray(np.asarray(inputs["expert_u"], dtype=np.float32)[0])
    exp_v = np.ascontiguousarray(np.asarray(inputs["expert_v"], dtype=np.float32)[0])
    gmix = np.ascontiguousarray(np.asarray(inputs["norm_mix_gain"], dtype=np.float32)[0].reshape(16, 128).T)
    gffn = np.ascontiguousarray(np.broadcast_to(np.asarray(inputs["norm_ffn_gain"], dtype=np.float32)[0][None, :], (128, D)))
    gfin = np.ascontiguousarray(np.broadcast_to(np.asarray(inputs["norm_final_gain"], dtype=np.float32)[None, :], (128, D)))
    bfor = np.ascontiguousarray(np.broadcast_to(np.asarray(inputs["b_forget"], dtype=np.float32)[0][None, :], (128, 8)))
    idx = np.arange(128)
    ident = np.eye(128, dtype=np.float32)
    triu = (idx[:, None] <= idx[None, :]).astype(np.float32)
    cst = np.ascontiguousarray(np.concatenate([ident, triu, np.ones((128, 128), np.float32)], axis=1))
    xT = [np.ascontiguousarray(x[b].T) for b in range(2)]
    iota = np.ascontiguousarray(np.broadcast_to(np.arange(256, dtype=np.float32)[None, :], (128, 256)))
    maps = []
    toks = []
    for c in range(8):
        b, j = c // 4, c % 4
        tok = np.concatenate([np.arange((4 * i + j) * 128, (4 * i + j + 1) * 128) for i in range(8)])
        toks.append((b, tok))
        sb01 = np.zeros((128, 4, 128), np.float32)
        fx01 = np.zeros((128, 4, 128), np.float32)
        for k in range(4):
            if k < j:
                sb01[:, k, :] = 1.0
                fx01[:, k, :] = 1.0
            elif k == j:
                sb01[:, k, :] = (idx[None, :] < idx[:, None])
                fx01[:, k, :] = (idx[None, :] <= idx[:, None])
        sbadd = (1.0 - sb01) * NEG
        fxadd = (1.0 - fx01) * NEG
        msk = np.ascontiguousarray(np.concatenate(
            [sb01.reshape(128, 512), sbadd.reshape(128, 512), fxadd.reshape(128, 512), np.zeros((128, 512), np.float32)],
            axis=1).astype(np.float32))
        sel = np.zeros((128, 4), np.float32)
        sel[:, j] = 1.0
        maps.append({
            "xT_full": xT[b], "xT_own": np.ascontiguousarray(xT[b][:, tok]), "x_own": np.ascontiguousarray(x[b][tok]),
            "w_in": w_in, "w_ba": w_ba, "w_bb": w_bb, "w_out": w_out, "w_query": w_query, "skT": skT,
            "exp_u": exp_u, "exp_v": exp_v, "gmix": gmix, "gffn": gffn, "gfin": gfin, "bfor": bfor,
            "cst": cst, "msk": msk, "sel": sel, "iota": iota,
        })
    return maps, toks


def kernel(**inputs):
    maps, toks = make_core_inputs(inputs)
    nc = build_program()
    res = run_bass_kernel_spmd(nc, maps, core_ids=list(range(8)))
    outp = np.zeros((2, 4096, D), np.float32)
    for c in range(8):
        b, tok = toks[c]
        outp[b, tok] = np.asarray(res.results[c]["out"], dtype=np.float32)
    return outp
```

```python
import math
from contextlib import ExitStack

import numpy as np
import concourse.bass as bass
import concourse.mybir as mybir
from concourse.bass_utils import run_bass_kernel_spmd

F32 = mybir.dt.float32
BF16 = mybir.dt.bfloat16
U32 = mybir.dt.uint32
AF = mybir.ActivationFunctionType
ALU = mybir.AluOpType
AX = mybir.AxisListType

ENGS = ['pe', 'act', 'dve', 'pool', 'sp']
NRING = 8
EPS = 1e-6
D = 2048
NEG = -30000.0
STAGE = 99
DEBUG = False


def _conflict(a, b):
    n = min(len(a), len(b))
    return a[:n] == b[:n]


class Prog:
    def __init__(self, nc, stack):
        self.nc = nc
        self.ops = {e: [] for e in ENGS}
        self.ncomp = {e: 0 for e in ENGS}
        self.ndma = {e: 0 for e in ENGS}
        self.waited = {e: {} for e in ENGS}
        self.track = {}
        self.S = {e: stack.enter_context(nc.semaphore('S_' + e)) for e in ['pe', 'act', 'dve', 'pool']}
        self.Dm = {e: [stack.enter_context(nc.semaphore('D_%s%d' % (e, i))) for i in range(NRING)]
                   for e in ['sp', 'act', 'pool']}

    def _sem_of(self, dep):
        kind, e, i = dep
        if kind == 'c':
            return ('S', e), self.S[e], i + 1
        return ('D', e, i % NRING), self.Dm[e][i % NRING], 16 * (i // NRING + 1)

    def _collect(self, reads, writes, me):
        deps = set()
        reads = [r if isinstance(r, tuple) else (r,) for r in reads]
        writes = [w if isinstance(w, tuple) else (w,) for w in writes]
        for r in reads:
            tr = self.track.setdefault(r[0], {})
            for k, (lw, rd) in tr.items():
                if _conflict(k, r) and lw is not None:
                    deps.add(lw)
        for w in writes:
            tr = self.track.setdefault(w[0], {})
            for k, (lw, rd) in tr.items():
                if _conflict(k, w):
                    if lw is not None:
                        deps.add(lw)
                    deps.update(rd)
        for w in writes:
            tr = self.track[w[0]]
            for k in [k for k in tr if _conflict(k, w) and len(k) > len(w)]:
                del tr[k]
            tr[w] = [me, []]
        for r in reads:
            tr = self.track[r[0]]
            if r not in tr:
                lw = None
                for k, (lw2, rd) in tr.items():
                    if _conflict(k, r) and len(k) < len(r) and lw2 is not None:
                        lw = lw2
                tr[r] = [lw, []]
            tr[r][1].append(me)
        deps.discard(me)
        return deps

    def _waits(self, eng, deps, is_dma):
        waits = []
        for dep in sorted(deps):
            kind, e, i = dep
            if kind == 'c' and e == eng and not is_dma and eng == 'pe':
                continue
            key, sem, val = self._sem_of(dep)
            if self.waited[eng].get(key, 0) >= val:
                continue
            self.waited[eng][key] = val
            waits.append((sem, val))
        return waits

    def op(self, eng, fn, reads=(), writes=()):
        me = ('c', eng, self.ncomp[eng])
        deps = self._collect(list(reads), list(writes), me)
        waits = self._waits(eng, deps, False)
        self.ncomp[eng] += 1
        self.ops[eng].append((waits, fn, (self.S[eng], 1)))

    def dma(self, eng, fn, reads=(), writes=()):
        k = self.ndma[eng]
        me = ('d', eng, k)
        deps = self._collect(list(reads), list(writes), me)
        if k >= NRING:
            deps.add(('d', eng, k - NRING))
        waits = self._waits(eng, deps, True)
        self.ndma[eng] += 1
        self.ops[eng].append((waits, fn, (self.Dm[eng][k % NRING], 16)))

    def barrier(self):
        allw = []
        for x in ['pe', 'act', 'dve', 'pool']:
            if self.ncomp[x] > 0:
                allw.append((('S', x), self.S[x], self.ncomp[x]))
        for q in ['sp', 'act', 'pool']:
            for r in range(NRING):
                n = 0 if self.ndma[q] <= r else (self.ndma[q] - r + NRING - 1) // NRING
                if n > 0:
                    allw.append((('D', q, r), self.Dm[q][r], 16 * n))
        for e in ENGS:
            waits = []
            for key, sem, val in allw:
                if self.waited[e].get(key, 0) >= val:
                    continue
                self.waited[e][key] = val
                waits.append((sem, val))
            self.ops[e].append((waits, None, None))

    def emit(self):
        nc = self.nc
        names = {'pe': 'tensor', 'act': 'scalar', 'dve': 'vector', 'pool': 'gpsimd', 'sp': 'sync'}
        with nc.Block() as block:
            for e in ENGS:
                ops = self.ops[e]

                def body(engine, ops=ops):
                    for waits, fn, inc in ops:
                        for sem, val in waits:
                            engine.wait_ge(sem, val)
                        if fn is not None:
                            ins = fn(engine)
                            ins.then_inc(inc[0], inc[1])
                getattr(block, names[e])(body)


def DMA(P, q, out, in_, reads, writes):
    P.dma(q, lambda e: e.dma_start(out=out, in_=in_), reads, writes)


def MM(P, out, lhsT, rhs, start, stop, reads, writes):
    P.op('pe', lambda e: e.matmul(out, lhsT=lhsT, rhs=rhs, start=start, stop=stop), reads, writes)


def TR(P, out, in_, ident, reads, writes):
    P.op('pe', lambda e: e.transpose(out=out, in_=in_, identity=ident), reads, writes)


def ACT(P, out, in_, func, reads, writes, **kw):
    P.op('act', lambda e: e.activation(out=out, in_=in_, func=func, **kw), reads, writes)


def TT(P, eng, out, in0, in1, op, reads, writes):
    P.op(eng, lambda e: e.tensor_tensor(out=out, in0=in0, in1=in1, op=op), reads, writes)


def TS(P, eng, out, in0, s1, s2, op0, op1, reads, writes, **kw):
    if op1 is None:
        P.op(eng, lambda e: e.tensor_scalar(out=out, in0=in0, scalar1=s1, scalar2=None, op0=op0, **kw), reads, writes)
    else:
        P.op(eng, lambda e: e.tensor_scalar(out=out, in0=in0, scalar1=s1, scalar2=s2, op0=op0, op1=op1, **kw),
             reads, writes)


def STT(P, out, in0, scalar, in1, op0, op1, reads, writes, **kw):
    P.op('dve', lambda e: e.scalar_tensor_tensor(out=out, in0=in0, scalar=scalar, in1=in1, op0=op0, op1=op1, **kw),
         reads, writes)


def CP(P, eng, out, in_, reads, writes):
    if eng == 'act':
        P.op('act', lambda e: e.activation(out=out, in_=in_, func=AF.Copy), reads, writes)
    else:
        P.op(eng, lambda e: e.tensor_copy(out=out, in_=in_), reads, writes)


C_QA, C_KA, C_VA, C_QB, C_KB, C_VB, C_F, C_GA, C_GB = 0, 1024, 2048, 3072, 4096, 5120, 6144, 6152, 8200
IN_W = 10248


def build_program():
    nc = bass.Bass("TRN2", target_bir_lowering=False)

    def din(name, shape, dt=F32):
        return nc.dram_tensor(name, shape, dt, kind="ExternalInput").ap()

    xT_full = din("xT_full", [D, 4096])
    xT_own = din("xT_own", [D, 1024])
    x_own = din("x_own", [1024, D])
    w_in = din("w_in", [D, IN_W])
    w_ba = din("w_ba", [1024, D])
    w_bb = din("w_bb", [1024, D])
    w_out = din("w_out", [D, D])
    w_query = din("w_query", [D, D])
    skT = din("skT", [16, 128, 128])
    exp_u = din("exp_u", [16384, D])
    exp_v = din("exp_v", [16384, D])
    gmix_in = din("gmix", [128, 16])
    gffn_in = din("gffn", [128, D])
    gfin_in = din("gfin", [128, D])
    bfor_in = din("bfor", [128, 8])
    cst_in = din("cst", [128, 3 * 128])
    msk_in = din("msk", [128, 4 * 512])
    sel_in = din("sel", [128, 4])
    iota_in = din("iota", [128, 256])
    out = nc.dram_tensor("out", [1024, D], F32, kind="ExternalOutput").ap()
    kt_scr = nc.dram_tensor("kt_scr", [16, 128, 4096], BF16, kind="Internal").ap()
    v_scr = nc.dram_tensor("v_scr", [16, 128, 32, 128], BF16, kind="Internal").ap()
    x1_scr = nc.dram_tensor("x1_scr", [8, 128, D], F32, kind="Internal").ap()
    q_scr = nc.dram_tensor("q_scr", [128, 16, 1024], BF16, kind="Internal").ap()
    uv_bf = nc.dram_tensor("uv_bf", [16384, 2 * D], BF16, kind="Internal").ap()
    dbg = {}
    if DEBUG:
        dbg['OT'] = nc.dram_tensor("dbg_OT", [128, 16, 1024], BF16, kind="ExternalOutput").ap()
        dbg['x1'] = nc.dram_tensor("dbg_x1", [8, 128, D], F32, kind="ExternalOutput").ap()
        dbg['NF'] = nc.dram_tensor("dbg_NF", [128, 32, 8], F32, kind="ExternalOutput").ap()
        dbg['ids'] = nc.dram_tensor("dbg_ids", [128, 8, 128], F32, kind="ExternalOutput").ap()
        dbg['gw'] = nc.dram_tensor("dbg_gw", [128, 8, 128], F32, kind="ExternalOutput").ap()
        dbg['acc'] = nc.dram_tensor("dbg_acc", [8, 128, D], F32, kind="ExternalOutput").ap()

    with ExitStack() as st:
        P = Prog(nc, st)

        def sb(stack, name, shape, dt):
            return stack.enter_context(nc.sbuf_tensor("s_" + name, shape, dt))

        def ps(stack, name, shape, dt):
            return stack.enter_context(nc.psum_tensor("p_" + name, shape, dt))

        cst = sb(st, "cst", [128, 384], F32)
        cstb = sb(st, "cstb", [128, 384], BF16)
        gmix = sb(st, "gmix", [128, 16], F32)
        bfor = sb(st, "bfor", [128, 8], F32)
        sel = sb(st, "sel", [128, 4], F32)
        msk = sb(st, "msk", [128, 2048], F32)
        NFt = sb(st, "NFt", [128, 32, 8], F32)
        nNFo = sb(st, "nNFo", [128, 8, 8], F32)
        DMA(P, 'sp', cst[:], cst_in[:, :], [], ['cst'])
        DMA(P, 'sp', gmix[:], gmix_in[:, :], [], ['gmix'])
        DMA(P, 'sp', bfor[:], bfor_in[:, :], [], ['bfor'])
        DMA(P, 'sp', sel[:], sel_in[:, :], [], ['sel'])
        DMA(P, 'sp', msk[:], msk_in[:, :], [], ['msk'])
        CP(P, 'dve', cstb[:], cst[:], ['cst'], ['cstb'])
        ident_f, triu_f, ones_f = cst[:, 0:128], cst[:, 128:256], cst[:, 256:384]
        ident_b, ones_b = cstb[:, 0:128], cstb[:, 256:384]
        sb01, sbadd, fxadd = msk[:, 0:512], msk[:, 512:1024], msk[:, 1024:1536]

        def normT(ph, src3, nchunks, dst, dkey, pss):
            xss = [sb(ph, "xs%d_" % i + dkey, [128, 16, 256], F32) for i in range(2)]
            sqs = [sb(ph, "sq%d_" % i + dkey, [128, 16, 256], BF16) for i in range(2)]
            rss = [sb(ph, "rs%d_" % i + dkey, [128, 256], F32) for i in range(2)]
            for c in range(nchunks):
                xs, sq, rs = xss[c % 2], sqs[c % 2], rss[c % 2]
                xk, sk_, rk = 'xs%d' % (c % 2), 'sq%d' % (c % 2), 'rs%d' % (c % 2)
                pcs = pss[:, (c % 2) * 256:(c % 2) * 256 + 256]
                pk_ = ('pss', c % 2)
                DMA(P, 'sp', xs[:], src3[:, :, c * 256:(c + 1) * 256], [], [xk])
                ACT(P, sq[:], xs[:], AF.Square, [xk], [sk_])
                for kc in range(16):
                    MM(P, pcs, ones_b, sq[:, kc, :], kc == 0, kc == 15, [sk_, 'cstb'], [pk_])
                TS(P, 'dve', rs[:], pcs, 1.0 / D, EPS, ALU.mult, ALU.add, [pk_], [rk])
                ACT(P, rs[:], rs[:], AF.Sqrt, [rk], [rk])
                P.op('dve', lambda e, rs=rs: e.reciprocal(out=rs[:], in_=rs[:]), [rk], [rk])
                for kc in range(16):
                    STT(P, dst[:, kc, c * 256:(c + 1) * 256], xs[:, kc, :], gmix[:, kc:kc + 1], rs[:],
                        ALU.mult, ALU.mult, [xk, rk, 'gmix'], [(dkey, c // 2, c % 2, kc)])

        w_in3 = w_in.rearrange("(kc p) n -> p kc n", p=128)

        with ExitStack() as ph:
            hTf = sb(ph, "hTf", [128, 16, 4096], BF16)
            pss = ps(ph, "pssA", [128, 512], F32)
            with ExitStack() as ph1:
                normT(ph1, xT_full.rearrange("(kc p) t -> p kc t", p=128), 16, hTf, 'hTf', pss)
                P.barrier()
            wg = [sb(ph, "wg%d" % i, [128, 16, 256], BF16) for i in range(2)]
            wf = sb(ph, "wf", [128, 16, 8], BF16)
            kst = [sb(ph, "kst%d" % i, [128, 512], BF16) for i in range(4)]
            vst = [sb(ph, "vst%d" % i, [128, 256], BF16) for i in range(4)]
            pk = [ps(ph, "pk%d" % i, [128, 512], F32) for i in range(4)]
            pf = ps(ph, "pf", [128, 8], F32)
            flg = sb(ph, "flg", [128, 32, 8], F32)
            gi = 0
            nev = 0
            nk = 0
            for (c0, h0) in [(C_KA + 256 * g, 2 * g) for g in range(4)] + [(C_KB + 256 * g, 8 + 2 * g) for g in range(4)]:
                w = wg[gi % 2]
                wk = 'wg%d' % (gi % 2)
                gi += 1
                DMA(P, 'pool', w[:], w_in3[:, :, c0:c0 + 256], [], [wk])
                for hh in range(2):
                    for c in range(8):
                        pt = pk[nev % 4]
                        pkk = 'pk%d' % (nev % 4)
                        for kc in range(16):
                            MM(P, pt[:], w[:, kc, hh * 128:(hh + 1) * 128], hTf[:, kc, c * 512:(c + 1) * 512],
                               kc == 0, kc == 15, [wk, ('hTf', c)], [pkk])
                        ks = kst[nk % 4]
                        kk = 'kst%d' % (nk % 4)
                        nk += 1
                        CP(P, 'act' if nev % 2 == 0 else 'dve', ks[:], pt[:], [pkk], [kk])
                        nev += 1
                        DMA(P, 'sp', kt_scr[h0 + hh][:, c * 512:(c + 1) * 512], ks[:], [kk], [('kt_scr', h0 + hh, c)])
            nv = 0
            for (c0, h0) in [(C_VA + 256 * g, 2 * g) for g in range(4)] + [(C_VB + 256 * g, 8 + 2 * g) for g in range(4)]:
                w = wg[gi % 2]
                wk = 'wg%d' % (gi % 2)
                gi += 1
                DMA(P, 'pool', w[:], w_in3[:, :, c0:c0 + 256], [], [wk])
                for tb in range(32):
                    pt = pk[nev % 4]
                    pkk = 'pk%d' % (nev % 4)
                    for kc in range(16):
                        MM(P, pt[:, 0:256], hTf[:, kc, tb * 128:(tb + 1) * 128], w[:, kc, :], kc == 0, kc == 15,
                           [wk, ('hTf', tb // 4)], [pkk])
                    vs = vst[nv % 4]
                    vk = 'vst%d' % (nv % 4)
                    nv += 1
                    CP(P, 'act' if nev % 2 == 0 else 'dve', vs[:], pt[:, 0:256], [pkk], [vk])
                    nev += 1
                    DMA(P, 'sp', v_scr[h0:h0 + 2, :, tb, :].rearrange("h p d -> p h d"),
                        vs[:].rearrange("p (h d) -> p h d", h=2), [vk], [('v_scr', h0 // 2, tb)])
            DMA(P, 'pool', wf[:], w_in3[:, :, C_F:C_F + 8], [], ['wf'])
            for tb in range(32):
                for kc in range(16):
                    MM(P, pf[:], hTf[:, kc, tb * 128:(tb + 1) * 128], wf[:, kc, :], kc == 0, kc == 15,
                       ['wf', ('hTf', tb // 4)], ['pf'])
                CP(P, 'dve', flg[:, tb, :], pf[:], ['pf'], [('flg', tb)])
            TT(P, 'dve', flg[:], flg[:], bfor[:].unsqueeze(1).to_broadcast([128, 32, 8]), ALU.add,
               ['flg', 'bfor'], ['flg'])
            ACT(P, flg[:], flg[:], AF.Exp, ['flg'], ['flg'], scale=-1.0)
            ACT(P, flg[:], flg[:], AF.Ln, ['flg'], ['flg'], bias=1.0)
            ppre = pk[0]
            ptot = pk[1]
            flat = flg[:].rearrange("p a b -> p (a b)")
            MM(P, ppre[:, 0:256], triu_f, flat, True, True, ['flg', 'cst'], ['pk0'])
            MM(P, ptot[:, 0:256], ones_f, flat, True, True, ['flg', 'cst'], ['pk1'])
            tot = sb(ph, "tot", [128, 32, 8], F32)
            car = sb(ph, "car", [128, 32, 8], F32)
            CP(P, 'dve', tot[:].rearrange("p a b -> p (a b)"), ptot[:, 0:256], ['pk1'], ['tot'])
            P.op('dve', lambda e: e.memset(car[:, 0, :], 0.0), [], ['car'])
            for bk in range(1, 32):
                TT(P, 'dve', car[:, bk, :], car[:, bk - 1, :], tot[:, bk - 1, :], ALU.add, ['car', 'tot'], ['car'])
            TT(P, 'dve', NFt[:].rearrange("p a b -> p (a b)"), ppre[:, 0:256],
               car[:].rearrange("p a b -> p (a b)"), ALU.add, ['pk0', 'car'], ['NFt'])
            NF4 = NFt[:].rearrange("p (i k) h -> p i k h", k=4)
            TS(P, 'dve', nNFo[:], NF4[:, :, 0, :], sel[:, 0:1], None, ALU.mult, None, ['NFt', 'sel'], ['nNFo'])
            for k in range(1, 4):
                STT(P, nNFo[:], NF4[:, :, k, :], sel[:, k:k + 1], nNFo[:], ALU.mult, ALU.add,
                    ['NFt', 'sel', 'nNFo'], ['nNFo'])
            TS(P, 'dve', nNFo[:], nNFo[:], -1.0, None, ALU.mult, None, ['nNFo'], ['nNFo'])
            if DEBUG:
                DMA(P, 'sp', dbg['NF'], NFt[:], ['NFt'], ['dbgNF'])
            P.barrier()

        bc = ExitStack()
        hTo = sb(bc, "hTo", [128, 16, 1024], BF16)
        OT = sb(bc, "OT", [128, 16, 1024], BF16)
        if STAGE >= 2:
            with ExitStack() as ph:
                pss = ps(ph, "pssB", [128, 512], F32)
                with ExitStack() as ph1:
                    normT(ph1, xT_own.rearrange("(kc p) t -> p kc t", p=128), 4, hTo, 'hTo', pss)
                    P.barrier()
                KTs = [sb(ph, "KT%d" % i, [128, 4096], BF16) for i in range(2)]
                VH_ = sb(ph, "VH", [128, 32, 128], BF16)
                VHs = [VH_, VH_]
                wq = sb(ph, "wq", [128, 16, 128], BF16)
                QH = sb(ph, "QH", [128, 1024], BF16)
                Zs = [sb(ph, "Z%d" % i, [128, 4096], F32) for i in range(2)]
                T1 = sb(ph, "T1", [128, 4096], F32)
                PP = sb(ph, "PP", [128, 4096], F32)
                NFbc = sb(ph, "NFbc", [128, 4096], F32)
                Ws = [sb(ph, "W%d" % i, [128, 4096], BF16) for i in range(2)]
                negt = sb(ph, "negt", [128, 4], F32)
                lpart = sb(ph, "lpart", [128, 2, 8], F32)
                lsum = sb(ph, "lsum", [128, 4], F32)
                WT = [sb(ph, "WT%d" % i, [128, 512], BF16) for i in range(2)]
                Ob = sb(ph, "Ob", [128, 128], BF16)
                small = sb(ph, "small", [128, 8], F32)
                zp = [ps(ph, "zp%d" % i, [128, 512], F32) for i in range(2)]
                tp = [ps(ph, "tp%d" % i, [128, 512], BF16) for i in range(2)]
                op_ = ps(ph, "op", [128, 128], F32)
                otp = ps(ph, "otp", [128, 128], BF16)
                scale = 1.0 / math.sqrt(128.0)
                def load_k(hd):
                    DMA(P, 'sp', KTs[hd % 2][:], kt_scr[hd], [('kt_scr', hd)], ['KT%d' % (hd % 2)])

                def load_v(hd):
                    DMA(P, 'sp', VH_[:], v_scr[hd], [('v_scr', hd // 2)], ['VH'])

                def csl(c):
                    return slice(512 * c, 512 * c + 512)

                cnt = {'nz': 0, 'ntp': 0}

                def prologue(hd):
                    is_sb = hd < 8
                    if hd + 1 < 16:
                        load_k(hd + 1)
                    qc = C_QA + hd * 128 if is_sb else C_QB + (hd - 8) * 128
                    DMA(P, 'pool', wq[:], w_in3[:, :, qc:qc + 128], [], ['wq'])
                    src_t, c0_ = (exp_u, 0) if hd < 8 else (exp_v, D)
                    r0 = (hd % 8) * 2048
                    DMA(P, 'pool', uv_bf[r0:r0 + 2048, c0_:c0_ + D], src_t[r0:r0 + 2048, :], [], [('uv_bf', hd)])
                    for half in range(2):
                        for kc in range(16):
                            MM(P, pss[:], wq[:, kc, :], hTo[:, kc, half * 512:(half + 1) * 512], kc == 0, kc == 15,
                               ['wq', 'hTo'], ['pss'])
                        ACT(P, QH[:, half * 512:(half + 1) * 512], pss[:], AF.Copy, ['pss'], ['QH'], scale=scale)
                    if not is_sb:
                        h = hd - 8
                        dg = T1[:].rearrange("p (a b) -> p a b", b=128)
                        TT(P, 'dve', dg, ident_f.unsqueeze(1).to_broadcast([128, 32, 128]),
                           NFt[:, :, h:h + 1].to_broadcast([128, 32, 128]), ALU.mult, ['cst', 'NFt'], ['T1'])
                        for c in range(8):
                            z = zp[cnt['nz'] % 2]
                            zk = 'zp%d' % (cnt['nz'] % 2)
                            cnt['nz'] += 1
                            MM(P, z[:], ones_f, T1[:, csl(c)], True, True, ['cst', 'T1'], [zk])
                            CP(P, 'act', NFbc[:, csl(c)], z[:], [zk], [('NFbc', c)])

                def stage_q(r, hd, i):
                    is_sb = hd < 8
                    nch = i + 1
                    KT, ktk = KTs[hd % 2], 'KT%d' % (hd % 2)
                    Z, zn = Zs[r % 2], 'Z%d' % (r % 2)
                    for c in range(nch):
                        z = zp[cnt['nz'] % 2]
                        zk = 'zp%d' % (cnt['nz'] % 2)
                        cnt['nz'] += 1
                        MM(P, z[:], QH[:, i * 128:(i + 1) * 128], KT[:, csl(c)], True, True, ['QH', ktk], [zk])
                        if is_sb:
                            CP(P, 'act', Z[:, csl(c)], z[:], [zk], [(zn, c)])
                        else:
                            TT(P, 'dve', Z[:, csl(c)], z[:], NFbc[:, csl(c)], ALU.add, [zk, ('NFbc', c)], [(zn, c)])

                def stage_e(r, hd, i):
                    is_sb = hd < 8
                    nch = i + 1
                    L = 512 * nch
                    Z, zn = Zs[r % 2], 'Z%d' % (r % 2)
                    W, wn = Ws[r % 2], 'W%d' % (r % 2)
                    ngc = r % 4
                    lp = lpart[:, r % 2, :]
                    lpk = ('lpart', r % 2)
                    if is_sb:
                        for c in range(nch):
                            ACT(P, T1[:, csl(c)], Z[:, csl(c)], AF.Exp, [(zn, c)], [('T1', c)])
                        for c in range(nch):
                            ACT(P, T1[:, csl(c)], T1[:, csl(c)], AF.Ln, [('T1', c)], [('T1', c)], bias=1.0)
                        TT(P, 'pool', T1[:, csl(i)], T1[:, csl(i)], sb01, ALU.mult, [('T1', i), 'msk'], [('T1', i)])
                        for c in range(nch):
                            init = 0.0 if c == 0 else PP[:, 512 * c - 1:512 * c]
                            o_ap, d0_ap, d1_ap = PP[:, csl(c)], ones_f[:, 0:1].to_broadcast([128, 512]), T1[:, csl(c)]
                            P.op('dve', lambda e, o_ap=o_ap, d0_ap=d0_ap, d1_ap=d1_ap, init=init: e.tensor_tensor_scan(
                                out=o_ap, data0=d0_ap, data1=d1_ap, initial=init, op0=ALU.mult, op1=ALU.add),
                                [('T1', c), 'cst'] + ([('PP', c - 1)] if c else []), [('PP', c)])
                        TS(P, 'dve', negt[:, ngc:ngc + 1], PP[:, L - 1:L], -1.0, None, ALU.mult, None,
                           [('PP', nch - 1)], [('negt', ngc)])
                        for c in range(nch):
                            if c == 0:
                                TT(P, 'dve', Z[:, 1:512], Z[:, 1:512], PP[:, 0:511], ALU.add,
                                   [(zn, 0), ('PP', 0)], [(zn, 0)])
                            else:
                                TT(P, 'dve', Z[:, csl(c)], Z[:, csl(c)], PP[:, 512 * c - 1:512 * c + 511], ALU.add,
                                   [(zn, c), ('PP', c), ('PP', c - 1)], [(zn, c)])
                        TT(P, 'pool', Z[:, csl(i)], Z[:, csl(i)], sbadd, ALU.add, [(zn, i), 'msk'], [(zn, i)])
                    else:
                        TT(P, 'pool', Z[:, csl(i)], Z[:, csl(i)], fxadd, ALU.add, [(zn, i), 'msk'], [(zn, i)])

                def stage_e2(r, hd, i):
                    is_sb = hd < 8
                    nch = i + 1
                    Z, zn = Zs[r % 2], 'Z%d' % (r % 2)
                    W, wn = Ws[r % 2], 'W%d' % (r % 2)
                    ngc = r % 4
                    lp = lpart[:, r % 2, :]
                    lpk = ('lpart', r % 2)
                    if is_sb:
                        for c in range(nch):
                            ACT(P, W[:, csl(c)], Z[:, csl(c)], AF.Exp, [(zn, c), ('negt', ngc)], [(wn, c)],
                                bias=negt[:, ngc:ngc + 1])
                    else:
                        for c in range(nch):
                            ACT(P, W[:, csl(c)], Z[:, csl(c)], AF.Exp, [(zn, c), 'nNFo'], [(wn, c), lpk + (c,)],
                                bias=nNFo[:, i, hd - 8:hd - 7], accum_out=lp[:, c:c + 1])
                        P.op('dve', lambda e, lp=lp, nch=nch, ngc=ngc: e.reduce_sum(
                            out=lsum[:, ngc:ngc + 1], in_=lp[:, 0:nch], axis=AX.X), [lpk], [('lsum', ngc)])
                        P.op('dve', lambda e, ngc=ngc: e.reciprocal(out=lsum[:, ngc:ngc + 1], in_=lsum[:, ngc:ngc + 1]),
                             [('lsum', ngc)], [('lsum', ngc)])

                def stage_b(r, hd, i):
                    is_sb = hd < 8
                    nch = i + 1
                    W, wn = Ws[r % 2], 'W%d' % (r % 2)
                    ngc = r % 4
                    nkb = 4 * nch
                    slots = []

                    def tr_group(g):
                        t = tp[cnt['ntp'] % 2]
                        tk = 'tp%d' % (cnt['ntp'] % 2)
                        wt = WT[cnt['ntp'] % 2]
                        wtk = 'WT%d' % (cnt['ntp'] % 2)
                        cnt['ntp'] += 1
                        for q in range(4):
                            kb = 4 * g + q
                            TR(P, t[:, q * 128:(q + 1) * 128], W[:, kb * 128:(kb + 1) * 128], ident_b,
                               [(wn, g), 'cstb'], [tk])
                        CP(P, 'dve', wt[:], t[:], [tk], [wtk])
                        slots.append((wt, wtk))

                    def pv_group(g):
                        wt, wtk = slots[g]
                        for q in range(4):
                            kb = 4 * g + q
                            MM(P, op_[:], wt[:, q * 128:(q + 1) * 128], VH_[:, kb, :], kb == 0, kb == nkb - 1,
                               [wtk, 'VH'], ['op'])
                    tr_group(0)
                    for g in range(nch):
                        if g + 1 < nch:
                            tr_group(g + 1)
                        pv_group(g)
                    if is_sb:
                        CP(P, 'dve', Ob[:], op_[:], ['op'], ['Ob'])
                    else:
                        TS(P, 'dve', Ob[:], op_[:], lsum[:, ngc:ngc + 1], None, ALU.mult, None,
                           ['op', ('lsum', ngc)], ['Ob'])
                    TR(P, otp[:], Ob[:], ident_b, ['Ob', 'cstb'], ['otp'])
                    CP(P, 'dve', OT[:, hd, i * 128:(i + 1) * 128], otp[:], ['otp'], [('OT', hd)])

                rows = [(hd, i) for hd in range(16) for i in range(8)]
                nr = len(rows)
                load_k(0)
                for it in range(nr + 2):
                    if 0 <= it - 1 < nr:
                        hd, i = rows[it - 1]
                        stage_e(it - 1, hd, i)
                    if it < nr:
                        hd, i = rows[it]
                        if i == 0:
                            prologue(hd)
                        stage_q(it, hd, i)
                    if 0 <= it - 2 < nr:
                        hd, i = rows[it - 2]
                        if i == 0:
                            load_v(hd)
                        stage_b(it - 2, hd, i)
                    if 0 <= it - 1 < nr:
                        hd, i = rows[it - 1]
                        stage_e2(it - 1, hd, i)
                if DEBUG:
                    DMA(P, 'sp', dbg['OT'], OT[:], ['OT'], ['dbgOT'])
                P.barrier()

        if STAGE >= 3:
            with ExitStack() as ph:
                mT = sb(ph, "mT", [128, 16, 1024], BF16)
                wga = sb(ph, "wga", [128, 16, 512], BF16)
                wgb = sb(ph, "wgb", [128, 16, 512], BF16)
                wa = sb(ph, "wa", [128, 8, 512], BF16)
                wb = sb(ph, "wb", [128, 8, 512], BF16)
                sga = sb(ph, "sga", [128, 512], F32)
                sgb = sb(ph, "sgb", [128, 512], F32)
                m1 = sb(ph, "m1", [128, 512], F32)
                m2 = sb(ph, "m2", [128, 512], F32)
                pc = [ps(ph, "pc%d" % i, [128, 512], F32) for i in range(8)]
                w_ba3 = w_ba.rearrange("(kc p) n -> p kc n", p=128)
                w_bb3 = w_bb.rearrange("(kc p) n -> p kc n", p=128)
                w_out3 = w_out.rearrange("(kc p) n -> p kc n", p=128)
                xo = [sb(ph, "xo%d" % i, [128, D], F32) for i in range(2)]
                it = 0
                for ng in range(4):
                    DMA(P, 'pool', wga[:], w_in3[:, :, C_GA + ng * 512:C_GA + (ng + 1) * 512], [], ['wga'])
                    DMA(P, 'pool', wgb[:], w_in3[:, :, C_GB + ng * 512:C_GB + (ng + 1) * 512], [], ['wgb'])
                    DMA(P, 'pool', wa[:], w_ba3[:, :, ng * 512:(ng + 1) * 512], [], ['wa'])
                    DMA(P, 'pool', wb[:], w_bb3[:, :, ng * 512:(ng + 1) * 512], [], ['wb'])
                    for nt in range(4):
                        ns = slice(nt * 128, (nt + 1) * 128)
                        for half in range(2):
                            hs = slice(half * 512, (half + 1) * 512)
                            b0 = 4 * (it % 2)
                            it += 1
                            pga, pgb, pya, pyb = pc[b0], pc[b0 + 1], pc[b0 + 2], pc[b0 + 3]
                            kga, kgb, kya, kyb = ['pc%d' % (b0 + x) for x in range(4)]
                            for kc in range(16):
                                MM(P, pga[:], wga[:, kc, ns], hTo[:, kc, hs], kc == 0, kc == 15, ['wga', 'hTo'], [kga])
                            for kc in range(16):
                                MM(P, pgb[:], wgb[:, kc, ns], hTo[:, kc, hs], kc == 0, kc == 15, ['wgb', 'hTo'], [kgb])
                            for kc in range(8):
                                MM(P, pya[:], wa[:, kc, ns], OT[:, kc, hs], kc == 0, kc == 7, ['wa', 'OT'], [kya])
                            for kc in range(8):
                                MM(P, pyb[:], wb[:, kc, ns], OT[:, 8 + kc, hs], kc == 0, kc == 7, ['wb', 'OT'], [kyb])
                            ACT(P, sga[:], pga[:], AF.Sigmoid, [kga], ['sga'])
                            ACT(P, sgb[:], pgb[:], AF.Sigmoid, [kgb], ['sgb'])
                            TT(P, 'dve', m1[:], sga[:], pya[:], ALU.mult, ['sga', kya], ['m1'])
                            TT(P, 'dve', m2[:], sgb[:], pyb[:], ALU.mult, ['sgb', kyb], ['m2'])
                            TT(P, 'pool', mT[:, ng * 4 + nt, hs], m1[:], m2[:], ALU.add, ['m1', 'm2'], ['mT'])
                wo = sb(ph, "wo", [128, 16, 512], BF16)
                it = 0
                for ng in range(4):
                    DMA(P, 'pool', wo[:], w_out3[:, :, ng * 512:(ng + 1) * 512], [], ['wo'])
                    for tb in range(8):
                        pt = pc[it % 4]
                        pk_ = 'pc%d' % (it % 4)
                        xb_ = xo[it % 2]
                        xk = 'xo%d' % (it % 2)
                        it += 1
                        cs = slice(ng * 512, (ng + 1) * 512)
                        DMA(P, 'sp', xb_[:, 0:512], x_own[tb * 128:(tb + 1) * 128, cs], [], [xk])
                        for kc in range(16):
                            MM(P, pt[:], mT[:, kc, tb * 128:(tb + 1) * 128], wo[:, kc, :], kc == 0, kc == 15,
                               ['mT', 'wo'], [pk_])
                        TT(P, 'dve', xb_[:, 0:512], xb_[:, 0:512], pt[:], ALU.add, [xk, pk_], [xk])
                        DMA(P, 'sp', x1_scr[tb][:, cs], xb_[:, 0:512], [xk], [('x1s', tb, ng)])
                        if DEBUG:
                            DMA(P, 'sp', dbg['x1'][tb][:, cs], xb_[:, 0:512], [xk], [('dbgx1', tb, ng)])
                P.barrier()

        bc.close()
        if STAGE >= 4:
            with ExitStack() as ph:
                gffn = sb(ph, "gffn", [128, D], F32)
                gfin = sb(ph, "gfin", [128, D], F32)
                skb = sb(ph, "skb", [128, 16, 128], BF16)
                rstd2 = sb(ph, "rstd2", [128, 8], F32)
                DMA(P, 'sp', gffn[:], gffn_in[:, :], [], ['gffn'])
                DMA(P, 'sp', gfin[:], gfin_in[:, :], [], ['gfin'])
                DMA(P, 'pool', skb[:], skT.rearrange("ch c n -> c ch n"), [], ['skb'])
                pd = [ps(ph, "pd%d" % i, [128, 512], F32) for i in range(4)]
                jb = sb(ph, "jb", [128, D], BF16)
                x1bs = [sb(ph, "x1b%d" % i, [128, D], F32) for i in range(2)]
                x1b = x1bs[0]
                with ExitStack() as ph1:
                    pdb = [ps(ph1, "pdb%d" % i, [128, 512], BF16) for i in range(2)]
                    h2T = sb(ph1, "h2T", [128, 16, 1024], BF16)
                    qT = sb(ph1, "qT", [128, 16, 1024], BF16)
                    h2b = sb(ph1, "h2b", [128, D], BF16)
                    wqp = sb(ph1, "wqp", [128, 16, 512], BF16)
                    w_q3 = w_query.rearrange("(kc p) n -> p kc n", p=128)
                    ntp = 0
                    for tb in range(8):
                        DMA(P, 'sp', x1b[:], x1_scr[tb], [('x1s', tb)], ['x1b'])
                        ACT(P, jb[:], x1b[:], AF.Square, ['x1b'], ['jb', ('rstd2', tb)],
                            accum_out=rstd2[:, tb:tb + 1])
                        TS(P, 'dve', rstd2[:, tb:tb + 1], rstd2[:, tb:tb + 1], 1.0 / D, EPS, ALU.mult, ALU.add,
                           [('rstd2', tb)], [('rstd2', tb)])
                        ACT(P, rstd2[:, tb:tb + 1], rstd2[:, tb:tb + 1], AF.Sqrt, [('rstd2', tb)], [('rstd2', tb)])
                        P.op('dve', lambda e, tb=tb: e.reciprocal(out=rstd2[:, tb:tb + 1], in_=rstd2[:, tb:tb + 1]),
                             [('rstd2', tb)], [('rstd2', tb)])
                        STT(P, h2b[:], x1b[:], rstd2[:, tb:tb + 1], gffn[:], ALU.mult, ALU.mult,
                            ['x1b', ('rstd2', tb), 'gffn'], ['h2b'])
                        for g in range(4):
                            t = pdb[ntp % 2]
                            tk = 'pdb%d' % (ntp % 2)
                            ntp += 1
                            for q in range(4):
                                kc = 4 * g + q
                                TR(P, t[:, q * 128:(q + 1) * 128], h2b[:, kc * 128:(kc + 1) * 128], ident_b,
                                   ['h2b', 'cstb'], [tk])
                            CP(P, 'act', h2T[:, 4 * g:4 * g + 4, tb * 128:(tb + 1) * 128],
                               t[:].rearrange("p (a b) -> p a b", a=4), [tk], ['h2T'])
                    it = 0
                    for ng in range(4):
                        DMA(P, 'pool', wqp[:], w_q3[:, :, ng * 512:(ng + 1) * 512], [], ['wqp'])
                        for nt in range(4):
                            for half in range(2):
                                pt = pd[it % 4]
                                pk_ = 'pd%d' % (it % 4)
                                it += 1
                                for kc in range(16):
                                    MM(P, pt[:], wqp[:, kc, nt * 128:(nt + 1) * 128],
                                       h2T[:, kc, half * 512:(half + 1) * 512], kc == 0, kc == 15, ['wqp', 'h2T'], [pk_])
                                CP(P, 'act' if it % 2 else 'dve', qT[:, ng * 4 + nt, half * 512:(half + 1) * 512], pt[:],
                                   [pk_], ['qT'])
                    DMA(P, 'sp', q_scr, qT[:], ['qT'], ['q_scr'])
                    P.barrier()
                po = [ps(ph, "po%d" % i, [128, 512], F32) for i in range(4)]
                sc = sb(ph, "sc", [128, 16, 128], F32)
                tmp = sb(ph, "tmp", [128, 256], F32)
                v16 = sb(ph, "v16", [128, 8, 2, 16], F32)
                ix = sb(ph, "ix", [128, 8, 2, 16], U32)
                ixf = sb(ph, "ixf", [128, 8, 2, 16], F32)
                cand = sb(ph, "cand", [128, 8, 16, 16], F32)
                cid = sb(ph, "cid", [128, 8, 16, 16], F32)
                t16 = sb(ph, "t16", [128, 8, 16], F32)
                pos = sb(ph, "pos", [128, 8, 16], U32)
                posf = sb(ph, "posf", [128, 8, 16], F32)
                iot = sb(ph, "iot", [128, 256], F32)
                DMA(P, 'sp', iot[:], iota_in[:, :], [], ['iot'])
                idf = sb(ph, "idf", [128, 128], F32)
                jk2 = sb(ph, "jk2", [128, 256], F32)
                idus = [sb(ph, "idu%d" % i, [128, 128], U32) for i in range(2)]
                gts = [sb(ph, "gt%d" % i, [128, 8, 16], F32) for i in range(2)]
                gs = sb(ph, "gs", [128, 8], F32)
                aa = sb(ph, "aa", [128, 128], F32)
                ww = sb(ph, "ww", [128, 128], F32)
                g1 = sb(ph, "g1", [128, 128], F32)
                h2f = sb(ph, "h2f", [128, D], F32)
                qTbs = [sb(ph, "qTb%d" % i, [128, 16, 128], BF16) for i in range(2)]
                ssf = sb(ph, "ssf", [128, 2], F32)
                GSZ = 4
                dgs = [sb(ph, "dg%d" % i, [128, GSZ, 128], BF16) for i in range(2)]
                NG_, NVR = 8, 8
                gbs = [sb(ph, "gb%d" % i, [128, 2 * D], BF16) for i in range(NG_)]
                vrs = [sb(ph, "vr%d" % i, [128, D], BF16) for i in range(NVR)]
                ngb = [0]

                def topk(tb):
                    idu = idus[tb % 2]
                    ik = 'idu%d' % (tb % 2)
                    gt = gts[tb % 2]
                    gk = 'gt%d' % (tb % 2)
                    qTb = qTbs[tb % 2]
                    qk = 'qTb%d' % (tb % 2)
                    DMA(P, 'sp', qTb[:], q_scr[:, :, tb * 128:(tb + 1) * 128], ['q_scr'], [qk])
                    for g in range(4):
                        for q in range(4):
                            ch = 4 * g + q
                            MM(P, pd[g][:, q * 128:(q + 1) * 128], qTb[:, ch, :], skb[:, ch, :],
                               True, True, [qk, 'skb'], ['pd%d' % g])
                        CP(P, 'act', sc[:, 4 * g:4 * g + 4, :], pd[g][:].rearrange("p (a b) -> p a b", a=4),
                           ['pd%d' % g], ['sc'])
                    for ch in range(16):
                        hh, pp = ch // 2, ch % 2
                        P.op('dve', lambda e, ch=ch, hh=hh, pp=pp: e.max(out=v16[:, hh, pp, 0:8], in_=sc[:, ch, :]),
                             ['sc'], ['v16'])
                        P.op('dve', lambda e, ch=ch, hh=hh, pp=pp: e.max_index(
                            out=ix[:, hh, pp, 0:8], in_max=v16[:, hh, pp, 0:8], in_values=sc[:, ch, :]),
                            ['sc', 'v16'], ['ix'])
                        P.op('dve', lambda e, ch=ch, hh=hh, pp=pp: e.match_replace(
                            out=tmp[:, 0:128], in_to_replace=v16[:, hh, pp, 0:8], in_values=sc[:, ch, :],
                            imm_value=-1e30), ['sc', 'v16'], ['tmp'])
                        P.op('dve', lambda e, ch=ch, hh=hh, pp=pp: e.max(out=v16[:, hh, pp, 8:16], in_=tmp[:, 0:128]),
                             ['tmp'], ['v16'])
                        P.op('dve', lambda e, ch=ch, hh=hh, pp=pp: e.max_index(
                            out=ix[:, hh, pp, 8:16], in_max=v16[:, hh, pp, 8:16], in_values=tmp[:, 0:128]),
                            ['tmp', 'v16'], ['ix'])
                    CP(P, 'dve', ixf[:], ix[:], ['ix'], ['ixf'])
                    TS(P, 'dve', ixf[:, :, 0, :], ixf[:, :, 0, :], 128.0, None, ALU.mult, None, ['ixf'], ['ixf'])
                    TT(P, 'dve', cand[:], v16[:, :, 0, :].unsqueeze(3).to_broadcast([128, 8, 16, 16]),
                       v16[:, :, 1, :].unsqueeze(2).to_broadcast([128, 8, 16, 16]), ALU.add, ['v16'], ['cand'])
                    TT(P, 'dve', cid[:], ixf[:, :, 0, :].unsqueeze(3).to_broadcast([128, 8, 16, 16]),
                       ixf[:, :, 1, :].unsqueeze(2).to_broadcast([128, 8, 16, 16]), ALU.add, ['ixf'], ['cid'])
                    for hh in range(8):
                        cf = cand[:, hh].rearrange("p a b -> p (a b)")
                        P.op('dve', lambda e, hh=hh, cf=cf: e.max(out=t16[:, hh, 0:8], in_=cf), ['cand'], ['t16'])
                        P.op('dve', lambda e, hh=hh, cf=cf: e.max_index(
                            out=pos[:, hh, 0:8], in_max=t16[:, hh, 0:8], in_values=cf), ['cand', 't16'], ['pos'])
                        P.op('dve', lambda e, hh=hh, cf=cf: e.match_replace(
                            out=tmp[:], in_to_replace=t16[:, hh, 0:8], in_values=cf, imm_value=-1e30),
                            ['cand', 't16'], ['tmp'])
                        P.op('dve', lambda e, hh=hh: e.max(out=t16[:, hh, 8:16], in_=tmp[:]), ['tmp'], ['t16'])
                        P.op('dve', lambda e, hh=hh: e.max_index(
                            out=pos[:, hh, 8:16], in_max=t16[:, hh, 8:16], in_values=tmp[:]), ['tmp', 't16'], ['pos'])
                    CP(P, 'dve', posf[:], pos[:], ['pos'], ['posf'])
                    for hh in range(8):
                        cidf = cid[:, hh].rearrange("p a b -> p (a b)")
                        for k in range(16):
                            STT(P, jk2[:], iot[:], posf[:, hh, k:k + 1], cidf, ALU.is_equal, ALU.mult,
                                ['iot', 'cid', 'posf'], [('idf', hh * 16 + k)],
                                accum_out=idf[:, hh * 16 + k:hh * 16 + k + 1])
                    CP(P, 'dve', idu[:], idf[:], ['idf'], [ik])
                    if DEBUG:
                        DMA(P, 'sp', dbg['ids'][:, tb, :], idf[:], ['idf'], [('dbgids', tb)])
                    TT(P, 'dve', gt[:], t16[:], t16[:, :, 0:1].to_broadcast([128, 8, 16]), ALU.subtract, ['t16'], [gk])
                    ACT(P, gt[:], gt[:], AF.Exp, [gk], [gk])
                    P.op('dve', lambda e, gt=gt: e.reduce_sum(out=gs[:], in_=gt[:], axis=AX.X), [gk], ['gs'])
                    P.op('dve', lambda e: e.reciprocal(out=gs[:], in_=gs[:]), ['gs'], ['gs'])
                    TT(P, 'dve', gt[:], gt[:], gs[:].unsqueeze(2).to_broadcast([128, 8, 16]), ALU.mult, [gk, 'gs'], [gk])

                def gather(idu, ik, hk):
                    k = ngb[0]
                    ngb[0] += 1
                    bfr, bk = gbs[k % NG_], 'gb%d' % (k % NG_)
                    vr, vk = vrs[k % NVR], 'vr%d' % (k % NVR)
                    P.dma('pool', lambda e, bfr=bfr, hk=hk: e.indirect_dma_start(
                        out=bfr[:, :], out_offset=None, in_=uv_bf[:, :],
                        in_offset=bass.IndirectOffsetOnAxis(ap=idu[:, hk:hk + 1], axis=0)), [ik, 'uv_bf'], [bk])
                    CP(P, 'act', vr[:], bfr[:, D:2 * D], [bk], [vk])
                    return bfr[:, 0:D], bk, vr, vk

                def u_prep(tb):
                    x1b = x1bs[tb % 2]
                    xk = 'x1b%d' % (tb % 2)
                    DMA(P, 'sp', x1b[:], x1_scr[tb], [('x1s', tb)], [xk])
                    STT(P, h2f[:], x1b[:], rstd2[:, tb:tb + 1], gffn[:], ALU.mult, ALU.mult,
                        [xk, 'rstd2', 'gffn'], ['h2f'])

                ngrp = [0]

                def grp_dots(tb, g, j0, j1, st):
                    idu = idus[tb % 2]
                    ik = 'idu%d' % (tb % 2)
                    for j in range(j0, j1):
                        hk = g * GSZ + j
                        ub, uk, vb, vk = gather(idu, ik, hk)
                        st['v'].append((vb, vk))
                        STT(P, jb[:], ub, 1.0, h2f[:], ALU.mult, ALU.mult, [uk, 'h2f'], [('aa', hk)],
                            accum_out=aa[:, hk:hk + 1])

                def grp_gelu_a(tb, g):
                    hs = slice(g * GSZ, (g + 1) * GSZ)
                    ak = [('aa', g * GSZ + j) for j in range(GSZ)]
                    g1k = ('g1', g % 2)
                    TT(P, 'dve', g1[:, hs], aa[:, hs], aa[:, hs], ALU.mult, ak, [g1k])
                    TS(P, 'dve', g1[:, hs], g1[:, hs], 0.044715, 1.0, ALU.mult, ALU.add, [g1k], [g1k])
                    TT(P, 'dve', g1[:, hs], g1[:, hs], aa[:, hs], ALU.mult, [g1k] + ak, [g1k])
                    ACT(P, g1[:, hs], g1[:, hs], AF.Sigmoid, [g1k], [g1k], scale=2.0 * math.sqrt(2.0 / math.pi))

                def grp_finish(tb, g, st):
                    gt = gts[tb % 2]
                    gk = 'gt%d' % (tb % 2)
                    dg = dgs[ngrp[0] % 2]
                    dk = 'dg%d' % (ngrp[0] % 2)
                    ngrp[0] += 1
                    hs = slice(g * GSZ, (g + 1) * GSZ)
                    ak = [('aa', g * GSZ + j) for j in range(GSZ)]
                    g1k = ('g1', g % 2)
                    TT(P, 'dve', g1[:, hs], g1[:, hs], aa[:, hs], ALU.mult, [g1k] + ak, [g1k])
                    TT(P, 'dve', ww[:, hs], g1[:, hs], gt[:].rearrange("p a b -> p (a b)")[:, hs], ALU.mult,
                       [g1k, gk], [('ww', g % 2)])
                    for j in range(GSZ):
                        hk = g * GSZ + j
                        ACT(P, dg[:, j, :], ident_b, AF.Copy, ['cstb', ('ww', g % 2)], [(dk, j)],
                            scale=ww[:, hk:hk + 1])
                    for j in range(GSZ):
                        hk = g * GSZ + j
                        vb, vk = st['v'][j]
                        for c in range(4):
                            MM(P, po[c][:], dg[:, j, :], vb[:, c * 512:(c + 1) * 512], hk == 0, hk == 127,
                               [(dk, j), vk], ['po%d' % c])

                def final(tb):
                    x1b = x1bs[tb % 2]
                    xk = 'x1b%d' % (tb % 2)
                    if DEBUG:
                        DMA(P, 'sp', dbg['gw'][:, tb, :], ww[:], ['ww'], [('dbggw', tb)])
                    for c in range(4):
                        TT(P, 'dve', x1b[:, c * 512:(c + 1) * 512], x1b[:, c * 512:(c + 1) * 512], po[c][:], ALU.add,
                           [xk, 'po%d' % c], [xk])
                    if DEBUG:
                        DMA(P, 'sp', dbg['acc'][tb], x1b[:], [xk], [('dbgacc', tb)])
                    ACT(P, jb[:], x1b[:], AF.Square, [xk], ['ssf'], accum_out=ssf[:, 0:1])
                    TS(P, 'dve', ssf[:, 0:1], ssf[:, 0:1], 1.0 / D, EPS, ALU.mult, ALU.add, ['ssf'], ['ssf'])
                    ACT(P, ssf[:, 0:1], ssf[:, 0:1], AF.Sqrt, ['ssf'], ['ssf'])
                    P.op('dve', lambda e: e.reciprocal(out=ssf[:, 0:1], in_=ssf[:, 0:1]), ['ssf'], ['ssf'])
                    STT(P, x1b[:], x1b[:], ssf[:, 0:1], gfin[:], ALU.mult, ALU.mult, [xk, 'ssf', 'gfin'], [xk])
                    DMA(P, 'sp', out[tb * 128:(tb + 1) * 128, :], x1b[:], [xk], [('out', tb)])

                topk(0)
                for tb in range(8):
                    u_prep(tb)
                    NG = 128 // GSZ
                    prev = None
                    for g in range(NG):
                        st_ = {'v': []}
                        grp_dots(tb, g, 0, GSZ // 2, st_)
                        if prev is not None:
                            grp_finish(tb, g - 1, prev)
                        grp_dots(tb, g, GSZ // 2, GSZ, st_)
                        grp_gelu_a(tb, g)
                        prev = st_
                        if g == 7 and tb + 1 < 8:
                            topk(tb + 1)
                    grp_finish(tb, NG - 1, prev)
                    final(tb)
                P.barrier()
        P.barrier()
        P.emit()
    return nc


def make_core_inputs(inputs):
    x = np.asarray(inputs["x"], dtype=np.float32)
    w_in = np.ascontiguousarray(np.asarray(inputs["w_in"], dtype=np.float32)[0])
    w_ba = np.ascontiguousarray(np.asarray(inputs["w_branch_a"], dtype=np.float32)[0])
    w_bb = np.ascontiguousarray(np.asarray(inputs["w_branch_b"], dtype=np.float32)[0])
    w_out = np.ascontiguousarray(np.asarray(inputs["w_out"], dtype=np.float32)[0])
    w_query = np.ascontiguousarray(np.asarray(inputs["w_query"], dtype=np.float32)[0])
    sk = np.asarray(inputs["sub_keys"], dtype=np.float32)[0]
    skT = np.ascontiguousarray(sk.transpose(0, 1, 3, 2).reshape(16, 128, 128))
    exp_u = np.ascontiguousarray(np.asarray(inputs["expert_u"], dtype=np.float32)[0])
    exp_v = np.ascontiguousarray(np.asarray(inputs["expert_v"], dtype=np.float32)[0])
    gmix = np.ascontiguousarray(np.asarray(inputs["norm_mix_gain"], dtype=np.float32)[0].reshape(16, 128).T)
    gffn = np.ascontiguousarray(np.broadcast_to(np.asarray(inputs["norm_ffn_gain"], dtype=np.float32)[0][None, :], (128, D)))
    gfin = np.ascontiguousarray(np.broadcast_to(np.asarray(inputs["norm_final_gain"], dtype=np.float32)[None, :], (128, D)))
    bfor = np.ascontiguousarray(np.broadcast_to(np.asarray(inputs["b_forget"], dtype=np.float32)[0][None, :], (128, 8)))
    idx = np.arange(128)
    ident = np.eye(128, dtype=np.float32)
    triu = (idx[:, None] <= idx[None, :]).astype(np.float32)
    cst = np.ascontiguousarray(np.concatenate([ident, triu, np.ones((128, 128), np.float32)], axis=1))
    xT = [np.ascontiguousarray(x[b].T) for b in range(2)]
    iota = np.ascontiguousarray(np.broadcast_to(np.arange(256, dtype=np.float32)[None, :], (128, 256)))
    maps = []
    toks = []
    for c in range(8):
        b, j = c // 4, c % 4
        tok = np.concatenate([np.arange((4 * i + j) * 128, (4 * i + j + 1) * 128) for i in range(8)])
        toks.append((b, tok))
        sb01 = np.zeros((128, 4, 128), np.float32)
        fx01 = np.zeros((128, 4, 128), np.float32)
        for k in range(4):
            if k < j:
                sb01[:, k, :] = 1.0
                fx01[:, k, :] = 1.0
            elif k == j:
                sb01[:, k, :] = (idx[None, :] < idx[:, None])
                fx01[:, k, :] = (idx[None, :] <= idx[:, None])
        sbadd = (1.0 - sb01) * NEG
        fxadd = (1.0 - fx01) * NEG
        msk = np.ascontiguousarray(np.concatenate(
            [sb01.reshape(128, 512), sbadd.reshape(128, 512), fxadd.reshape(128, 512), np.zeros((128, 512), np.float32)],
            axis=1).astype(np.float32))
        sel = np.zeros((128, 4), np.float32)
        sel[:, j] = 1.0
        maps.append({
            "xT_full": xT[b], "xT_own": np.ascontiguousarray(xT[b][:, tok]), "x_own": np.ascontiguousarray(x[b][tok]),
            "w_in": w_in, "w_ba": w_ba, "w_bb": w_bb, "w_out": w_out, "w_query": w_query, "skT": skT,
            "exp_u": exp_u, "exp_v": exp_v, "gmix": gmix, "gffn": gffn, "gfin": gfin, "bfor": bfor,
            "cst": cst, "msk": msk, "sel": sel, "iota": iota,
        })
    return maps, toks


def kernel(**inputs):
    maps, toks = make_core_inputs(inputs)
    nc = build_program()
    res = run_bass_kernel_spmd(nc, maps, core_ids=list(range(8)))
    outp = np.zeros((2, 4096, D), np.float32)
    for c in range(8):
        b, tok = toks[c]
        outp[b, tok] = np.asarray(res.results[c]["out"], dtype=np.float32)
    return outp
```

```python
import math
from contextlib import ExitStack

import numpy as np
import concourse.bass as bass
import concourse.mybir as mybir
from concourse.bass_utils import run_bass_kernel_spmd

F32 = mybir.dt.float32
BF16 = mybir.dt.bfloat16
U32 = mybir.dt.uint32
AF = mybir.ActivationFunctionType
ALU = mybir.AluOpType
AX = mybir.AxisListType

ENGS = ['pe', 'act', 'dve', 'pool', 'sp']
NRING = 8
EPS = 1e-6
D = 2048
NEG = -30000.0
STAGE = 99
DEBUG = False


def _conflict(a, b):
    n = min(len(a), len(b))
    return a[:n] == b[:n]


class Prog:
    def __init__(self, nc, stack):
        self.nc = nc
        self.ops = {e: [] for e in ENGS}
        self.ncomp = {e: 0 for e in ENGS}
        self.ndma = {e: 0 for e in ENGS}
        self.waited = {e: {} for e in ENGS}
        self.track = {}
        self.S = {e: stack.enter_context(nc.semaphore('S_' + e)) for e in ['pe', 'act', 'dve', 'pool']}
        self.Dm = {e: [stack.enter_context(nc.semaphore('D_%s%d' % (e, i))) for i in range(NRING)]
                   for e in ['sp', 'act', 'pool']}

    def _sem_of(self, dep):
        kind, e, i = dep
        if kind == 'c':
            return ('S', e), self.S[e], i + 1
        return ('D', e, i % NRING), self.Dm[e][i % NRING], 16 * (i // NRING + 1)

    def _collect(self, reads, writes, me):
        deps = set()
        reads = [r if isinstance(r, tuple) else (r,) for r in reads]
        writes = [w if isinstance(w, tuple) else (w,) for w in writes]
        for r in reads:
            tr = self.track.setdefault(r[0], {})
            for k, (lw, rd) in tr.items():
                if _conflict(k, r) and lw is not None:
                    deps.add(lw)
        for w in writes:
            tr = self.track.setdefault(w[0], {})
            for k, (lw, rd) in tr.items():
                if _conflict(k, w):
                    if lw is not None:
                        deps.add(lw)
                    deps.update(rd)
        for w in writes:
            tr = self.track[w[0]]
            for k in [k for k in tr if _conflict(k, w) and len(k) > len(w)]:
                del tr[k]
            tr[w] = [me, []]
        for r in reads:
            tr = self.track[r[0]]
            if r not in tr:
                lw = None
                for k, (lw2, rd) in tr.items():
                    if _conflict(k, r) and len(k) < len(r) and lw2 is not None:
                        lw = lw2
                tr[r] = [lw, []]
            tr[r][1].append(me)
        deps.discard(me)
        return deps

    def _waits(self, eng, deps, is_dma):
        waits = []
        for dep in sorted(deps):
            kind, e, i = dep
            if kind == 'c' and e == eng and not is_dma and eng == 'pe':
                continue
            key, sem, val = self._sem_of(dep)
            if self.waited[eng].get(key, 0) >= val:
                continue
            self.waited[eng][key] = val
            waits.append((sem, val))
        return waits

    def op(self, eng, fn, reads=(), writes=()):
        me = ('c', eng, self.ncomp[eng])
        deps = self._collect(list(reads), list(writes), me)
        waits = self._waits(eng, deps, False)
        self.ncomp[eng] += 1
        self.ops[eng].append((waits, fn, (self.S[eng], 1)))

    def dma(self, eng, fn, reads=(), writes=()):
        k = self.ndma[eng]
        me = ('d', eng, k)
        deps = self._collect(list(reads), list(writes), me)
        if k >= NRING:
            deps.add(('d', eng, k - NRING))
        waits = self._waits(eng, deps, True)
        self.ndma[eng] += 1
        self.ops[eng].append((waits, fn, (self.Dm[eng][k % NRING], 16)))

    def barrier(self):
        allw = []
        for x in ['pe', 'act', 'dve', 'pool']:
            if self.ncomp[x] > 0:
                allw.append((('S', x), self.S[x], self.ncomp[x]))
        for q in ['sp', 'act', 'pool']:
            for r in range(NRING):
                n = 0 if self.ndma[q] <= r else (self.ndma[q] - r + NRING - 1) // NRING
                if n > 0:
                    allw.append((('D', q, r), self.Dm[q][r], 16 * n))
        for e in ENGS:
            waits = []
            for key, sem, val in allw:
                if self.waited[e].get(key, 0) >= val:
                    continue
                self.waited[e][key] = val
                waits.append((sem, val))
            self.ops[e].append((waits, None, None))

    def emit(self):
        nc = self.nc
        names = {'pe': 'tensor', 'act': 'scalar', 'dve': 'vector', 'pool': 'gpsimd', 'sp': 'sync'}
        with nc.Block() as block:
            for e in ENGS:
                ops = self.ops[e]

                def body(engine, ops=ops):
                    for waits, fn, inc in ops:
                        for sem, val in waits:
                            engine.wait_ge(sem, val)
                        if fn is not None:
                            ins = fn(engine)
                            ins.then_inc(inc[0], inc[1])
                getattr(block, names[e])(body)


def DMA(P, q, out, in_, reads, writes):
    P.dma(q, lambda e: e.dma_start(out=out, in_=in_), reads, writes)


def MM(P, out, lhsT, rhs, start, stop, reads, writes):
    P.op('pe', lambda e: e.matmul(out, lhsT=lhsT, rhs=rhs, start=start, stop=stop), reads, writes)


def TR(P, out, in_, ident, reads, writes):
    P.op('pe', lambda e: e.transpose(out=out, in_=in_, identity=ident), reads, writes)


def ACT(P, out, in_, func, reads, writes, **kw):
    P.op('act', lambda e: e.activation(out=out, in_=in_, func=func, **kw), reads, writes)


def TT(P, eng, out, in0, in1, op, reads, writes):
    P.op(eng, lambda e: e.tensor_tensor(out=out, in0=in0, in1=in1, op=op), reads, writes)


def TS(P, eng, out, in0, s1, s2, op0, op1, reads, writes, **kw):
    if op1 is None:
        P.op(eng, lambda e: e.tensor_scalar(out=out, in0=in0, scalar1=s1, scalar2=None, op0=op0, **kw), reads, writes)
    else:
        P.op(eng, lambda e: e.tensor_scalar(out=out, in0=in0, scalar1=s1, scalar2=s2, op0=op0, op1=op1, **kw),
             reads, writes)


def STT(P, out, in0, scalar, in1, op0, op1, reads, writes, **kw):
    P.op('dve', lambda e: e.scalar_tensor_tensor(out=out, in0=in0, scalar=scalar, in1=in1, op0=op0, op1=op1, **kw),
         reads, writes)


def CP(P, eng, out, in_, reads, writes):
    if eng == 'act':
        P.op('act', lambda e: e.activation(out=out, in_=in_, func=AF.Copy), reads, writes)
    else:
        P.op(eng, lambda e: e.tensor_copy(out=out, in_=in_), reads, writes)


C_QA, C_KA, C_VA, C_QB, C_KB, C_VB, C_F, C_GA, C_GB = 0, 1024, 2048, 3072, 4096, 5120, 6144, 6152, 8200
IN_W = 10248


def build_program():
    nc = bass.Bass("TRN2", target_bir_lowering=False)

    def din(name, shape, dt=F32):
        return nc.dram_tensor(name, shape, dt, kind="ExternalInput").ap()

    xT_full = din("xT_full", [D, 4096])
    xT_own = din("xT_own", [D, 1024])
    x_own = din("x_own", [1024, D])
    w_in = din("w_in", [D, IN_W])
    w_ba = din("w_ba", [1024, D])
    w_bb = din("w_bb", [1024, D])
    w_out = din("w_out", [D, D])
    w_query = din("w_query", [D, D])
    skT = din("skT", [16, 128, 128])
    exp_u = din("exp_u", [16384, D])
    exp_v = din("exp_v", [16384, D])
    gmix_in = din("gmix", [128, 16])
    gffn_in = din("gffn", [128, D])
    gfin_in = din("gfin", [128, D])
    bfor_in = din("bfor", [128, 8])
    cst_in = din("cst", [128, 3 * 128])
    msk_in = din("msk", [128, 4 * 512])
    sel_in = din("sel", [128, 4])
    iota_in = din("iota", [128, 256])
    out = nc.dram_tensor("out", [1024, D], F32, kind="ExternalOutput").ap()
    kt_scr = nc.dram_tensor("kt_scr", [16, 128, 4096], BF16, kind="Internal").ap()
    v_scr = nc.dram_tensor("v_scr", [16, 128, 32, 128], BF16, kind="Internal").ap()
    x1_scr = nc.dram_tensor("x1_scr", [8, 128, D], F32, kind="Internal").ap()
    q_scr = nc.dram_tensor("q_scr", [128, 16, 1024], BF16, kind="Internal").ap()
    uv_bf = nc.dram_tensor("uv_bf", [16384, 2 * D], BF16, kind="Internal").ap()
    dbg = {}
    if DEBUG:
        dbg['OT'] = nc.dram_tensor("dbg_OT", [128, 16, 1024], BF16, kind="ExternalOutput").ap()
        dbg['x1'] = nc.dram_tensor("dbg_x1", [8, 128, D], F32, kind="ExternalOutput").ap()
        dbg['NF'] = nc.dram_tensor("dbg_NF", [128, 32, 8], F32, kind="ExternalOutput").ap()
        dbg['ids'] = nc.dram_tensor("dbg_ids", [128, 8, 128], F32, kind="ExternalOutput").ap()
        dbg['gw'] = nc.dram_tensor("dbg_gw", [128, 8, 128], F32, kind="ExternalOutput").ap()
        dbg['acc'] = nc.dram_tensor("dbg_acc", [8, 128, D], F32, kind="ExternalOutput").ap()

    with ExitStack() as st:
        P = Prog(nc, st)

        def sb(stack, name, shape, dt):
            return stack.enter_context(nc.sbuf_tensor("s_" + name, shape, dt))

        def ps(stack, name, shape, dt):
            return stack.enter_context(nc.psum_tensor("p_" + name, shape, dt))

        cst = sb(st, "cst", [128, 384], F32)
        cstb = sb(st, "cstb", [128, 384], BF16)
        gmix = sb(st, "gmix", [128, 16], F32)
        bfor = sb(st, "bfor", [128, 8], F32)
        sel = sb(st, "sel", [128, 4], F32)
        msk = sb(st, "msk", [128, 2048], F32)
        NFt = sb(st, "NFt", [128, 32, 8], F32)
        nNFo = sb(st, "nNFo", [128, 8, 8], F32)
        DMA(P, 'sp', cst[:], cst_in[:, :], [], ['cst'])
        DMA(P, 'sp', gmix[:], gmix_in[:, :], [], ['gmix'])
        DMA(P, 'sp', bfor[:], bfor_in[:, :], [], ['bfor'])
        DMA(P, 'sp', sel[:], sel_in[:, :], [], ['sel'])
        DMA(P, 'sp', msk[:], msk_in[:, :], [], ['msk'])
        CP(P, 'dve', cstb[:], cst[:], ['cst'], ['cstb'])
        ident_f, triu_f, ones_f = cst[:, 0:128], cst[:, 128:256], cst[:, 256:384]
        ident_b, ones_b = cstb[:, 0:128], cstb[:, 256:384]
        sb01, sbadd, fxadd = msk[:, 0:512], msk[:, 512:1024], msk[:, 1024:1536]

        def normT(ph, src3, nchunks, dst, dkey, pss):
            xss = [sb(ph, "xs%d_" % i + dkey, [128, 16, 256], F32) for i in range(2)]
            sqs = [sb(ph, "sq%d_" % i + dkey, [128, 16, 256], BF16) for i in range(2)]
            rss = [sb(ph, "rs%d_" % i + dkey, [128, 256], F32) for i in range(2)]
            for c in range(nchunks):
                xs, sq, rs = xss[c % 2], sqs[c % 2], rss[c % 2]
                xk, sk_, rk = 'xs%d' % (c % 2), 'sq%d' % (c % 2), 'rs%d' % (c % 2)
                pcs = pss[:, (c % 2) * 256:(c % 2) * 256 + 256]
                pk_ = ('pss', c % 2)
                DMA(P, 'sp', xs[:], src3[:, :, c * 256:(c + 1) * 256], [], [xk])
                ACT(P, sq[:], xs[:], AF.Square, [xk], [sk_])
                for kc in range(16):
                    MM(P, pcs, ones_b, sq[:, kc, :], kc == 0, kc == 15, [sk_, 'cstb'], [pk_])
                TS(P, 'dve', rs[:], pcs, 1.0 / D, EPS, ALU.mult, ALU.add, [pk_], [rk])
                ACT(P, rs[:], rs[:], AF.Sqrt, [rk], [rk])
                P.op('dve', lambda e, rs=rs: e.reciprocal(out=rs[:], in_=rs[:]), [rk], [rk])
                for kc in range(16):
                    STT(P, dst[:, kc, c * 256:(c + 1) * 256], xs[:, kc, :], gmix[:, kc:kc + 1], rs[:],
                        ALU.mult, ALU.mult, [xk, rk, 'gmix'], [(dkey, c // 2, c % 2, kc)])

        w_in3 = w_in.rearrange("(kc p) n -> p kc n", p=128)

        with ExitStack() as ph:
            hTf = sb(ph, "hTf", [128, 16, 4096], BF16)
            pss = ps(ph, "pssA", [128, 512], F32)
            with ExitStack() as ph1:
                normT(ph1, xT_full.rearrange("(kc p) t -> p kc t", p=128), 16, hTf, 'hTf', pss)
                P.barrier()
            wg = [sb(ph, "wg%d" % i, [128, 16, 256], BF16) for i in range(2)]
            wf = sb(ph, "wf", [128, 16, 8], BF16)
            kst = [sb(ph, "kst%d" % i, [128, 512], BF16) for i in range(4)]
            vst = [sb(ph, "vst%d" % i, [128, 256], BF16) for i in range(4)]
            pk = [ps(ph, "pk%d" % i, [128, 512], F32) for i in range(4)]
            pf = ps(ph, "pf", [128, 8], F32)
            flg = sb(ph, "flg", [128, 32, 8], F32)
            gi = 0
            nev = 0
            nk = 0
            for (c0, h0) in [(C_KA + 256 * g, 2 * g) for g in range(4)] + [(C_KB + 256 * g, 8 + 2 * g) for g in range(4)]:
                w = wg[gi % 2]
                wk = 'wg%d' % (gi % 2)
                gi += 1
                DMA(P, 'pool', w[:], w_in3[:, :, c0:c0 + 256], [], [wk])
                for hh in range(2):
                    for c in range(8):
                        pt = pk[nev % 4]
                        pkk = 'pk%d' % (nev % 4)
                        for kc in range(16):
                            MM(P, pt[:], w[:, kc, hh * 128:(hh + 1) * 128], hTf[:, kc, c * 512:(c + 1) * 512],
                               kc == 0, kc == 15, [wk, ('hTf', c)], [pkk])
                        ks = kst[nk % 4]
                        kk = 'kst%d' % (nk % 4)
                        nk += 1
                        CP(P, 'act' if nev % 2 == 0 else 'dve', ks[:], pt[:], [pkk], [kk])
                        nev += 1
                        DMA(P, 'sp', kt_scr[h0 + hh][:, c * 512:(c + 1) * 512], ks[:], [kk], [('kt_scr', h0 + hh, c)])
            nv = 0
            for (c0, h0) in [(C_VA + 256 * g, 2 * g) for g in range(4)] + [(C_VB + 256 * g, 8 + 2 * g) for g in range(4)]:
                w = wg[gi % 2]
                wk = 'wg%d' % (gi % 2)
                gi += 1
                DMA(P, 'pool', w[:], w_in3[:, :, c0:c0 + 256], [], [wk])
                for tb in range(32):
                    pt = pk[nev % 4]
                    pkk = 'pk%d' % (nev % 4)
                    for kc in range(16):
                        MM(P, pt[:, 0:256], hTf[:, kc, tb * 128:(tb + 1) * 128], w[:, kc, :], kc == 0, kc == 15,
                           [wk, ('hTf', tb // 4)], [pkk])
                    vs = vst[nv % 4]
                    vk = 'vst%d' % (nv % 4)
                    nv += 1
                    CP(P, 'act' if nev % 2 == 0 else 'dve', vs[:], pt[:, 0:256], [pkk], [vk])
                    nev += 1
                    DMA(P, 'sp', v_scr[h0:h0 + 2, :, tb, :].rearrange("h p d -> p h d"),
                        vs[:].rearrange("p (h d) -> p h d", h=2), [vk], [('v_scr', h0 // 2, tb)])
            DMA(P, 'pool', wf[:], w_in3[:, :, C_F:C_F + 8], [], ['wf'])
            for tb in range(32):
                for kc in range(16):
                    MM(P, pf[:], hTf[:, kc, tb * 128:(tb + 1) * 128], wf[:, kc, :], kc == 0, kc == 15,
                       ['wf', ('hTf', tb // 4)], ['pf'])
                CP(P, 'dve', flg[:, tb, :], pf[:], ['pf'], [('flg', tb)])
            TT(P, 'dve', flg[:], flg[:], bfor[:].unsqueeze(1).to_broadcast([128, 32, 8]), ALU.add,
               ['flg', 'bfor'], ['flg'])
            ACT(P, flg[:], flg[:], AF.Exp, ['flg'], ['flg'], scale=-1.0)
            ACT(P, flg[:], flg[:], AF.Ln, ['flg'], ['flg'], bias=1.0)
            ppre = pk[0]
            ptot = pk[1]
            flat = flg[:].rearrange("p a b -> p (a b)")
            MM(P, ppre[:, 0:256], triu_f, flat, True, True, ['flg', 'cst'], ['pk0'])
            MM(P, ptot[:, 0:256], ones_f, flat, True, True, ['flg', 'cst'], ['pk1'])
            tot = sb(ph, "tot", [128, 32, 8], F32)
            car = sb(ph, "car", [128, 32, 8], F32)
            CP(P, 'dve', tot[:].rearrange("p a b -> p (a b)"), ptot[:, 0:256], ['pk1'], ['tot'])
            P.op('dve', lambda e: e.memset(car[:, 0, :], 0.0), [], ['car'])
            for bk in range(1, 32):
                TT(P, 'dve', car[:, bk, :], car[:, bk - 1, :], tot[:, bk - 1, :], ALU.add, ['car', 'tot'], ['car'])
            TT(P, 'dve', NFt[:].rearrange("p a b -> p (a b)"), ppre[:, 0:256],
               car[:].rearrange("p a b -> p (a b)"), ALU.add, ['pk0', 'car'], ['NFt'])
            NF4 = NFt[:].rearrange("p (i k) h -> p i k h", k=4)
            TS(P, 'dve', nNFo[:], NF4[:, :, 0, :], sel[:, 0:1], None, ALU.mult, None, ['NFt', 'sel'], ['nNFo'])
            for k in range(1, 4):
                STT(P, nNFo[:], NF4[:, :, k, :], sel[:, k:k + 1], nNFo[:], ALU.mult, ALU.add,
                    ['NFt', 'sel', 'nNFo'], ['nNFo'])
            TS(P, 'dve', nNFo[:], nNFo[:], -1.0, None, ALU.mult, None, ['nNFo'], ['nNFo'])
            if DEBUG:
                DMA(P, 'sp', dbg['NF'], NFt[:], ['NFt'], ['dbgNF'])
            P.barrier()

        bc = ExitStack()
        hTo = sb(bc, "hTo", [128, 16, 1024], BF16)
        OT = sb(bc, "OT", [128, 16, 1024], BF16)
        if STAGE >= 2:
            with ExitStack() as ph:
                pss = ps(ph, "pssB", [128, 512], F32)
                with ExitStack() as ph1:
                    normT(ph1, xT_own.rearrange("(kc p) t -> p kc t", p=128), 4, hTo, 'hTo', pss)
                    P.barrier()
                KTs = [sb(ph, "KT%d" % i, [128, 4096], BF16) for i in range(2)]
                VH_ = sb(ph, "VH", [128, 32, 128], BF16)
                VHs = [VH_, VH_]
                wq_ = sb(ph, "wq", [128, 16, 128], BF16)
                wqs = [wq_, wq_]
                QHs = [sb(ph, "QH%d" % i, [128, 1024], BF16) for i in range(2)]
                Zs = [sb(ph, "Z%d" % i, [128, 4096], F32) for i in range(2)]
                T1 = sb(ph, "T1", [128, 4096], F32)
                PP = sb(ph, "PP", [128, 4096], F32)
                NFbc = sb(ph, "NFbc", [128, 4096], F32)
                Ws = [sb(ph, "W%d" % i, [128, 4096], BF16) for i in range(2)]
                negt = sb(ph, "negt", [128, 4], F32)
                lpart = sb(ph, "lpart", [128, 2, 8], F32)
                lsum = sb(ph, "lsum", [128, 4], F32)
                WT = [sb(ph, "WT%d" % i, [128, 512], BF16) for i in range(2)]
                Ob = sb(ph, "Ob", [128, 128], BF16)
                small = sb(ph, "small", [128, 8], F32)
                zp = [ps(ph, "zp%d" % i, [128, 512], F32) for i in range(2)]
                tp = [ps(ph, "tp%d" % i, [128, 512], BF16) for i in range(2)]
                op_ = ps(ph, "op", [128, 128], F32)
                otp = ps(ph, "otp", [128, 128], BF16)
                scale = 1.0 / math.sqrt(128.0)
                def load_k(hd):
                    DMA(P, 'sp', KTs[hd % 2][:], kt_scr[hd], [('kt_scr', hd)], ['KT%d' % (hd % 2)])

                def load_v(hd):
                    DMA(P, 'sp', VH_[:], v_scr[hd], [('v_scr', hd // 2)], ['VH'])

                def csl(c):
                    return slice(512 * c, 512 * c + 512)

                cnt = {'nz': 0, 'ntp': 0}

                def prologue_q(hd):
                    is_sb = hd < 8
                    wq, wqk = wqs[hd % 2], 'wq'
                    QH, qhk = QHs[hd % 2], 'QH%d' % (hd % 2)
                    qc = C_QA + hd * 128 if is_sb else C_QB + (hd - 8) * 128
                    DMA(P, 'pool', wq[:], w_in3[:, :, qc:qc + 128], [], [wqk])
                    src_t, c0_ = (exp_u, 0) if hd < 8 else (exp_v, D)
                    r0 = (hd % 8) * 2048
                    DMA(P, 'pool', uv_bf[r0:r0 + 2048, c0_:c0_ + D], src_t[r0:r0 + 2048, :], [], [('uv_bf', hd)])
                    for half in range(2):
                        for kc in range(16):
                            MM(P, pss[:], wq[:, kc, :], hTo[:, kc, half * 512:(half + 1) * 512], kc == 0, kc == 15,
                               [wqk, 'hTo'], ['pss'])
                        ACT(P, QH[:, half * 512:(half + 1) * 512], pss[:], AF.Copy, ['pss'], [qhk], scale=scale)

                def prologue(hd):
                    is_sb = hd < 8
                    if hd + 1 < 16:
                        load_k(hd + 1)
                    if not is_sb:
                        h = hd - 8
                        dg = T1[:].rearrange("p (a b) -> p a b", b=128)
                        TT(P, 'dve', dg, ident_f.unsqueeze(1).to_broadcast([128, 32, 128]),
                           NFt[:, :, h:h + 1].to_broadcast([128, 32, 128]), ALU.mult, ['cst', 'NFt'], ['T1'])
                        for c in range(8):
                            z = zp[cnt['nz'] % 2]
                            zk = 'zp%d' % (cnt['nz'] % 2)
                            cnt['nz'] += 1
                            MM(P, z[:], ones_f, T1[:, csl(c)], True, True, ['cst', 'T1'], [zk])
                            CP(P, 'act', NFbc[:, csl(c)], z[:], [zk], [('NFbc', c)])

                def stage_q(r, hd, i):
                    is_sb = hd < 8
                    nch = i + 1
                    KT, ktk = KTs[hd % 2], 'KT%d' % (hd % 2)
                    Z, zn = Zs[r % 2], 'Z%d' % (r % 2)
                    for c in range(nch):
                        z = zp[cnt['nz'] % 2]
                        zk = 'zp%d' % (cnt['nz'] % 2)
                        cnt['nz'] += 1
                        MM(P, z[:], QHs[hd % 2][:, i * 128:(i + 1) * 128], KT[:, csl(c)], True, True,
                           ['QH%d' % (hd % 2), ktk], [zk])
                        if is_sb:
                            CP(P, 'act', Z[:, csl(c)], z[:], [zk], [(zn, c)])
                        else:
                            TT(P, 'dve', Z[:, csl(c)], z[:], NFbc[:, csl(c)], ALU.add, [zk, ('NFbc', c)], [(zn, c)])

                def stage_e(r, hd, i):
                    is_sb = hd < 8
                    nch = i + 1
                    L = 512 * nch
                    Z, zn = Zs[r % 2], 'Z%d' % (r % 2)
                    W, wn = Ws[r % 2], 'W%d' % (r % 2)
                    ngc = r % 4
                    lp = lpart[:, r % 2, :]
                    lpk = ('lpart', r % 2)
                    if is_sb:
                        for c in range(nch):
                            ACT(P, T1[:, csl(c)], Z[:, csl(c)], AF.Exp, [(zn, c)], [('T1', c)])
                        for c in range(nch):
                            ACT(P, T1[:, csl(c)], T1[:, csl(c)], AF.Ln, [('T1', c)], [('T1', c)], bias=1.0)
                        TT(P, 'pool', T1[:, csl(i)], T1[:, csl(i)], sb01, ALU.mult, [('T1', i), 'msk'], [('T1', i)])
                        for c in range(nch):
                            init = 0.0 if c == 0 else PP[:, 512 * c - 1:512 * c]
                            o_ap, d0_ap, d1_ap = PP[:, csl(c)], ones_f[:, 0:1].to_broadcast([128, 512]), T1[:, csl(c)]
                            P.op('dve', lambda e, o_ap=o_ap, d0_ap=d0_ap, d1_ap=d1_ap, init=init: e.tensor_tensor_scan(
                                out=o_ap, data0=d0_ap, data1=d1_ap, initial=init, op0=ALU.mult, op1=ALU.add),
                                [('T1', c), 'cst'] + ([('PP', c - 1)] if c else []), [('PP', c)])
                        TS(P, 'dve', negt[:, ngc:ngc + 1], PP[:, L - 1:L], -1.0, None, ALU.mult, None,
                           [('PP', nch - 1)], [('negt', ngc)])
                        for c in range(nch):
                            if c == 0:
                                TT(P, 'dve', Z[:, 1:512], Z[:, 1:512], PP[:, 0:511], ALU.add,
                                   [(zn, 0), ('PP', 0)], [(zn, 0)])
                            else:
                                TT(P, 'dve', Z[:, csl(c)], Z[:, csl(c)], PP[:, 512 * c - 1:512 * c + 511], ALU.add,
                                   [(zn, c), ('PP', c), ('PP', c - 1)], [(zn, c)])
                        TT(P, 'pool', Z[:, csl(i)], Z[:, csl(i)], sbadd, ALU.add, [(zn, i), 'msk'], [(zn, i)])
                    else:
                        TT(P, 'pool', Z[:, csl(i)], Z[:, csl(i)], fxadd, ALU.add, [(zn, i), 'msk'], [(zn, i)])

                def stage_e2(r, hd, i):
                    is_sb = hd < 8
                    nch = i + 1
                    Z, zn = Zs[r % 2], 'Z%d' % (r % 2)
                    W, wn = Ws[r % 2], 'W%d' % (r % 2)
                    ngc = r % 4
                    lp = lpart[:, r % 2, :]
                    lpk = ('lpart', r % 2)
                    if is_sb:
                        for c in range(nch):
                            ACT(P, W[:, csl(c)], Z[:, csl(c)], AF.Exp, [(zn, c), ('negt', ngc)], [(wn, c)],
                                bias=negt[:, ngc:ngc + 1])
                    else:
                        for c in range(nch):
                            ACT(P, W[:, csl(c)], Z[:, csl(c)], AF.Exp, [(zn, c), 'nNFo'], [(wn, c), lpk + (c,)],
                                bias=nNFo[:, i, hd - 8:hd - 7], accum_out=lp[:, c:c + 1])
                        P.op('dve', lambda e, lp=lp, nch=nch, ngc=ngc: e.reduce_sum(
                            out=lsum[:, ngc:ngc + 1], in_=lp[:, 0:nch], axis=AX.X), [lpk], [('lsum', ngc)])
                        P.op('dve', lambda e, ngc=ngc: e.reciprocal(out=lsum[:, ngc:ngc + 1], in_=lsum[:, ngc:ngc + 1]),
                             [('lsum', ngc)], [('lsum', ngc)])

                def stage_b(r, hd, i):
                    is_sb = hd < 8
                    nch = i + 1
                    W, wn = Ws[r % 2], 'W%d' % (r % 2)
                    ngc = r % 4
                    nkb = 4 * nch
                    slots = []

                    def tr_group(g):
                        t = tp[cnt['ntp'] % 2]
                        tk = 'tp%d' % (cnt['ntp'] % 2)
                        wt = WT[cnt['ntp'] % 2]
                        wtk = 'WT%d' % (cnt['ntp'] % 2)
                        cnt['ntp'] += 1
                        for q in range(4):
                            kb = 4 * g + q
                            TR(P, t[:, q * 128:(q + 1) * 128], W[:, kb * 128:(kb + 1) * 128], ident_b,
                               [(wn, g), 'cstb'], [tk])
                        CP(P, 'dve', wt[:], t[:], [tk], [wtk])
                        slots.append((wt, wtk))

                    def pv_group(g):
                        wt, wtk = slots[g]
                        for q in range(4):
                            kb = 4 * g + q
                            MM(P, op_[:], wt[:, q * 128:(q + 1) * 128], VH_[:, kb, :], kb == 0, kb == nkb - 1,
                               [wtk, 'VH'], ['op'])
                    tr_group(0)
                    for g in range(nch):
                        if g + 1 < nch:
                            tr_group(g + 1)
                        pv_group(g)
                    if is_sb:
                        CP(P, 'dve', Ob[:], op_[:], ['op'], ['Ob'])
                    else:
                        TS(P, 'dve', Ob[:], op_[:], lsum[:, ngc:ngc + 1], None, ALU.mult, None,
                           ['op', ('lsum', ngc)], ['Ob'])
                    TR(P, otp[:], Ob[:], ident_b, ['Ob', 'cstb'], ['otp'])
                    CP(P, 'dve', OT[:, hd, i * 128:(i + 1) * 128], otp[:], ['otp'], [('OT', hd)])

                rows = [(hd, i) for hd in range(16) for i in range(8)]
                nr = len(rows)
                load_k(0)
                for it in range(nr + 2):
                    if 0 <= it - 1 < nr:
                        hd, i = rows[it - 1]
                        stage_e(it - 1, hd, i)
                    if it < nr:
                        hd, i = rows[it]
                        if i == 0:
                            if hd == 0:
                                prologue_q(0)
                            prologue(hd)
                        if i == 5 and hd + 1 < 16:
                            prologue_q(hd + 1)
                        stage_q(it, hd, i)
                    if 0 <= it - 2 < nr:
                        hd, i = rows[it - 2]
                        if i == 0:
                            load_v(hd)
                        stage_b(it - 2, hd, i)
                    if 0 <= it - 1 < nr:
                        hd, i = rows[it - 1]
                        stage_e2(it - 1, hd, i)
                if DEBUG:
                    DMA(P, 'sp', dbg['OT'], OT[:], ['OT'], ['dbgOT'])
                P.barrier()

        if STAGE >= 3:
            with ExitStack() as ph:
                mT = sb(ph, "mT", [128, 16, 1024], BF16)
                wga = sb(ph, "wga", [128, 16, 512], BF16)
                wgb = sb(ph, "wgb", [128, 16, 512], BF16)
                wa = sb(ph, "wa", [128, 8, 512], BF16)
                wb = sb(ph, "wb", [128, 8, 512], BF16)
                sga = sb(ph, "sga", [128, 512], F32)
                sgb = sb(ph, "sgb", [128, 512], F32)
                m1 = sb(ph, "m1", [128, 512], F32)
                m2 = sb(ph, "m2", [128, 512], F32)
                pc = [ps(ph, "pc%d" % i, [128, 512], F32) for i in range(8)]
                w_ba3 = w_ba.rearrange("(kc p) n -> p kc n", p=128)
                w_bb3 = w_bb.rearrange("(kc p) n -> p kc n", p=128)
                w_out3 = w_out.rearrange("(kc p) n -> p kc n", p=128)
                xo = [sb(ph, "xo%d" % i, [128, D], F32) for i in range(2)]
                it = 0
                for ng in range(4):
                    DMA(P, 'pool', wga[:], w_in3[:, :, C_GA + ng * 512:C_GA + (ng + 1) * 512], [], ['wga'])
                    DMA(P, 'pool', wgb[:], w_in3[:, :, C_GB + ng * 512:C_GB + (ng + 1) * 512], [], ['wgb'])
                    DMA(P, 'pool', wa[:], w_ba3[:, :, ng * 512:(ng + 1) * 512], [], ['wa'])
                    DMA(P, 'pool', wb[:], w_bb3[:, :, ng * 512:(ng + 1) * 512], [], ['wb'])
                    for nt in range(4):
                        ns = slice(nt * 128, (nt + 1) * 128)
                        for half in range(2):
                            hs = slice(half * 512, (half + 1) * 512)
                            b0 = 4 * (it % 2)
                            it += 1
                            pga, pgb, pya, pyb = pc[b0], pc[b0 + 1], pc[b0 + 2], pc[b0 + 3]
                            kga, kgb, kya, kyb = ['pc%d' % (b0 + x) for x in range(4)]
                            for kc in range(16):
                                MM(P, pga[:], wga[:, kc, ns], hTo[:, kc, hs], kc == 0, kc == 15, ['wga', 'hTo'], [kga])
                            for kc in range(16):
                                MM(P, pgb[:], wgb[:, kc, ns], hTo[:, kc, hs], kc == 0, kc == 15, ['wgb', 'hTo'], [kgb])
                            for kc in range(8):
                                MM(P, pya[:], wa[:, kc, ns], OT[:, kc, hs], kc == 0, kc == 7, ['wa', 'OT'], [kya])
                            for kc in range(8):
                                MM(P, pyb[:], wb[:, kc, ns], OT[:, 8 + kc, hs], kc == 0, kc == 7, ['wb', 'OT'], [kyb])
                            ACT(P, sga[:], pga[:], AF.Sigmoid, [kga], ['sga'])
                            ACT(P, sgb[:], pgb[:], AF.Sigmoid, [kgb], ['sgb'])
                            TT(P, 'dve', m1[:], sga[:], pya[:], ALU.mult, ['sga', kya], ['m1'])
                            TT(P, 'dve', m2[:], sgb[:], pyb[:], ALU.mult, ['sgb', kyb], ['m2'])
                            TT(P, 'pool', mT[:, ng * 4 + nt, hs], m1[:], m2[:], ALU.add, ['m1', 'm2'], ['mT'])
                wo = sb(ph, "wo", [128, 16, 512], BF16)
                it = 0
                for ng in range(4):
                    DMA(P, 'pool', wo[:], w_out3[:, :, ng * 512:(ng + 1) * 512], [], ['wo'])
                    for tb in range(8):
                        pt = pc[it % 4]
                        pk_ = 'pc%d' % (it % 4)
                        xb_ = xo[it % 2]
                        xk = 'xo%d' % (it % 2)
                        it += 1
                        cs = slice(ng * 512, (ng + 1) * 512)
                        DMA(P, 'sp', xb_[:, 0:512], x_own[tb * 128:(tb + 1) * 128, cs], [], [xk])
                        for kc in range(16):
                            MM(P, pt[:], mT[:, kc, tb * 128:(tb + 1) * 128], wo[:, kc, :], kc == 0, kc == 15,
                               ['mT', 'wo'], [pk_])
                        TT(P, 'dve', xb_[:, 0:512], xb_[:, 0:512], pt[:], ALU.add, [xk, pk_], [xk])
                        DMA(P, 'sp', x1_scr[tb][:, cs], xb_[:, 0:512], [xk], [('x1s', tb, ng)])
                        if DEBUG:
                            DMA(P, 'sp', dbg['x1'][tb][:, cs], xb_[:, 0:512], [xk], [('dbgx1', tb, ng)])
                P.barrier()

        bc.close()
        if STAGE >= 4:
            with ExitStack() as ph:
                gffn = sb(ph, "gffn", [128, D], F32)
                gfin = sb(ph, "gfin", [128, D], F32)
                skb = sb(ph, "skb", [128, 16, 128], BF16)
                rstd2 = sb(ph, "rstd2", [128, 8], F32)
                DMA(P, 'sp', gffn[:], gffn_in[:, :], [], ['gffn'])
                DMA(P, 'sp', gfin[:], gfin_in[:, :], [], ['gfin'])
                DMA(P, 'pool', skb[:], skT.rearrange("ch c n -> c ch n"), [], ['skb'])
                pd = [ps(ph, "pd%d" % i, [128, 512], F32) for i in range(4)]
                jb = sb(ph, "jb", [128, D], BF16)
                x1bs = [sb(ph, "x1b%d" % i, [128, D], F32) for i in range(2)]
                x1b = x1bs[0]
                with ExitStack() as ph1:
                    pdb = [ps(ph1, "pdb%d" % i, [128, 512], BF16) for i in range(2)]
                    h2T = sb(ph1, "h2T", [128, 16, 1024], BF16)
                    qT = sb(ph1, "qT", [128, 16, 1024], BF16)
                    h2b = sb(ph1, "h2b", [128, D], BF16)
                    wqp = sb(ph1, "wqp", [128, 16, 512], BF16)
                    w_q3 = w_query.rearrange("(kc p) n -> p kc n", p=128)
                    ntp = 0
                    for tb in range(8):
                        DMA(P, 'sp', x1b[:], x1_scr[tb], [('x1s', tb)], ['x1b'])
                        ACT(P, jb[:], x1b[:], AF.Square, ['x1b'], ['jb', ('rstd2', tb)],
                            accum_out=rstd2[:, tb:tb + 1])
                        TS(P, 'dve', rstd2[:, tb:tb + 1], rstd2[:, tb:tb + 1], 1.0 / D, EPS, ALU.mult, ALU.add,
                           [('rstd2', tb)], [('rstd2', tb)])
                        ACT(P, rstd2[:, tb:tb + 1], rstd2[:, tb:tb + 1], AF.Sqrt, [('rstd2', tb)], [('rstd2', tb)])
                        P.op('dve', lambda e, tb=tb: e.reciprocal(out=rstd2[:, tb:tb + 1], in_=rstd2[:, tb:tb + 1]),
                             [('rstd2', tb)], [('rstd2', tb)])
                        STT(P, h2b[:], x1b[:], rstd2[:, tb:tb + 1], gffn[:], ALU.mult, ALU.mult,
                            ['x1b', ('rstd2', tb), 'gffn'], ['h2b'])
                        for g in range(4):
                            t = pdb[ntp % 2]
                            tk = 'pdb%d' % (ntp % 2)
                            ntp += 1
                            for q in range(4):
                                kc = 4 * g + q
                                TR(P, t[:, q * 128:(q + 1) * 128], h2b[:, kc * 128:(kc + 1) * 128], ident_b,
                                   ['h2b', 'cstb'], [tk])
                            CP(P, 'act', h2T[:, 4 * g:4 * g + 4, tb * 128:(tb + 1) * 128],
                               t[:].rearrange("p (a b) -> p a b", a=4), [tk], ['h2T'])
                    it = 0
                    for ng in range(4):
                        DMA(P, 'pool', wqp[:], w_q3[:, :, ng * 512:(ng + 1) * 512], [], ['wqp'])
                        for nt in range(4):
                            for half in range(2):
                                pt = pd[it % 4]
                                pk_ = 'pd%d' % (it % 4)
                                it += 1
                                for kc in range(16):
                                    MM(P, pt[:], wqp[:, kc, nt * 128:(nt + 1) * 128],
                                       h2T[:, kc, half * 512:(half + 1) * 512], kc == 0, kc == 15, ['wqp', 'h2T'], [pk_])
                                CP(P, 'act' if it % 2 else 'dve', qT[:, ng * 4 + nt, half * 512:(half + 1) * 512], pt[:],
                                   [pk_], ['qT'])
                    DMA(P, 'sp', q_scr, qT[:], ['qT'], ['q_scr'])
                    P.barrier()
                po = [ps(ph, "po%d" % i, [128, 512], F32) for i in range(4)]
                sc = sb(ph, "sc", [128, 16, 128], F32)
                tmp = sb(ph, "tmp", [128, 256], F32)
                v16 = sb(ph, "v16", [128, 8, 2, 16], F32)
                ix = sb(ph, "ix", [128, 8, 2, 16], U32)
                ixf = sb(ph, "ixf", [128, 8, 2, 16], F32)
                cand = sb(ph, "cand", [128, 8, 16, 16], F32)
                cid = sb(ph, "cid", [128, 8, 16, 16], F32)
                t16 = sb(ph, "t16", [128, 8, 16], F32)
                pos = sb(ph, "pos", [128, 8, 16], U32)
                posf = sb(ph, "posf", [128, 8, 16], F32)
                pa = sb(ph, "pa", [128, 8, 16], U32)
                pb = sb(ph, "pb", [128, 8, 16], U32)
                paf = sb(ph, "paf", [128, 8, 16], F32)
                pbf = sb(ph, "pbf", [128, 8, 16], F32)
                ida = sb(ph, "ida", [128, 8, 16], F32)
                idb = sb(ph, "idb", [128, 8, 16], F32)
                iot = sb(ph, "iot", [128, 256], F32)
                DMA(P, 'sp', iot[:], iota_in[:, :], [], ['iot'])
                idf = sb(ph, "idf", [128, 128], F32)
                idus = [sb(ph, "idu%d" % i, [128, 128], U32) for i in range(2)]
                gts = [sb(ph, "gt%d" % i, [128, 8, 16], F32) for i in range(2)]
                gs = sb(ph, "gs", [128, 8], F32)
                aa = sb(ph, "aa", [128, 128], F32)
                ww = sb(ph, "ww", [128, 128], F32)
                g1 = sb(ph, "g1", [128, 128], F32)
                h2f = sb(ph, "h2f", [128, D], F32)
                qTbs = [sb(ph, "qTb%d" % i, [128, 16, 128], BF16) for i in range(2)]
                ssf = sb(ph, "ssf", [128, 2], F32)
                GSZ = 4
                dgs = [sb(ph, "dg%d" % i, [128, GSZ, 128], BF16) for i in range(2)]
                NG_, NVR = 8, 7
                jbs = [jb, sb(ph, "jb2", [128, D], BF16)]
                gbs = [sb(ph, "gb%d" % i, [128, 2 * D], BF16) for i in range(NG_)]
                vrs = [sb(ph, "vr%d" % i, [128, D], BF16) for i in range(NVR)]
                ngb = [0]

                def topk(tb):
                    idu = idus[tb % 2]
                    ik = 'idu%d' % (tb % 2)
                    gt = gts[tb % 2]
                    gk = 'gt%d' % (tb % 2)
                    qTb = qTbs[tb % 2]
                    qk = 'qTb%d' % (tb % 2)
                    DMA(P, 'sp', qTb[:], q_scr[:, :, tb * 128:(tb + 1) * 128], ['q_scr'], [qk])
                    for g in range(4):
                        for q in range(4):
                            ch = 4 * g + q
                            MM(P, pd[g][:, q * 128:(q + 1) * 128], qTb[:, ch, :], skb[:, ch, :],
                               True, True, [qk, 'skb'], ['pd%d' % g])
                        CP(P, 'act', sc[:, 4 * g:4 * g + 4, :], pd[g][:].rearrange("p (a b) -> p a b", a=4),
                           ['pd%d' % g], ['sc'])
                    for ch in range(16):
                        hh, pp = ch // 2, ch % 2
                        P.op('dve', lambda e, ch=ch, hh=hh, pp=pp: e.max(out=v16[:, hh, pp, 0:8], in_=sc[:, ch, :]),
                             ['sc'], ['v16'])
                        P.op('dve', lambda e, ch=ch, hh=hh, pp=pp: e.max_index(
                            out=ix[:, hh, pp, 0:8], in_max=v16[:, hh, pp, 0:8], in_values=sc[:, ch, :]),
                            ['sc', 'v16'], ['ix'])
                        P.op('dve', lambda e, ch=ch, hh=hh, pp=pp: e.match_replace(
                            out=tmp[:, 0:128], in_to_replace=v16[:, hh, pp, 0:8], in_values=sc[:, ch, :],
                            imm_value=-1e30), ['sc', 'v16'], ['tmp'])
                        P.op('dve', lambda e, ch=ch, hh=hh, pp=pp: e.max(out=v16[:, hh, pp, 8:16], in_=tmp[:, 0:128]),
                             ['tmp'], ['v16'])
                        P.op('dve', lambda e, ch=ch, hh=hh, pp=pp: e.max_index(
                            out=ix[:, hh, pp, 8:16], in_max=v16[:, hh, pp, 8:16], in_values=tmp[:, 0:128]),
                            ['tmp', 'v16'], ['ix'])
                    CP(P, 'dve', ixf[:], ix[:], ['ix'], ['ixf'])
                    TS(P, 'dve', ixf[:, :, 0, :], ixf[:, :, 0, :], 128.0, None, ALU.mult, None, ['ixf'], ['ixf'])
                    TT(P, 'dve', cand[:], v16[:, :, 0, :].unsqueeze(3).to_broadcast([128, 8, 16, 16]),
                       v16[:, :, 1, :].unsqueeze(2).to_broadcast([128, 8, 16, 16]), ALU.add, ['v16'], ['cand'])
                    for hh in range(8):
                        cf = cand[:, hh].rearrange("p a b -> p (a b)")
                        P.op('dve', lambda e, hh=hh, cf=cf: e.max(out=t16[:, hh, 0:8], in_=cf), ['cand'], ['t16'])
                        P.op('dve', lambda e, hh=hh, cf=cf: e.max_index(
                            out=pos[:, hh, 0:8], in_max=t16[:, hh, 0:8], in_values=cf), ['cand', 't16'], ['pos'])
                        P.op('dve', lambda e, hh=hh, cf=cf: e.match_replace(
                            out=tmp[:], in_to_replace=t16[:, hh, 0:8], in_values=cf, imm_value=-1e30),
                            ['cand', 't16'], ['tmp'])
                        P.op('dve', lambda e, hh=hh: e.max(out=t16[:, hh, 8:16], in_=tmp[:]), ['tmp'], ['t16'])
                        P.op('dve', lambda e, hh=hh: e.max_index(
                            out=pos[:, hh, 8:16], in_max=t16[:, hh, 8:16], in_values=tmp[:]), ['tmp', 't16'], ['pos'])
                    P.op('dve', lambda e: e.tensor_single_scalar(out=pa[:], in_=pos[:], scalar=4,
                                                                 op=ALU.logical_shift_right), ['pos'], ['pa'])
                    P.op('dve', lambda e: e.tensor_single_scalar(out=pb[:], in_=pos[:], scalar=15,
                                                                 op=ALU.bitwise_and), ['pos'], ['pb'])
                    CP(P, 'dve', paf[:], pa[:], ['pa'], ['paf'])
                    CP(P, 'dve', pbf[:], pb[:], ['pb'], ['pbf'])
                    io16 = iot[:, 0:16].unsqueeze(1).unsqueeze(1).to_broadcast([128, 8, 16, 16])
                    for half, pf, pk_, dst in ((0, paf, 'paf', ida), (1, pbf, 'pbf', idb)):
                        TT(P, 'dve', cid[:], io16, pf[:].unsqueeze(3).to_broadcast([128, 8, 16, 16]), ALU.is_equal,
                           ['iot', pk_], ['cid'])
                        TT(P, 'dve', cid[:], cid[:], ixf[:, :, half, :].unsqueeze(2).to_broadcast([128, 8, 16, 16]),
                           ALU.mult, ['cid', 'ixf'], ['cid'])
                        P.op('dve', lambda e, dst=dst: e.reduce_sum(out=dst[:], in_=cid[:], axis=AX.X),
                             ['cid'], ['id%d' % half])
                    TT(P, 'dve', idf[:].rearrange("p (h k) -> p h k", h=8), ida[:], idb[:], ALU.add,
                       ['id0', 'id1'], ['idf'])
                    CP(P, 'dve', idu[:], idf[:], ['idf'], [ik])
                    if DEBUG:
                        DMA(P, 'sp', dbg['ids'][:, tb, :], idf[:], ['idf'], [('dbgids', tb)])
                    TT(P, 'dve', gt[:], t16[:], t16[:, :, 0:1].to_broadcast([128, 8, 16]), ALU.subtract, ['t16'], [gk])
                    ACT(P, gt[:], gt[:], AF.Exp, [gk], [gk])
                    P.op('dve', lambda e, gt=gt: e.reduce_sum(out=gs[:], in_=gt[:], axis=AX.X), [gk], ['gs'])
                    P.op('dve', lambda e: e.reciprocal(out=gs[:], in_=gs[:]), ['gs'], ['gs'])
                    TT(P, 'dve', gt[:], gt[:], gs[:].unsqueeze(2).to_broadcast([128, 8, 16]), ALU.mult, [gk, 'gs'], [gk])

                def gather(idu, ik, hk):
                    k = ngb[0]
                    ngb[0] += 1
                    bfr, bk = gbs[k % NG_], 'gb%d' % (k % NG_)
                    vr, vk = vrs[k % NVR], 'vr%d' % (k % NVR)
                    P.dma('pool', lambda e, bfr=bfr, hk=hk: e.indirect_dma_start(
                        out=bfr[:, :], out_offset=None, in_=uv_bf[:, :],
                        in_offset=bass.IndirectOffsetOnAxis(ap=idu[:, hk:hk + 1], axis=0)), [ik, 'uv_bf'], [bk])
                    CP(P, 'act', vr[:], bfr[:, D:2 * D], [bk], [vk])
                    return bfr[:, 0:D], bk, vr, vk

                def u_prep(tb):
                    x1b = x1bs[tb % 2]
                    xk = 'x1b%d' % (tb % 2)
                    DMA(P, 'sp', x1b[:], x1_scr[tb], [('x1s', tb)], [xk])
                    STT(P, h2f[:], x1b[:], rstd2[:, tb:tb + 1], gffn[:], ALU.mult, ALU.mult,
                        [xk, 'rstd2', 'gffn'], ['h2f'])

                ngrp = [0]

                def grp_dots(tb, g, j0, j1, st):
                    idu = idus[tb % 2]
                    ik = 'idu%d' % (tb % 2)
                    for j in range(j0, j1):
                        hk = g * GSZ + j
                        ub, uk, vb, vk = gather(idu, ik, hk)
                        st['v'].append((vb, vk))
                        jslot = jbs[hk % 2]
                        STT(P, jslot[:], ub, 1.0, h2f[:], ALU.mult, ALU.mult, [uk, 'h2f'], [('aa', hk), ('jbd', hk % 2)],
                            accum_out=aa[:, hk:hk + 1])

                def grp_gelu_a(tb, g):
                    hs = slice(g * GSZ, (g + 1) * GSZ)
                    ak = [('aa', g * GSZ + j) for j in range(GSZ)]
                    g1k = ('g1', g % 2)
                    TT(P, 'dve', g1[:, hs], aa[:, hs], aa[:, hs], ALU.mult, ak, [g1k])
                    TS(P, 'dve', g1[:, hs], g1[:, hs], 0.044715, 1.0, ALU.mult, ALU.add, [g1k], [g1k])
                    TT(P, 'dve', g1[:, hs], g1[:, hs], aa[:, hs], ALU.mult, [g1k] + ak, [g1k])
                    ACT(P, g1[:, hs], g1[:, hs], AF.Sigmoid, [g1k], [g1k], scale=2.0 * math.sqrt(2.0 / math.pi))

                def grp_finish(tb, g, st):
                    gt = gts[tb % 2]
                    gk = 'gt%d' % (tb % 2)
                    dg = dgs[ngrp[0] % 2]
                    dk = 'dg%d' % (ngrp[0] % 2)
                    ngrp[0] += 1
                    hs = slice(g * GSZ, (g + 1) * GSZ)
                    ak = [('aa', g * GSZ + j) for j in range(GSZ)]
                    g1k = ('g1', g % 2)
                    TT(P, 'dve', g1[:, hs], g1[:, hs], aa[:, hs], ALU.mult, [g1k] + ak, [g1k])
                    TT(P, 'dve', ww[:, hs], g1[:, hs], gt[:].rearrange("p a b -> p (a b)")[:, hs], ALU.mult,
                       [g1k, gk], [('ww', g % 2)])
                    for j in range(GSZ):
                        hk = g * GSZ + j
                        ACT(P, dg[:, j, :], ident_b, AF.Copy, ['cstb', ('ww', g % 2)], [(dk, j)],
                            scale=ww[:, hk:hk + 1])
                    for j in range(GSZ):
                        hk = g * GSZ + j
                        vb, vk = st['v'][j]
                        for c in range(4):
                            MM(P, po[c][:], dg[:, j, :], vb[:, c * 512:(c + 1) * 512], hk == 0, hk == 127,
                               [(dk, j), vk], ['po%d' % c])

                def final(tb):
                    x1b = x1bs[tb % 2]
                    xk = 'x1b%d' % (tb % 2)
                    if DEBUG:
                        DMA(P, 'sp', dbg['gw'][:, tb, :], ww[:], ['ww'], [('dbggw', tb)])
                    for c in range(4):
                        TT(P, 'dve', x1b[:, c * 512:(c + 1) * 512], x1b[:, c * 512:(c + 1) * 512], po[c][:], ALU.add,
                           [xk, 'po%d' % c], [xk])
                    if DEBUG:
                        DMA(P, 'sp', dbg['acc'][tb], x1b[:], [xk], [('dbgacc', tb)])
                    ACT(P, jb[:], x1b[:], AF.Square, [xk], ['ssf', ('jbd', 0)], accum_out=ssf[:, 0:1])
                    TS(P, 'dve', ssf[:, 0:1], ssf[:, 0:1], 1.0 / D, EPS, ALU.mult, ALU.add, ['ssf'], ['ssf'])
                    ACT(P, ssf[:, 0:1], ssf[:, 0:1], AF.Sqrt, ['ssf'], ['ssf'])
                    P.op('dve', lambda e: e.reciprocal(out=ssf[:, 0:1], in_=ssf[:, 0:1]), ['ssf'], ['ssf'])
                    STT(P, x1b[:], x1b[:], ssf[:, 0:1], gfin[:], ALU.mult, ALU.mult, [xk, 'ssf', 'gfin'], [xk])
                    DMA(P, 'sp', out[tb * 128:(tb + 1) * 128, :], x1b[:], [xk], [('out', tb)])

                topk(0)
                for tb in range(8):
                    u_prep(tb)
                    NG = 128 // GSZ
                    prev = None
                    for g in range(NG):
                        st_ = {'v': []}
                        grp_dots(tb, g, 0, GSZ // 2, st_)
                        if prev is not None:
                            grp_finish(tb, g - 1, prev)
                        grp_dots(tb, g, GSZ // 2, GSZ, st_)
                        grp_gelu_a(tb, g)
                        prev = st_
                        if g == 7 and tb + 1 < 8:
                            topk(tb + 1)
                    grp_finish(tb, NG - 1, prev)
                    final(tb)
                P.barrier()
        P.barrier()
        P.emit()
    return nc


def make_core_inputs(inputs):
    x = np.asarray(inputs["x"], dtype=np.float32)
    w_in = np.ascontiguousarray(np.asarray(inputs["w_in"], dtype=np.float32)[0])
    w_ba = np.ascontiguousarray(np.asarray(inputs["w_branch_a"], dtype=np.float32)[0])
    w_bb = np.ascontiguousarray(np.asarray(inputs["w_branch_b"], dtype=np.float32)[0])
    w_out = np.ascontiguousarray(np.asarray(inputs["w_out"], dtype=np.float32)[0])
    w_query = np.ascontiguousarray(np.asarray(inputs["w_query"], dtype=np.float32)[0])
    sk = np.asarray(inputs["sub_keys"], dtype=np.float32)[0]
    skT = np.ascontiguousarray(sk.transpose(0, 1, 3, 2).reshape(16, 128, 128))
    exp_u = np.ascontiguousarray(np.asarray(inputs["expert_u"], dtype=np.float32)[0])
    exp_v = np.ascontiguousarray(np.asarray(inputs["expert_v"], dtype=np.float32)[0])
    gmix = np.ascontiguousarray(np.asarray(inputs["norm_mix_gain"], dtype=np.float32)[0].reshape(16, 128).T)
    gffn = np.ascontiguousarray(np.broadcast_to(np.asarray(inputs["norm_ffn_gain"], dtype=np.float32)[0][None, :], (128, D)))
    gfin = np.ascontiguousarray(np.broadcast_to(np.asarray(inputs["norm_final_gain"], dtype=np.float32)[None, :], (128, D)))
    bfor = np.ascontiguousarray(np.broadcast_to(np.asarray(inputs["b_forget"], dtype=np.float32)[0][None, :], (128, 8)))
    idx = np.arange(128)
    ident = np.eye(128, dtype=np.float32)
    triu = (idx[:, None] <= idx[None, :]).astype(np.float32)
    cst = np.ascontiguousarray(np.concatenate([ident, triu, np.ones((128, 128), np.float32)], axis=1))
    xT = [np.ascontiguousarray(x[b].T) for b in range(2)]
    iota = np.ascontiguousarray(np.broadcast_to(np.arange(256, dtype=np.float32)[None, :], (128, 256)))
    maps = []
    toks = []
    for c in range(8):
        b, j = c // 4, c % 4
        tok = np.concatenate([np.arange((4 * i + j) * 128, (4 * i + j + 1) * 128) for i in range(8)])
        toks.append((b, tok))
        sb01 = np.zeros((128, 4, 128), np.float32)
        fx01 = np.zeros((128, 4, 128), np.float32)
        for k in range(4):
            if k < j:
                sb01[:, k, :] = 1.0
                fx01[:, k, :] = 1.0
            elif k == j:
                sb01[:, k, :] = (idx[None, :] < idx[:, None])
                fx01[:, k, :] = (idx[None, :] <= idx[:, None])
        sbadd = (1.0 - sb01) * NEG
        fxadd = (1.0 - fx01) * NEG
        msk = np.ascontiguousarray(np.concatenate(
            [sb01.reshape(128, 512), sbadd.reshape(128, 512), fxadd.reshape(128, 512), np.zeros((128, 512), np.float32)],
            axis=1).astype(np.float32))
        sel = np.zeros((128, 4), np.float32)
        sel[:, j] = 1.0
        maps.append({
            "xT_full": xT[b], "xT_own": np.ascontiguousarray(xT[b][:, tok]), "x_own": np.ascontiguousarray(x[b][tok]),
            "w_in": w_in, "w_ba": w_ba, "w_bb": w_bb, "w_out": w_out, "w_query": w_query, "skT": skT,
            "exp_u": exp_u, "exp_v": exp_v, "gmix": gmix, "gffn": gffn, "gfin": gfin, "bfor": bfor,
            "cst": cst, "msk": msk, "sel": sel, "iota": iota,
        })
    return maps, toks


def kernel(**inputs):
    maps, toks = make_core_inputs(inputs)
    nc = build_program()
    res = run_bass_kernel_spmd(nc, maps, core_ids=list(range(8)))
    outp = np.zeros((2, 4096, D), np.float32)
    for c in range(8):
        b, tok = toks[c]
        outp[b, tok] = np.asarray(res.results[c]["out"], dtype=np.float32)
    return outp
```
